# Optimizing a Trainium2 kernel written in Bass

```python
import math
import jax, jax.numpy as jnp
from jax import lax
import numpy as np

D_MODEL = 2048
BATCH = 4
SEQ = 2048
DEPTH = 1

MLA_HEADS = 8
QK_NOPE_DIM = 128
QK_ROPE_DIM = 64
V_HEAD_DIM = 128
Q_LORA_RANK = 768
KV_LORA_RANK = 512
ROPE_THETA = 10000.0
ATTN_BLOCK = 128
MLA_WIDTH = MLA_HEADS * V_HEAD_DIM

SGU_GROUPS = 8
SGU_GROUP_DIM = 128
SGU_CHUNK = 128
SGU_WIDTH = SGU_GROUPS * SGU_GROUP_DIM

MIX_WIDTH = MLA_WIDTH + SGU_WIDTH
IN_PROJ_DIM = Q_LORA_RANK + KV_LORA_RANK + QK_ROPE_DIM + 2 * SGU_WIDTH

N_GROUPS = 4
EXPERTS_PER_GROUP = 8
N_EXPERTS = N_GROUPS * EXPERTS_PER_GROUP
TOP_K_EXPERTS = 2
EXPERT_FF = 512

DEEPNORM_ALPHA = (2 * DEPTH) ** 0.25
DEEPNORM_BETA = (8 * DEPTH) ** -0.25
EPS = 1e-6
N_MOD = 6

kernel_name = "hymba_mla_sgu_hiermoe_deepnorm_adaln"


def _layernorm(x, g=None, b=None):
    xf = x.astype(jnp.float32)
    mu = jnp.mean(xf, axis=-1, keepdims=True)
    var = jnp.mean(jnp.square(xf - mu), axis=-1, keepdims=True)
    y = (xf - mu) * lax.rsqrt(var + EPS)
    if g is not None:
        y = y * g.astype(jnp.float32) + b.astype(jnp.float32)
    return y.astype(x.dtype)


def _rmsnorm(x, g):
    xf = x.astype(jnp.float32)
    y = xf * lax.rsqrt(jnp.mean(jnp.square(xf), axis=-1, keepdims=True) + EPS)
    return (y * g.astype(jnp.float32)).astype(x.dtype)


def _rope(x, cos, sin):
    x1, x2 = jnp.split(x, 2, axis=-1)
    return jnp.concatenate([x1 * cos - x2 * sin, x2 * cos + x1 * sin], axis=-1)


def _causal_block_attention(q, k, v):
    B, S, H, Dk = q.shape
    nb = S // ATTN_BLOCK
    scale = Dk ** -0.5
    q_blocks = q.reshape(B, nb, ATTN_BLOCK, H, Dk).transpose(1, 0, 2, 3, 4)
    key_pos = jnp.arange(S)

    def one_block(args):
        qb, bi = args
        s = jnp.einsum('bqhd,bkhd->bhqk', qb, k).astype(jnp.float32) * scale
        q_pos = bi * ATTN_BLOCK + jnp.arange(ATTN_BLOCK)
        mask = key_pos[None, :] <= q_pos[:, None]
        s = jnp.where(mask, s, -1e30)
        p = jax.nn.softmax(s, axis=-1).astype(v.dtype)
        return jnp.einsum('bhqk,bkhd->bqhd', p, v)

    out = lax.map(one_block, (q_blocks, jnp.arange(nb)))
    return out.transpose(1, 0, 2, 3, 4).reshape(B, S, H, v.shape[-1])


def _mixer(h, positions, w_in, q_norm_g, w_uq, kv_norm_g, w_ukv,
           sgu_norm_g, sgu_norm_b, w_spatial, b_spatial, w_o):
    B, S, _ = h.shape
    proj = h @ w_in
    c_q, c_kv, k_pe, z = jnp.split(
        proj, [Q_LORA_RANK, Q_LORA_RANK + KV_LORA_RANK, Q_LORA_RANK + KV_LORA_RANK + QK_ROPE_DIM], axis=-1)

    q = (_rmsnorm(c_q, q_norm_g) @ w_uq).reshape(B, S, MLA_HEADS, QK_NOPE_DIM + QK_ROPE_DIM)
    q_nope, q_pe = jnp.split(q, [QK_NOPE_DIM], axis=-1)
    kv = (_rmsnorm(c_kv, kv_norm_g) @ w_ukv).reshape(B, S, MLA_HEADS, QK_NOPE_DIM + V_HEAD_DIM)
    k_nope, v = jnp.split(kv, [QK_NOPE_DIM], axis=-1)
    inv_freq = 1.0 / (ROPE_THETA ** (jnp.arange(0, QK_ROPE_DIM, 2, dtype=jnp.float32) / QK_ROPE_DIM))
    ang = positions.astype(jnp.float32)[..., None] * inv_freq
    cos = jnp.cos(ang).astype(h.dtype)
    sin = jnp.sin(ang).astype(h.dtype)
    q_pe = _rope(q_pe, cos[:, :, None, :], sin[:, :, None, :])
    k_pe = _rope(k_pe, cos, sin)
    q_full = jnp.concatenate([q_nope, q_pe], axis=-1)
    k_full = jnp.concatenate(
        [k_nope, jnp.broadcast_to(k_pe[:, :, None, :], (B, S, MLA_HEADS, QK_ROPE_DIM))], axis=-1)
    attn = _causal_block_attention(q_full, k_full, v).reshape(B, S, MLA_WIDTH)

    u, vs = jnp.split(jax.nn.gelu(z), 2, axis=-1)
    vs = _layernorm(vs, sgu_norm_g, sgu_norm_b)
    nc = S // SGU_CHUNK
    vs = vs.reshape(B, nc, SGU_CHUNK, SGU_GROUPS, SGU_GROUP_DIM)
    causal = jnp.tril(jnp.ones((SGU_CHUNK, SGU_CHUNK), dtype=bool))
    ws = jnp.where(causal[None], w_spatial, jnp.zeros_like(w_spatial))
    mixed = jnp.einsum('gts,bcsgd->bctgd', ws, vs) + b_spatial.T[None, None, :, :, None]
    sgu = u * mixed.reshape(B, S, SGU_WIDTH)

    return jnp.concatenate([attn, sgu], axis=-1) @ w_o


def _hier_moe(h, w_router_group, b_router_group, w_router_expert, b_router_expert,
              w_gate, w_up, w_down):
    B, S, D = h.shape
    xt = h.reshape(B * S, D)
    lg = (xt @ w_router_group + b_router_group).astype(jnp.float32)
    pg = jax.nn.softmax(lg, axis=-1)
    pg_top, g_idx = lax.top_k(pg, 1)
    le = (xt @ w_router_expert + b_router_expert).astype(jnp.float32).reshape(-1, N_GROUPS, EXPERTS_PER_GROUP)
    le_sel = jnp.take_along_axis(le, g_idx[:, :, None], axis=1)[:, 0]
    pe = jax.nn.softmax(le_sel, axis=-1)
    pe_top, e_idx = lax.top_k(pe, TOP_K_EXPERTS)
    pe_top = pe_top / jnp.sum(pe_top, axis=-1, keepdims=True)
    weights = pg_top * pe_top
    expert_ids = g_idx * EXPERTS_PER_GROUP + e_idx
    combine = jnp.sum(jax.nn.one_hot(expert_ids, N_EXPERTS, dtype=jnp.float32) * weights[..., None], axis=1)
    combine = combine.astype(h.dtype)
    out = jnp.zeros_like(xt)
    for e in range(N_EXPERTS):
        hid = jax.nn.silu(xt @ w_gate[e]) * (xt @ w_up[e])
        out = out + combine[:, e:e + 1] * (hid @ w_down[e])
    return out.reshape(B, S, D)


def setup_inputs(seed: int = 0) -> dict:
    key = jax.random.key(seed)
    ks = jax.random.split(key, 26)
    L, D = DEPTH, D_MODEL
    f32 = jnp.float32

    def nrm(k, shape, scale):
        return jax.random.normal(k, shape, f32) * scale

    x = jax.random.normal(ks[0], (BATCH, SEQ, D), f32)
    c = jax.random.normal(ks[1], (BATCH, D), f32)
    offsets = jax.random.randint(ks[2], (BATCH,), 0, 4096, dtype=jnp.int32)
    positions = (offsets[:, None] + jnp.arange(SEQ, dtype=jnp.int32)[None, :]).astype(jnp.int32)
    return {
        "x": x,
        "c": c,
        "positions": positions,
        "w_ada": nrm(ks[3], (L, D, N_MOD * D), 0.5 * D ** -0.5),
        "b_ada": nrm(ks[4], (L, N_MOD * D), 0.01),
        "w_in": nrm(ks[5], (L, D, IN_PROJ_DIM), D ** -0.5),
        "q_norm_g": 1.0 + nrm(ks[6], (L, Q_LORA_RANK), 0.02),
        "w_uq": nrm(ks[7], (L, Q_LORA_RANK, MLA_HEADS * (QK_NOPE_DIM + QK_ROPE_DIM)), Q_LORA_RANK ** -0.5),
        "kv_norm_g": 1.0 + nrm(ks[8], (L, KV_LORA_RANK), 0.02),
        "w_ukv": nrm(ks[9], (L, KV_LORA_RANK, MLA_HEADS * (QK_NOPE_DIM + V_HEAD_DIM)), KV_LORA_RANK ** -0.5),
        "sgu_norm_g": 1.0 + nrm(ks[10], (L, SGU_WIDTH), 0.02),
        "sgu_norm_b": nrm(ks[11], (L, SGU_WIDTH), 0.01),
        "w_spatial": nrm(ks[12], (L, SGU_GROUPS, SGU_CHUNK, SGU_CHUNK), SGU_CHUNK ** -0.5),
        "b_spatial": 1.0 + nrm(ks[13], (L, SGU_GROUPS, SGU_CHUNK), 0.02),
        "w_o": nrm(ks[14], (L, MIX_WIDTH, D), DEEPNORM_BETA * MIX_WIDTH ** -0.5),
        "ln1_g": 1.0 + nrm(ks[15], (L, D), 0.02),
        "ln1_b": nrm(ks[16], (L, D), 0.01),
        "w_router_group": nrm(ks[17], (L, D, N_GROUPS), D ** -0.5),
        "b_router_group": nrm(ks[18], (L, N_GROUPS), 0.01),
        "w_router_expert": nrm(ks[19], (L, D, N_EXPERTS), D ** -0.5),
        "b_router_expert": nrm(ks[20], (L, N_EXPERTS), 0.01),
        "w_gate": nrm(ks[21], (L, N_EXPERTS, D, EXPERT_FF), D ** -0.5),
        "w_up": nrm(ks[22], (L, N_EXPERTS, D, EXPERT_FF), D ** -0.5),
        "w_down": nrm(ks[23], (L, N_EXPERTS, EXPERT_FF, D), DEEPNORM_BETA * EXPERT_FF ** -0.5),
        "ln2_g": 1.0 + nrm(ks[24], (L, D), 0.02),
        "ln2_b": nrm(ks[25], (L, D), 0.01),
    }


def reference(x, c, positions, w_ada, b_ada, w_in, q_norm_g, w_uq, kv_norm_g, w_ukv,
              sgu_norm_g, sgu_norm_b, w_spatial, b_spatial, w_o, ln1_g, ln1_b,
              w_router_group, b_router_group, w_router_expert, b_router_expert,
              w_gate, w_up, w_down, ln2_g, ln2_b):
    for l in range(DEPTH):
        mod = (c @ w_ada[l] + b_ada[l])[:, None, :]
        sh1, sc1, g1, sh2, sc2, g2 = jnp.split(mod, N_MOD, axis=-1)
        h = _layernorm(x) * (1.0 + sc1) + sh1
        y = _mixer(h, positions, w_in[l], q_norm_g[l], w_uq[l], kv_norm_g[l], w_ukv[l],
                   sgu_norm_g[l], sgu_norm_b[l], w_spatial[l], b_spatial[l], w_o[l])
        x = _layernorm(DEEPNORM_ALPHA * x + g1 * y, ln1_g[l], ln1_b[l])
        h = _layernorm(x) * (1.0 + sc2) + sh2
        y = _hier_moe(h, w_router_group[l], b_router_group[l], w_router_expert[l], b_router_expert[l],
                      w_gate[l], w_up[l], w_down[l])
        x = _layernorm(DEEPNORM_ALPHA * x + g2 * y, ln2_g[l], ln2_b[l])
    return x
```

```python
import numpy as np
import concourse.bass as bass
import concourse.mybir as mybir
from concourse.bass_utils import run_bass_kernel_spmd

F32 = mybir.dt.float32
BF16 = mybir.dt.bfloat16
I32 = mybir.dt.int32
AF = mybir.ActivationFunctionType
ALU = mybir.AluOpType
AX = mybir.AxisListType

D = 2048
KC = 16
NB = 8
TOK = 1024
TL = 2048
H = 8
NEXP = 32
ALPHA = 2.0 ** 0.25
EPS = 1e-6
SM_SCALE = 192.0 ** -0.5
NEG = -30000.0
SBUF_BASE = 16640
PERSIST = 28672
SBUF_LIMIT = 229312

DEBUG = {}
SPARSE = True
PSUM_NAMES = ("pbig", "ptA", "ptB", "pq0", "pq1")


def dsize(dt):
    return {F32: 4, BF16: 2, I32: 4}[dt]


class Chan:
    def __init__(self, name, inc):
        self.name = name
        self.inc = inc
        self.count = 0
        self.sem = None


class Sched:
    ENG = ("pe", "act", "dve", "pool", "sp")

    def __init__(self, nc):
        self.nc = nc
        self.chan = {e: Chan(e, 1) for e in self.ENG}
        self.dch = {}
        self.ops = {e: [] for e in self.ENG}
        self.seen = {e: {} for e in self.ENG}
        self.last_w = {}
        self.readers = {}
        self.pending = {}
        self.allocs = []
        self.tomb = []
        self.tiles = {}
        self.peak = 0

    def alloc(self, name, shape, dt, base=SBUF_BASE + PERSIST, top=False):
        size = int(np.prod(shape[1:])) * dsize(dt)
        size = (size + 63) // 64 * 64
        self.allocs.sort()
        if top:
            off = SBUF_LIMIT // 64 * 64 - size
            for (o, s, _) in reversed(self.allocs):
                if o >= off + size:
                    continue
                if o + s <= off:
                    break
                off = o - size
            assert off >= base, f"SBUF overflow (top) allocating {name}"
        else:
            off = base
            for (o, s, _) in self.allocs:
                if o + s <= off:
                    continue
                if off + size <= o:
                    break
                off = o + s
        assert off + size <= SBUF_LIMIT, f"SBUF overflow allocating {name} size {size} at {off}"
        self.allocs.append((off, size, name))
        self.peak = max(self.peak, off + size)
        pend = {}
        keep = []
        for (o, s, deps) in self.tomb:
            if o < off + size and off < o + s:
                for ch, v in deps.items():
                    pend[ch] = max(pend.get(ch, 0), v)
            keep.append((o, s, deps))
        self.tomb = keep
        if pend:
            self.pending[name] = pend
        t = self.nc.alloc_sbuf_tensor_at(name, list(shape), dt, offset=off)
        self.tiles[name] = t
        return t

    def free(self, name):
        ent = [a for a in self.allocs if a[2] == name]
        assert len(ent) == 1, name
        self.allocs.remove(ent[0])
        deps = dict(self.pending.get(name, {}))
        for k, (ch, v) in self.last_w.items():
            if k[0] == name:
                deps[ch] = max(deps.get(ch, 0), v)
        for k, rd in self.readers.items():
            if k[0] == name:
                for ch, v in rd.items():
                    deps[ch] = max(deps.get(ch, 0), v)
        self.tomb.append((ent[0][0], ent[0][1], deps))

    def dchan(self, name):
        if name not in self.dch:
            self.dch[name] = Chan("d_" + name, 16)
        return self.dch[name]

    def _deps(self, eng, reads, writes):
        own = self.chan[eng]
        deps = {}

        def add(ch, v, raw):
            if ch is own and eng in ("pe", "sp"):
                return
            if deps.get(ch, 0) < v:
                deps[ch] = v

        for r in reads:
            lw = self.last_w.get(r)
            if lw:
                add(lw[0], lw[1], True)
            for ch, v in self.pending.get(r[0], {}).items():
                add(ch, v, True)
            if r[0] in PSUM_NAMES:
                for ch, v in self.readers.get(r, {}).items():
                    add(ch, v, False)
        for w in writes:
            lw = self.last_w.get(w)
            if lw:
                add(lw[0], lw[1], False)
            for ch, v in self.readers.get(w, {}).items():
                add(ch, v, False)
            for ch, v in self.pending.get(w[0], {}).items():
                add(ch, v, True)
        waits = []
        for ch, v in deps.items():
            if self.seen[eng].get(ch, 0) < v:
                waits.append((ch, v))
                self.seen[eng][ch] = v
        return waits

    def _record(self, ch, tick, reads, writes):
        for r in reads:
            rd = self.readers.setdefault(r, {})
            if rd.get(ch, 0) < tick:
                rd[ch] = tick
        for w in writes:
            self.last_w[w] = (ch, tick)
            self.readers[w] = {}

    def op(self, eng, fn, reads=(), writes=(), mark=True):
        waits = self._deps(eng, reads, writes)
        ch = self.chan[eng]
        if mark:
            ch.count += 1
            tick = ch.count
        else:
            tick = ch.count + 1
        self._record(ch, tick, reads, writes)
        self.ops[eng].append((waits, fn, ch if mark else None))

    def dma(self, queue, chname, fn, reads=(), writes=()):
        waits = self._deps(queue, reads, writes)
        ch = self.dchan(chname)
        ch.count += 16
        self._record(ch, ch.count, reads, writes)
        self.ops[queue].append((waits, fn, ch))

    def wait_all(self, eng, chans):
        waits = []
        for ch in chans:
            if ch.count > 0 and self.seen[eng].get(ch, 0) < ch.count:
                waits.append((ch, ch.count))
                self.seen[eng][ch] = ch.count
        self.ops[eng].append((waits, None, None))


class _Stop(Exception):
    pass


def build_nc(debug=(), stop=None, nexp_decl=NEXP):
    nc = bass.Bass("TRN2", target_bir_lowering=False)
    S = Sched(nc)

    def din(name, shape, dt=F32):
        return nc.dram_tensor(name, list(shape), dt, kind="ExternalInput").ap()

    x_loc = din("x_loc", [TL, D])
    cT_d = din("cT", [128, KC])
    pos_d = din("pos", [1, TL], I32)
    qidx_d = din("qidx", [128, NB])
    oblk_d = din("oblk", [128, NB])
    rope_d = din("ropec", [64, 2])
    w_ada = din("w_ada", [D, 6 * D])
    b_ada = din("b_ada", [1, 6 * D])
    w_in = din("w_in", [D, 3392])
    qg_d = din("qg", [128, 6])
    w_uq = din("w_uq", [768, 1536])
    kvg_d = din("kvg", [128, 4])
    w_ukv = din("w_ukv", [512, 2048])
    sgug_d = din("sgug", [1, 1024])
    sgub_d = din("sgub", [1, 1024])
    wspT_d = din("wspT", [128, 8, 128])
    bsp_d = din("bsp", [1, 1024])
    w_o = din("w_o", [D, D])
    ln1g_d = din("ln1g", [1, D])
    ln1b_d = din("ln1b", [1, D])
    wr_d = din("wr", [D, 36])
    br_d = din("br", [1, 36])
    if SPARSE:
        w_gate = din("w_gate", [nexp_decl * 512, 2048])
        w_up = din("w_up", [nexp_decl * 512, 2048])
        w_down = din("w_down", [nexp_decl * 512, D])
    else:
        w_gate = din("w_gate", [nexp_decl, D, 512])
        w_up = din("w_up", [nexp_decl, D, 512])
        w_down = din("w_down", [nexp_decl, 512, D])
    ln2g_d = din("ln2g", [1, D])
    ln2b_d = din("ln2b", [1, D])
    out_d = nc.dram_tensor("out", [TOK, D], F32, kind="ExternalOutput").ap()
    dbg_out = {}

    poff = [SBUF_BASE]

    def palloc(name, shape, dt):
        size = int(np.prod(shape[1:])) * dsize(dt)
        size = (size + 63) // 64 * 64
        t = nc.alloc_sbuf_tensor_at(name, list(shape), dt, offset=poff[0])
        poff[0] += size
        assert poff[0] <= SBUF_BASE + PERSIST
        return t

    ident = palloc("ident", [128, 128], BF16)
    ones_bf = palloc("ones_bf", [128, 128], BF16)
    onesf = palloc("onesf", [1, 128], F32)
    iot = palloc("iot", [128, 128], F32)
    tri = palloc("tri", [128, 128], F32)
    maskT = palloc("maskT", [128, 128], F32)
    cT = palloc("cT_sb", [128, KC], F32)
    cT_bf = palloc("cT_bf", [128, KC], BF16)
    modT = palloc("modT", [128, 64], F32)
    g1_b = palloc("g1_b", [128, D], F32)
    g2_b = palloc("g2_b", [128, D], F32)
    qidx = palloc("qidx_sb", [128, NB], F32)
    oblk = palloc("oblk_sb", [128, NB], F32)
    maskb = palloc("maskb", [128, NB, NB], F32)
    ropec = palloc("ropec_sb", [64, 2], F32)
    qg = palloc("qg_sb", [128, 6], F32)
    kvg = palloc("kvg_sb", [128, 4], F32)
    brb = palloc("brb", [128, 36], F32)
    logits = palloc("logits", [128, NB, 36], F32)
    comb = palloc("comb", [128, NB, 32], F32)
    st6s = [palloc(f"st6_{i}", [128, 4, 6], F32) for i in range(2)]
    mvs = [palloc(f"mv_{i}", [128, 2], F32) for i in range(2)]
    sds = [palloc(f"sd_{i}", [128, 1], F32) for i in range(2)]
    rstds = [palloc(f"rstd_{i}", [128, 1], F32) for i in range(2)]
    nmrs = [palloc(f"nmr_{i}", [128, 1], F32) for i in range(2)]
    lnc = [0]
    rs1 = palloc("rs1", [128, 1], F32)
    rs2 = palloc("rs2", [128, 1], F32)
    rs3 = palloc("rs3", [128, 1], F32)
    r_mg = palloc("r_mg", [128, NB], F32)
    r_sg = palloc("r_sg", [128, NB], F32)
    r_pg = palloc("r_pg", [128, NB], F32)
    r_eg = palloc("r_eg", [128, NB, 4], F32)
    r_oh = palloc("r_oh", [128, NB, 4], F32)
    r_t4 = palloc("r_t4", [128, NB, 4, 8], F32)
    r_sel = palloc("r_sel", [128, NB, 8], F32)
    r_l1 = palloc("r_l1", [128, NB], F32)
    r_l2 = palloc("r_l2", [128, NB], F32)
    r_oh1 = palloc("r_oh1", [128, NB, 8], F32)
    r_oh2 = palloc("r_oh2", [128, NB, 8], F32)
    r_msk = palloc("r_msk", [128, NB, 8], F32)
    r_w1 = palloc("r_w1", [128, NB], F32)
    r_w2 = palloc("r_w2", [128, NB], F32)
    r_ce = palloc("r_ce", [128, NB, 8], F32)

    pbig = nc.alloc_psum_tensor("pbig", [128, 2048], F32)
    ptA = nc.alloc_psum_tensor("ptA", [128, 1024], BF16)
    ptB = nc.alloc_psum_tensor("ptB", [128, 1024], BF16)
    pq0 = nc.alloc_psum_tensor("pq0", [128, 512], F32)
    pq1 = nc.alloc_psum_tensor("pq1", [128, 512], F32)

    def bank(i):
        return pbig[:, i * 512:(i + 1) * 512], ("pbig", i)

    def act(out, in_, func, reads, writes, bias=0.0, scale=1.0, accum=None):
        if accum is None:
            S.op("act", lambda e: e.activation(out=out, in_=in_, func=func, bias=bias, scale=scale),
                 reads, writes)
        else:
            S.op("act", lambda e: e.activation(out=out, in_=in_, func=func, bias=bias, scale=scale,
                                               accum_out=accum), reads, writes)

    def tscal(eng, out, in0, s1, s2, op0, op1, reads, writes):
        if s2 is None:
            S.op(eng, lambda e: e.tensor_scalar(out=out, in0=in0, scalar1=s1, scalar2=None, op0=op0),
                 reads, writes)
        else:
            S.op(eng, lambda e: e.tensor_scalar(out=out, in0=in0, scalar1=s1, scalar2=s2, op0=op0, op1=op1),
                 reads, writes)

    def tt(eng, out, in0, in1, op, reads, writes):
        S.op(eng, lambda e: e.tensor_tensor(out=out, in0=in0, in1=in1, op=op), reads, writes)

    def stt(out, in0, scalar, in1, op0, op1, reads, writes):
        S.op("dve", lambda e: e.scalar_tensor_tensor(out=out, in0=in0, scalar=scalar, in1=in1, op0=op0, op1=op1),
             reads, writes)

    def tcopy(eng, out, in_, reads, writes):
        S.op(eng, lambda e: e.tensor_copy(out=out, in_=in_), reads, writes)

    def mm(out, lhsT, rhs, start, stop, reads, writes=(), mark=False):
        S.op("pe", lambda e: e.matmul(out, lhsT=lhsT, rhs=rhs, start=start, stop=stop), reads, writes, mark=mark)

    def mm_group(out, okey, items):
        n = len(items)
        for i, (l, r, rk) in enumerate(items):
            first, last = (i == 0), (i == n - 1)
            mm(out, l, r, first, last, rk, writes=(okey,) if (first or last) else (), mark=last)

    def transpose(out, okey, in_, ikey, mark):
        S.op("pe", lambda e: e.transpose(out=out, in_=in_, identity=ident[:]), (ikey, ("ident",)),
             (okey,), mark=mark)

    def dma(queue, chname, out, in_, reads, writes):
        S.dma(queue, chname, lambda e: e.dma_start(out=out, in_=in_), reads, writes)

    def dbg(name, tile_ap, shape, dt, key):
        if name in debug:
            d = nc.dram_tensor("dbg_" + name, list(shape), dt, kind="ExternalOutput").ap()
            dbg_out[name] = d
            dma("sp", "dbg_" + name, d, tile_ap, (key,), ())

    def hk(blks):
        return tuple(("hT", b, par) for b in blks for par in (0, 1))

    def k2(base):
        return (base + (0,), base + (1,))

    def ckpt(name):
        if stop == name:
            raise _Stop()

    def evac(i, out, in_, reads, wkey):
        if i % 2 == 0:
            tcopy("dve", out, in_, reads, (wkey + (0,),))
        else:
            act(out, in_, AF.Copy, reads, (wkey + (1,),))

    def ln_stats(src, skey, n):
        k = lnc[0] % 2
        lnc[0] += 1
        st6, mv, sd, rstd, nmr = st6s[k], mvs[k], sds[k], rstds[k], nmrs[k]
        K6, KM, KS, KR, KN = (f"st6_{k}",), (f"mv_{k}",), (f"sd_{k}",), (f"rstd_{k}",), (f"nmr_{k}",)
        nch = n // 512
        for i in range(nch):
            S.op("dve", lambda e, i=i: e.bn_stats(out=st6[:, i, :], in_=src[:, i * 512:(i + 1) * 512]),
                 (skey,), (K6,))
        S.op("dve", lambda e: e.bn_aggr(out=mv[:], in_=st6[:, 0:nch, :]), (K6,), (KM,))
        act(sd[:], mv[:, 1:2], AF.Sqrt, (KM,), (KS,), bias=EPS, scale=1.0)
        S.op("dve", lambda e: e.reciprocal(out=rstd[:], in_=sd[:]), (KS,), (KR,))
        tscal("dve", nmr[:], mv[:, 0:1], rstd[:], -1.0, ALU.mult, ALU.mult, (KM, KR), (KN,))
        return rstd, nmr, KR, KN

    def body():
        S.op("pool", lambda e: e.iota(iot[:], pattern=[[1, 128]], base=0, channel_multiplier=-1,
                                      allow_small_or_imprecise_dtypes=True), (), (("iot",),))
        tscal("dve", ident[:], iot[:], 0.0, None, ALU.is_equal, None, (("iot",),), (("ident",),))
        tscal("dve", tri[:], iot[:], 0.0, NEG, ALU.is_gt, ALU.mult, (("iot",),), (("tri",),))
        tscal("dve", maskT[:], iot[:], 0.0, None, ALU.is_ge, None, (("iot",),), (("maskT",),))
        S.op("dve", lambda e: e.memset(ones_bf[:], 1.0), (), (("ones_bf",),))
        S.op("dve", lambda e: e.memset(onesf[:], 1.0), (), (("onesf",),))
        for (t, d_, nm) in ((cT, cT_d, "cT"), (qidx, qidx_d, "qidx"), (oblk, oblk_d, "oblk"), (ropec, rope_d, "ropec"),
                            (qg, qg_d, "qg"), (kvg, kvg_d, "kvg")):
            dma("sp", "c_" + nm, t[:], d_, (), ((nm,),))
        dma("sp", "c_brb", brb[:], br_d[0:1, :].to_broadcast([128, 36]), (), (("brb",),))
        tcopy("dve", cT_bf[:], cT[:], (("cT",),), (("cT_bf",),))
        for j in range(NB):
            tscal("dve", maskb[:, j, :], oblk[:], qidx[:, j:j + 1], NEG, ALU.is_gt, ALU.mult,
                  (("oblk",), ("qidx",)), (("maskb",),))

        cosT = S.alloc("cosT", [64, TL], BF16)
        sinT = S.alloc("sinT", [64, TL], BF16)
        posi = S.alloc("posi", [64, TL], I32)
        ang = S.alloc("ang", [64, TL], F32)
        tmpa = S.alloc("tmpa", [64, TL], F32)
        tmpi = S.alloc("tmpi", [64, TL], I32)
        dma("sp", "c_pos", posi[:], pos_d[0:1, :].to_broadcast([64, TL]), (), (("posi",),))
        tcopy("dve", ang[:], posi[:], (("posi",),), (("ang",),))
        tscal("dve", ang[:], ang[:], ropec[:, 0:1], None, ALU.mult, None, (("ang",), ("ropec",)), (("ang",),))
        TWO_PI = 2.0 * np.pi
        C1 = 6.28125
        C2 = TWO_PI - C1
        for which, shift, dst in (("sin", 0.0, sinT), ("cos", np.pi / 2, cosT)):
            tscal("dve", tmpa[:], ang[:], float(shift), None, ALU.add, None, (("ang",),), (("tmpa",),))
            tscal("dve", tmpi[:], tmpa[:], float(1.0 / TWO_PI), None, ALU.mult, None, (("tmpa",),), (("tmpi",),))
            kf = S.alloc("kf", [64, TL], F32)
            tcopy("dve", kf[:], tmpi[:], (("tmpi",),), (("kf",),))
            stt(tmpa[:], kf[:], -C1, tmpa[:], ALU.mult, ALU.add, (("kf",), ("tmpa",)), (("tmpa",),))
            stt(tmpa[:], kf[:], -C2, tmpa[:], ALU.mult, ALU.add, (("kf",), ("tmpa",)), (("tmpa",),))
            tscal("dve", kf[:], tmpa[:], float(np.pi), -TWO_PI, ALU.is_gt, ALU.mult, (("tmpa",),), (("kf",),))
            tt("dve", tmpa[:], tmpa[:], kf[:], ALU.add, (("tmpa",), ("kf",)), (("tmpa",),))
            tscal("dve", kf[:], tmpa[:], float(-np.pi), TWO_PI, ALU.is_lt, ALU.mult, (("tmpa",),), (("kf",),))
            tt("dve", tmpa[:], tmpa[:], kf[:], ALU.add, (("tmpa",), ("kf",)), (("tmpa",),))
            tscal("dve", tmpa[:], tmpa[:], float(np.pi), float(-np.pi), ALU.min, ALU.max, (("tmpa",),), (("tmpa",),))
            if which == "sin":
                act(kf[:], tmpa[:], AF.Sin, (("tmpa",),), (("kf",),))
                tscal("dve", dst[:], kf[:], ropec[:, 1:2], None, ALU.mult, None, (("kf",), ("ropec",)), ((which + "T",),))
            else:
                act(dst[:], tmpa[:], AF.Sin, (("tmpa",),), ((which + "T",),))
            S.free("kf")
        for nm in ("posi", "ang", "tmpa", "tmpi"):
            S.free(nm)
        dbg("cosT", cosT[:], [64, TL], BF16, ("cosT",))
        dbg("sinT", sinT[:], [64, TL], BF16, ("sinT",))
        dbg("maskb", maskb[:], [128, NB, NB], F32, ("maskb",))
        ckpt("const")
        RCOS, RSIN = ("cosT",), ("sinT",)

        wa = [S.alloc(f"wa{i}", [128, KC, 512], BF16) for i in range(2)]
        brow = [S.alloc(f"brow{i}", [1, 512], F32) for i in range(2)]
        rowsb = [S.alloc(f"rowsb{i}", [1, 512], F32) for i in range(2)]
        w_ada_v = w_ada.rearrange("(kc p) n -> p kc n", p=128)
        fm_slot = {0: 0, 1: 1, 3: 2, 4: 3}
        for j in range(8):
            sl = j % 2
            v, q4 = j // 4, j % 4
            dma("pool", f"wa{sl}", wa[sl][:], w_ada_v[:, :, j * 512:(j + 1) * 512], (), ((f"wa{sl}",),))
            dma("sp", f"brow{sl}", brow[sl][:], b_ada[0:1, j * 512:(j + 1) * 512], (), ((f"brow{sl}",),))
            mm_group(pq0[0:1, :], ("pq0",),
                     [(cT_bf[:, kc:kc + 1], wa[sl][:, kc, :], (("cT_bf",), (f"wa{sl}",))) for kc in range(KC)])
            tt("dve", rowsb[sl][:], pq0[0:1, :], brow[sl][:], ALU.add, (("pq0",), (f"brow{sl}",)), ((f"rowsb{sl}",),))
            base = fm_slot[v] * 16 + q4 * 4
            for q in range(4):
                mm(pq1[:, base + q:base + q + 1], rowsb[sl][0:1, q * 128:(q + 1) * 128], onesf[0:1, 0:1],
                   True, True, ((f"rowsb{sl}",), ("onesf",)), (("pq1",),), mark=True)
        tcopy("dve", modT[:, 0:32], pq1[:, 0:32], (("pq1",),), (("modT", 1),))
        tscal("dve", modT[:, 16:32], modT[:, 16:32], 1.0, None, ALU.add, None, (("modT", 1),), (("modT", 1),))
        for nm in ("wa0", "wa1", "brow0", "brow1", "rowsb0", "rowsb1"):
            S.free(nm)

        DCW = 256
        defer = {"c": 0}

        def mod_chunk_def(wa2, brow2, rowsb2):
            c = defer["c"]
            if c >= (4 * D) // DCW:
                return
            defer["c"] += 1
            sl = c % 2
            col0 = 2 * D + c * DCW
            v, off = col0 // D, col0 % D
            dma("pool", f"wb{sl}", wa2[sl][:], w_ada_v[:, :, col0:col0 + DCW], (), ((f"wb{sl}",),))
            dma("sp", f"brow2{sl}", brow2[sl][:], b_ada[0:1, col0:col0 + DCW], (), ((f"browb{sl}",),))
            mm_group(pq0[0:1, 0:DCW], ("pq0",),
                     [(cT_bf[:, kc:kc + 1], wa2[sl][:, kc, :], (("cT_bf",), (f"wb{sl}",))) for kc in range(KC)])
            tt("dve", rowsb2[sl][:], pq0[0:1, 0:DCW], brow2[sl][:], ALU.add, (("pq0",), (f"browb{sl}",)),
               ((f"rowsbb{sl}",),))
            if v in (3, 4):
                base = fm_slot[v] * 16 + off // 128
                for q in range(2):
                    mm(pq0[:, 256 + q:257 + q], rowsb2[sl][0:1, q * 128:(q + 1) * 128], onesf[0:1, 0:1],
                       True, True, ((f"rowsbb{sl}",), ("onesf",)), (("pq0",),), mark=True)
                tscal("dve", modT[:, base:base + 2], pq0[:, 256:258], 1.0 if v == 4 else 0.0, None, ALU.add, None,
                      (("pq0",),), (("modT", 2),))
            else:
                gb = g1_b if v == 2 else g2_b
                gk = ("g1_b",) if v == 2 else ("g2_b",)
                mm(pq0[:, 256:512], onesf[0:1, :], rowsb2[sl][:], True, True, ((f"rowsbb{sl}",), ("onesf",)),
                   (("pq0",),), mark=True)
                tcopy("dve", gb[:, off:off + DCW], pq0[:, 256:512], (("pq0",),), (gk,))

        ckpt("A")

        xs = [S.alloc(f"xs{i}", [128, D], F32) for i in range(2)]
        xn = [S.alloc(f"xn{i}", [128, D], BF16) for i in range(2)]
        XN = [xn]
        cnt = {"ln": 0, "ev": 0}

        def ln_mod_T(src, skey, dst_fn, dkey, moff):
            i = cnt["ln"] % 2
            cnt["ln"] += 1
            rstd, nmr, KR, KN = ln_stats(src, skey, D)
            xn = XN[0]
            act(xn[i][:], src, AF.Identity, (skey, KR, KN), ((f"xn{i}",),), bias=nmr[:], scale=rstd[:])
            for half, (pt, pk) in enumerate(((ptA, ("ptA",)), (ptB, ("ptB",)))):
                for q in range(8):
                    kc = half * 8 + q
                    transpose(pt[:, q * 128:(q + 1) * 128], pk, xn[i][:, kc * 128:(kc + 1) * 128], (f"xn{i}",),
                              mark=(q == 7))
                for q in range(8):
                    kc = half * 8 + q
                    sc = modT[:, moff + 16 + kc:moff + 17 + kc]
                    sh = modT[:, moff + kc:moff + kc + 1]
                    if half == 0:
                        tscal("dve", dst_fn(kc), pt[:, q * 128:(q + 1) * 128], sc, sh, ALU.mult, ALU.add,
                              (pk, ("modT", 1 if moff == 0 else 2)), (dkey + (0,),))
                    else:
                        act(dst_fn(kc), pt[:, q * 128:(q + 1) * 128], AF.Identity, (pk, ("modT", 1 if moff == 0 else 2)), (dkey + (1,),),
                            bias=sh, scale=sc)

        hT = S.alloc("hT", [128, KC, TOK], BF16)
        for blk in range(NB):
            sl = blk % 2
            dma("sp", f"xs{sl}", xs[sl][:], x_loc[blk * 128:(blk + 1) * 128, :], (), ((f"xs{sl}",),))
            ln_mod_T(xs[sl][:], (f"xs{sl}",), lambda kc, blk=blk: hT[:, kc, blk * 128:(blk + 1) * 128], ("hT", blk), 0)
        dbg("hT", hT[:], [128, KC, TOK], BF16, ("hT", 7, 1))
        ckpt("hT")
        for nm in ("xs0", "xs1", "xn0", "xn1"):
            S.free(nm)

        wi = [S.alloc(f"wi{i}", [128, KC, 512], BF16) for i in range(2)]
        w_in_v = w_in.rearrange("(kc p) n -> p kc n", p=128)
        wcnt = [0]

        def load_win(c0, ncol):
            sl = wcnt[0] % 2
            wcnt[0] += 1
            dma("pool", f"wi{sl}", wi[sl][:, :, 0:ncol], w_in_v[:, :, c0:c0 + ncol], (), ((f"wi{sl}",),))
            return wi[sl], (f"wi{sl}",)

        bcnt = [0]

        def nbank():
            b = bcnt[0] % 4
            bcnt[0] += 1
            return bank(b)

        mixT = S.alloc("mixT", [128, 16, TOK], BF16, top=True)
        uT = S.alloc("uT", [128, 8, TOK], BF16)
        vsg = S.alloc("vsg", [128, NB, 1024], BF16)
        sgug_b = S.alloc("sgug_b", [128, 1024], F32)
        sgub_b = S.alloc("sgub_b", [128, 1024], F32)
        bsp_b = S.alloc("bsp_b", [128, 1024], F32)
        wsT = S.alloc("wsT", [128, 8, 128], BF16)
        wsT_f = S.alloc("wsT_f", [128, 8, 128], F32)
        dma("sp", "c_sgug", sgug_b[:], sgug_d[0:1, :].to_broadcast([128, 1024]), (), (("sgug_b",),))
        dma("sp", "c_sgub", sgub_b[:], sgub_d[0:1, :].to_broadcast([128, 1024]), (), (("sgub_b",),))
        dma("sp", "c_bsp", bsp_b[:], bsp_d[0:1, :].to_broadcast([128, 1024]), (), (("bsp_b",),))
        dma("sp", "c_wsT", wsT_f[:], wspT_d, (), (("wsT_f",),))
        tt("dve", wsT[:], wsT_f[:], maskT[:].unsqueeze(1).to_broadcast([128, 8, 128]), ALU.mult,
           (("wsT_f",), ("maskT",)), (("wsT",),))
        S.free("wsT_f")
        ckpt("B3a")

        def gelu_evac(out, bk, bkey, wkey):
            act(out, bk, AF.Gelu, (bkey,), (wkey,))

        for ch in range(2):
            wt, wk = load_win(1344 + ch * 512, 512)
            for m in range(4):
                for n in range(2):
                    bk, bkey = nbank()
                    mm_group(bk, bkey, [(wt[:, kc, m * 128:(m + 1) * 128], hT[:, kc, n * 512:(n + 1) * 512],
                                         (wk,) + hk(range(4 * n, 4 * n + 4))) for kc in range(KC)])
                    gelu_evac(uT[:, ch * 4 + m, n * 512:(n + 1) * 512], bk, bkey, ("uT",))
        ckpt("B3b")
        for ch in range(2):
            wt, wk = load_win(2368 + ch * 512, 512)
            for blk in range(NB):
                bk, bkey = nbank()
                mm_group(bk, bkey, [(hT[:, kc, blk * 128:(blk + 1) * 128], wt[:, kc, 0:512],
                                     (wk,) + hk([blk])) for kc in range(KC)])
                gelu_evac(vsg[:, blk, ch * 512:(ch + 1) * 512], bk, bkey, ("vsg", blk))
        dbg("uT", uT[:], [128, 8, TOK], BF16, ("uT",))
        dbg("vsg", vsg[:], [128, NB, 1024], BF16, ("vsg", 0))
        ckpt("B3c")
        vtmp = [S.alloc(f"vtmp{i}", [128, 1024], F32) for i in range(2)]
        vsn = [S.alloc(f"vsn{i}", [128, 1024], BF16) for i in range(2)]
        for blk in range(NB):
            i = blk % 2
            rstd, nmr, KR, KN = ln_stats(vsg[:, blk, :], ("vsg", blk), 1024)
            act(vtmp[i][:], vsg[:, blk, :], AF.Identity, (("vsg", blk), KR, KN), ((f"vtmp{i}",),),
                bias=nmr[:], scale=rstd[:])
            tt("dve", vtmp[i][:], vtmp[i][:], sgug_b[:], ALU.mult, ((f"vtmp{i}",), ("sgug_b",)), ((f"vtmp{i}",),))
            tt("dve", vsn[i][:], vtmp[i][:], sgub_b[:], ALU.add, ((f"vtmp{i}",), ("sgub_b",)), ((f"vsn{i}",),))
            for half in range(2):
                bk, bkey = nbank()
                for gg in range(4):
                    g = half * 4 + gg
                    mm(bk[:, gg * 128:(gg + 1) * 128], vsn[i][:, g * 128:(g + 1) * 128], wsT[:, g, :], True, True,
                       ((f"vsn{i}",), ("wsT",)), (bkey,), mark=(gg == 3))
                tt("dve", vtmp[i][:, half * 512:(half + 1) * 512], bk, bsp_b[:, half * 512:(half + 1) * 512], ALU.add,
                   (bkey, ("bsp_b",)), ((f"vtmp{i}",),))
                tt("dve", mixT[:, 8 + half * 4:12 + half * 4, blk * 128:(blk + 1) * 128],
                   vtmp[i][:, half * 512:(half + 1) * 512].rearrange("p (g t) -> p g t", g=4),
                   uT[:, half * 4:half * 4 + 4, blk * 128:(blk + 1) * 128], ALU.mult,
                   ((f"vtmp{i}",), ("uT",)), (("mixT", "s"),))
        for nm in ("uT", "vsg", "sgug_b", "sgub_b", "bsp_b", "wsT", "vtmp0", "vtmp1", "vsn0", "vsn1"):
            S.free(nm)
        dbg("sguT", mixT[:, 8:16, :], [128, 8, TOK], BF16, ("mixT", "s"))
        ckpt("B3")

        sqt = [S.alloc(f"sqt{i}", [128, 512], BF16) for i in range(2)]
        rsb = S.alloc("rsb", [128, 512], F32)
        rt1 = S.alloc("rt1", [64, 512], F32)
        rt2 = S.alloc("rt2", [64, 512], F32)
        sqc = [0]

        def rms_feature_major(dstT, dkey, nchunk, wt_fn, src_fn, skeys, gain, ntot, gkey):
            for m in range(nchunk):
                bk, bkey = nbank()
                wt, wk, c0 = wt_fn(m)
                mm_group(bk, bkey, [(wt[:, kc, c0:c0 + 128], src_fn(kc), (wk,) + skeys) for kc in range(KC)])
                tcopy("dve", dstT[:, m, :], bk, (bkey,), (dkey,))
                i = sqc[0] % 2
                sqc[0] += 1
                act(sqt[i][:], bk, AF.Square, (bkey,), ((f"sqt{i}",),))
                mm(pq0[:, :], ones_bf[:], sqt[i][:], m == 0, m == nchunk - 1, ((f"sqt{i}",), ("ones_bf",)),
                   (("pq0",),) if (m == 0 or m == nchunk - 1) else (), mark=(m == nchunk - 1))
            act(rsb[:], pq0[:, :], AF.Sqrt, (("pq0",),), (("rsb",),), bias=EPS, scale=1.0 / ntot)
            S.op("dve", lambda e: e.reciprocal(out=rsb[:], in_=rsb[:]), (("rsb",),), (("rsb",),))
            for m in range(nchunk):
                stt(dstT[:, m, :], dstT[:, m, :], gain[:, m:m + 1], rsb[:], ALU.mult, ALU.mult,
                    (dkey, ("rsb",), gkey), (dkey,))

        def rope_evac(pe_ps, pe_key, sw_ps, sw_key, tok0, dst, dkey):
            tt("dve", rt1[:], pe_ps, cosT[:, tok0:tok0 + 512], ALU.mult, (pe_key, RCOS), (("rt1",),))
            tt("dve", rt2[:], sw_ps, sinT[:, tok0:tok0 + 512], ALU.mult, (sw_key, RSIN), (("rt2",),))
            tt("dve", dst, rt1[:], rt2[:], ALU.add, (("rt1",), ("rt2",)), (dkey,))

        qnT = S.alloc("qnT", [128, H, TOK], BF16, top=True)
        qrT = S.alloc("qrT", [64, H, TOK], BF16, top=True)
        cqT = S.alloc("cqT", [128, 6, TOK], BF16)
        wqn = S.alloc("wqn", [128, 6, H, 128], BF16)
        wqp = S.alloc("wqp", [128, 6, H, 96], BF16)
        w_uq_v = w_uq.rearrange("(kc p) (h d) -> p kc h d", p=128, d=192)
        for kc in range(6):
            dma("pool", "wq", wqn[:, kc, :, :], w_uq_v[:, kc, :, 0:128], (), (("wqn", kc), ("wqn", "ser")))
            dma("pool", "wq", wqp[:, kc, :, 0:64], w_uq_v[:, kc, :, 128:192], (), (("wqp", kc, 0), ("wqn", "ser")))
            dma("pool", "wq", wqp[:, kc, :, 64:96], w_uq_v[:, kc, :, 128:160], (), (("wqp", kc, 1), ("wqn", "ser")))
        wA, wAk = load_win(0, 512)
        wB, wBk = load_win(512, 256)
        for n in range(2):
            rms_feature_major(cqT[:, :, n * 512:(n + 1) * 512], ("cqT", n), 6,
                              lambda m: (wA, wAk, m * 128) if m < 4 else (wB, wBk, (m - 4) * 128),
                              lambda kc, n=n: hT[:, kc, n * 512:(n + 1) * 512], hk(range(4 * n, 4 * n + 4)), qg, 768.0, ("qg",))
        for n in range(2):
            for h in range(H):
                bk, bkey = nbank()
                mm_group(bk, bkey, [(wqn[:, kc, h, :], cqT[:, kc, n * 512:(n + 1) * 512], (("wqn", kc), ("cqT", n)))
                                    for kc in range(6)])
                evac(h, qnT[:, h, n * 512:(n + 1) * 512], bk, (bkey,), ("qnT",))
                b1, b1k = nbank()
                mm_group(b1[0:64, :], b1k, [(wqp[:, kc, h, 0:64], cqT[:, kc, n * 512:(n + 1) * 512],
                                             (("wqp", kc, 0), ("wqp", kc, 1), ("cqT", n))) for kc in range(6)])
                b2, b2k = nbank()
                mm_group(b2[0:64, :], b2k, [(wqp[:, kc, h, 32:96], cqT[:, kc, n * 512:(n + 1) * 512],
                                             (("wqp", kc, 0), ("wqp", kc, 1), ("cqT", n))) for kc in range(6)])
                rope_evac(b1[0:64, :], b1k, b2[0:64, :], b2k, n * 512, qrT[:, h, n * 512:(n + 1) * 512], ("qrT",))
        for nm in ("cqT", "wqn", "wqp"):
            S.free(nm)
        dbg("qnT", qnT[:], [128, H, TOK], BF16, ("qnT", 1))
        dbg("qrT", qrT[:], [64, H, TOK], BF16, ("qrT",))
        ckpt("B2")

        ckvT = S.alloc("ckvT", [128, 4, TL], BF16, top=True)
        krT = S.alloc("krT", [64, TL], BF16, top=True)
        wkp = S.alloc("wkp", [128, KC, 96], BF16, top=True)
        wC, wCk = load_win(768, 512)
        dma("pool", "wkpa", wkp[:, :, 0:64], w_in_v[:, :, 1280:1344], (), (("wkp", 0),))
        dma("pool", "wkpb", wkp[:, :, 64:96], w_in_v[:, :, 1280:1312], (), (("wkp", 1),))

        def kv_group(n, src_fn, skeys):
            rms_feature_major(ckvT[:, :, n * 512:(n + 1) * 512], ("ckvT", n), 4,
                              lambda m: (wC, wCk, m * 128), src_fn, skeys, kvg, 512.0, ("kvg",))
            b1, b1k = nbank()
            mm_group(b1[0:64, :], b1k, [(wkp[:, kc, 0:64], src_fn(kc), (("wkp", 0), ("wkp", 1)) + skeys) for kc in range(KC)])
            b2, b2k = nbank()
            mm_group(b2[0:64, :], b2k, [(wkp[:, kc, 32:96], src_fn(kc), (("wkp", 0), ("wkp", 1)) + skeys) for kc in range(KC)])
            rope_evac(b1[0:64, :], b1k, b2[0:64, :], b2k, n * 512, krT[:, n * 512:(n + 1) * 512], ("krT",))

        for n in range(2):
            kv_group(n, lambda kc, n=n: hT[:, kc, n * 512:(n + 1) * 512], hk(range(4 * n, 4 * n + 4)))
        S.free("hT")
        S.free("wi0" if wCk == ("wi1",) else "wi1")
        xs = [S.alloc(f"xs{i}", [128, D], F32) for i in range(2)]
        xn = [S.alloc(f"xn{i}", [128, D], BF16) for i in range(2)]
        XN[0] = xn
        hTo = [S.alloc(f"hTo{i}", [128, KC, 512], BF16) for i in range(2)]
        for n in range(2, 4):
            i = n % 2
            for bb in range(4):
                blk = n * 4 + bb
                sl = blk % 2
                dma("sp", f"xs{sl}", xs[sl][:], x_loc[blk * 128:(blk + 1) * 128, :], (), ((f"xs{sl}",),))
                ln_mod_T(xs[sl][:], (f"xs{sl}",), lambda kc, i=i, bb=bb: hTo[i][:, kc, bb * 128:(bb + 1) * 128],
                         (f"hTo{i}",), 0)
            kv_group(n, lambda kc, i=i: hTo[i][:, kc, :], k2((f"hTo{i}",)))
        for nm in ("hTo0", "hTo1", wCk[0], "wkp", "xs0", "xs1", "xn0", "xn1",
                   "sqt0", "sqt1", "rsb", "rt1", "rt2", "cosT", "sinT"):
            S.free(nm)
        dbg("ckvT", ckvT[:], [128, 4, TL], BF16, ("ckvT", 0))
        dbg("krT", krT[:], [64, TL], BF16, ("krT",))
        ckpt("B1b")

        wkk = S.alloc("wkk", [128, 4, H, 128], BF16)
        wvv = S.alloc("wvv", [128, 4, H, 128], BF16)
        w_ukv_v = w_ukv.rearrange("(kc p) (h two d) -> p kc h two d", p=128, two=2, d=128)
        for kc in range(4):
            dma("pool", "wkv", wkk[:, kc, :, :], w_ukv_v[:, kc, :, 0, :], (), (("wkk", kc), ("wkk", "ser")))
            dma("pool", "wkv", wvv[:, kc, :, :], w_ukv_v[:, kc, :, 1, :], (), (("wvv", kc), ("wkk", "ser")))
        knT = S.alloc("knT", [128, H, TL], BF16, top=True)
        V = S.alloc("V", [128, 16, H * 128], BF16, top=True)
        ec = 0
        for n in range(4):
            for h in range(H):
                bk, bkey = nbank()
                mm_group(bk, bkey, [(wkk[:, kc, h, :], ckvT[:, kc, n * 512:(n + 1) * 512], (("wkk", kc), ("ckvT", n)))
                                    for kc in range(4)])
                evac(ec, knT[:, h, n * 512:(n + 1) * 512], bk, (bkey,), ("knT",))
                ec += 1
            for bb in range(4):
                lb = n * 4 + bb
                for g in range(2):
                    bk, bkey = nbank()
                    mm_group(bk, bkey, [(ckvT[:, kc, lb * 128:(lb + 1) * 128],
                                         wvv[:, kc, g * 4:(g + 1) * 4, :], (("wvv", kc), ("ckvT", n))) for kc in range(4)])
                    evac(ec, V[:, lb, g * 512:(g + 1) * 512], bk, (bkey,), ("V",))
                    ec += 1
        for nm in ("ckvT", "wkk", "wvv"):
            S.free(nm)
        dbg("knT", knT[:], [128, H, TL], BF16, ("knT", 1))
        dbg("V", V[:], [128, 16, H * 128], BF16, ("V", 1))
        ckpt("B1c")

        Pm = [S.alloc(f"Pm{i}", [128, TL], BF16) for i in range(2)]
        PT = [S.alloc(f"PT{i}", [128, 16, 128], BF16) for i in range(2)]
        attn = [S.alloc(f"attn{i}", [128, H * 128], BF16) for i in range(2)]
        mx = S.alloc("mx", [128, 1], F32)
        nb_ = S.alloc("nb_", [128, 1], F32)
        rsum = S.alloc("rsum", [128, 1], F32)
        rinv = S.alloc("rinv", [128, 1], F32)
        wa2 = [S.alloc(f"wb{i}", [128, KC, DCW], BF16) for i in range(2)]
        brow2 = [S.alloc(f"browb{i}", [1, DCW], F32) for i in range(2)]
        rowsb2 = [S.alloc(f"rowsbb{i}", [1, DCW], F32) for i in range(2)]
        it = 0
        for j in range(NB):
            nk = j + 1
            W = nk * 128
            ai = j % 2
            for h in range(H):
                pi = it % 2
                it += 1
                if it % 2 == 0:
                    mod_chunk_def(wa2, brow2, rowsb2)
                segs = []
                for side in range(2):
                    k0 = side * 1024
                    c = 0
                    while c < W:
                        w_ = min(512 - ((side * W + c) % 512), W - c)
                        segs.append((side * W + c, k0 + c, w_))
                        c += w_
                for (col, key0, w_) in segs:
                    bnk = col // 512
                    assert (col + w_ - 1) // 512 == bnk
                    mm(pbig[:, col:col + w_], qnT[:, h, j * 128:(j + 1) * 128], knT[:, h, key0:key0 + w_], True, False,
                       k2(("qnT",)) + k2(("knT",)), (("pbig", bnk),), mark=False)
                    mm(pbig[:, col:col + w_], qrT[:, h, j * 128:(j + 1) * 128], krT[:, key0:key0 + w_], False, True,
                       (("qrT",), ("krT",)), (("pbig", bnk),), mark=True)
                banks = tuple(("pbig", b) for b in range((2 * W + 511) // 512))
                dcol = j * 128
                tt("dve", pbig[:, dcol:dcol + 128], pbig[:, dcol:dcol + 128], tri[:], ALU.add,
                   (("pbig", dcol // 512), ("tri",)), (("pbig", dcol // 512),))
                tt("dve", pbig[:, W:2 * W].rearrange("p (b k) -> p b k", k=128),
                   pbig[:, W:2 * W].rearrange("p (b k) -> p b k", k=128),
                   maskb[:, j, 0:nk].unsqueeze(2).to_broadcast([128, nk, 128]), ALU.add,
                   banks + (("maskb",),), banks)
                S.op("dve", lambda e, W=W: e.reduce_max(out=mx[:], in_=pbig[:, 0:2 * W], axis=AX.X), banks, (("mx",),))
                tscal("dve", nb_[:], mx[:], -SM_SCALE, None, ALU.mult, None, (("mx",),), (("nb_",),))
                act(Pm[pi][:, 0:2 * W], pbig[:, 0:2 * W], AF.Exp, banks + (("nb_",),), ((f"Pm{pi}",), ("rsum",)),
                    bias=nb_[:], scale=SM_SCALE, accum=rsum[:])
                S.op("dve", lambda e: e.reciprocal(out=rinv[:], in_=rsum[:]), (("rsum",),), (("rinv",),))
                nblk = 2 * nk
                for bi, b0 in enumerate(range(0, nblk, 8)):
                    pt, pk = (ptA, ("ptA",)) if bi % 2 == 0 else (ptB, ("ptB",))
                    nb8 = min(8, nblk - b0)
                    for q in range(nb8):
                        transpose(pt[:, q * 128:(q + 1) * 128], pk, Pm[pi][:, (b0 + q) * 128:(b0 + q + 1) * 128],
                                  (f"Pm{pi}",), mark=(q == nb8 - 1))
                    src = pt[:, 0:nb8 * 128].rearrange("p (b q) -> p b q", q=128)
                    if bi % 2 == 0:
                        tcopy("dve", PT[pi][:, b0:b0 + nb8, :], src, (pk,), ((f"PT{pi}",),))
                    else:
                        act(PT[pi][:, b0:b0 + nb8, :], src, AF.Copy, (pk,), ((f"PT{pi}",),))
                items = []
                for b in range(nblk):
                    lb = b if b < nk else 8 + (b - nk)
                    items.append((PT[pi][:, b, :], V[:, lb, h * 128:(h + 1) * 128], ((f"PT{pi}",),) + k2(("V",))))
                mm_group(pq1[:, 0:128], ("pq1",), items)
                tscal("dve", attn[ai][:, h * 128:(h + 1) * 128], pq1[:, 0:128], rinv[:], None, ALU.mult, None,
                      (("pq1",), ("rinv",)), ((f"attn{ai}",),))
            for q in range(8):
                transpose(ptA[:, q * 128:(q + 1) * 128], ("ptA",), attn[ai][:, q * 128:(q + 1) * 128], (f"attn{ai}",),
                          mark=(q == 7))
            tcopy("dve", mixT[:, 0:8, j * 128:(j + 1) * 128], ptA[:, :].rearrange("p (c q) -> p c q", q=128),
                  (("ptA",),), (("mixT", "a"),))
        while defer["c"] < (4 * D) // DCW:
            mod_chunk_def(wa2, brow2, rowsb2)
        for nm in ("wb0", "wb1", "browb0", "browb1", "rowsbb0", "rowsbb1"):
            S.free(nm)
        for nm in ("Pm0", "Pm1", "PT0", "PT1", "attn0", "attn1", "mx", "nb_", "rsum", "rinv",
                   "qnT", "qrT", "knT", "krT", "V"):
            S.free(nm)
        dbg("mixT", mixT[:], [128, 16, TOK], BF16, ("mixT", "a"))
        ckpt("C")

        x1 = S.alloc("x1", [128, NB, D], F32, top=True)
        xs = [S.alloc(f"xs{i}", [128, D], F32) for i in range(2)]
        wo = [S.alloc(f"wo{i}", [128, KC, 512], BF16) for i in range(2)]
        lng = S.alloc("lng", [128, D], F32)
        lnb = S.alloc("lnb", [128, D], F32)
        dma("sp", "c_ln1g", lng[:], ln1g_d[0:1, :].to_broadcast([128, D]), (), (("lng",),))
        dma("sp", "c_ln1b", lnb[:], ln1b_d[0:1, :].to_broadcast([128, D]), (), (("lnb",),))
        w_o_v = w_o.rearrange("(kc p) n -> p kc n", p=128)
        for cg in range(4):
            sl = cg % 2
            dma("pool", f"wo{sl}", wo[sl][:], w_o_v[:, :, cg * 512:(cg + 1) * 512], (), ((f"wo{sl}",),))
            for blk in range(NB):
                bk, bkey = nbank()
                mm_group(bk, bkey, [(mixT[:, kc, blk * 128:(blk + 1) * 128], wo[sl][:, kc, :],
                                     ((f"wo{sl}",), ("mixT", "a"), ("mixT", "s"))) for kc in range(KC)])
                tt("dve", x1[:, blk, cg * 512:(cg + 1) * 512], bk, g1_b[:, cg * 512:(cg + 1) * 512], ALU.mult,
                   (bkey, ("g1_b",)), (("x1", blk),))
        for blk in range(NB):
            sl = blk % 2
            dma("sp", f"xs{sl}", xs[sl][:], x_loc[blk * 128:(blk + 1) * 128, :], (), ((f"xs{sl}",),))
            stt(x1[:, blk, :], xs[sl][:], ALPHA, x1[:, blk, :], ALU.mult, ALU.add, ((f"xs{sl}",), ("x1", blk)),
                (("x1", blk),))
            rstd, nmr, KR, KN = ln_stats(x1[:, blk, :], ("x1", blk), D)
            act(x1[:, blk, :], x1[:, blk, :], AF.Identity, (("x1", blk), KR, KN), (("x1", blk),),
                bias=nmr[:], scale=rstd[:])
            tt("pool", x1[:, blk, :], x1[:, blk, :], lng[:], ALU.mult, (("x1", blk), ("lng",)), (("x1", blk),))
            tt("dve", x1[:, blk, :], x1[:, blk, :], lnb[:], ALU.add, (("x1", blk), ("lnb",)), (("x1", blk),))
        for nm in ("mixT", "wo0", "wo1", "lng", "lnb", "xs0", "xs1"):
            S.free(nm)
        dbg("x1", x1[:], [128, NB, D], F32, ("x1", 0))
        ckpt("D")

        h2T = S.alloc("h2T", [128, KC, TOK], BF16, top=True)
        xn = [S.alloc(f"xn{i}", [128, D], BF16) for i in range(2)]
        XN[0] = xn
        wr = S.alloc("wr", [128, KC, 36], BF16)
        dma("pool", "wr", wr[:], wr_d.rearrange("(kc p) n -> p kc n", p=128), (), (("wr",),))
        if SPARSE:
            h2tok = S.alloc("h2tok", [128, NB, D], BF16, top=True)
            sc2_b = S.alloc("sc2_b", [128, D], F32)
            sh2_b = S.alloc("sh2_b", [128, D], F32)
            identf = S.alloc("identf", [128, 128], F32)
            onesF = S.alloc("onesF", [128, 128], F32)
            dg = [S.alloc(f"dg{i}", [128, 128], F32) for i in range(2)]
            h2tmp = S.alloc("h2tmp", [128, D], F32)
            tscal("dve", identf[:], iot[:], 0.0, None, ALU.is_equal, None, (("iot",),), (("identf",),))
            S.op("dve", lambda e: e.memset(onesF[:], 1.0), (), (("onesF",),))
            dgc = 0
            for (dst, dk, c0) in ((sh2_b, ("sh2_b",), 32), (sc2_b, ("sc2_b",), 48)):
                for kq in range(4):
                    bk, bkey = nbank()
                    for q in range(4):
                        kc = kq * 4 + q
                        di = dgc % 2
                        dgc += 1
                        tscal("dve", dg[di][:], identf[:], modT[:, c0 + kc:c0 + kc + 1], None, ALU.mult, None,
                              (("identf",), ("modT", 2)), ((f"dg{di}",),))
                        mm(bk[:, q * 128:(q + 1) * 128], onesF[:], dg[di][:], True, True,
                           ((f"dg{di}",), ("onesF",)), (bkey,), mark=True)
                    tcopy("dve", dst[:, kq * 512:(kq + 1) * 512], bk, (bkey,), (dk,))
        for blk in range(NB):
            ln_mod_T(x1[:, blk, :], ("x1", blk), lambda kc, blk=blk: h2T[:, kc, blk * 128:(blk + 1) * 128],
                     ("h2T",), 32)
            if SPARSE:
                xi = (cnt["ln"] - 1) % 2
                tt("pool", h2tmp[:], XN[0][xi][:], sc2_b[:], ALU.mult, ((f"xn{xi}",), ("sc2_b",)), (("h2tmp",),))
                tt("dve", h2tok[:, blk, :], h2tmp[:], sh2_b[:], ALU.add, (("h2tmp",), ("sh2_b",)), (("h2tok", blk),))
            mm_group(pq0[:, 0:36], ("pq0",), [(h2T[:, kc, blk * 128:(blk + 1) * 128], wr[:, kc, :],
                                               k2(("h2T",)) + (("wr",),)) for kc in range(KC)])
            tt("dve", logits[:, blk, :], pq0[:, 0:36], brb[:], ALU.add, (("pq0",), ("brb",)), (("logits",),))
            tscal("pool", x1[:, blk, :], x1[:, blk, :], ALPHA, None, ALU.mult, None, (("x1", blk),), (("x1", blk),))
        S.free("xn0"); S.free("xn1"); S.free("wr")
        if SPARSE:
            for nm in ("sc2_b", "sh2_b", "identf", "onesF", "dg0", "dg1", "h2tmp"):
                S.free(nm)
        dbg("h2T", h2T[:], [128, KC, TOK], BF16, ("h2T", 1))
        dbg("logits", logits[:], [128, NB, 36], F32, ("logits",))

        L = ("logits",)
        lg = logits[:, :, 0:4]
        le = logits[:, :, 4:36].rearrange("p b (g e) -> p b g e", e=8)

        def bc3(t2, n):
            return t2.unsqueeze(2).to_broadcast([128, NB, n])

        S.op("dve", lambda e: e.tensor_reduce(out=r_mg[:], in_=lg, axis=AX.X, op=ALU.max), (L,), (("r_mg",),))
        tt("dve", r_eg[:], lg, bc3(r_mg[:], 4), ALU.subtract, (L, ("r_mg",)), (("r_eg",),))
        tt("dve", r_oh[:], lg, bc3(r_mg[:], 4), ALU.is_equal, (L, ("r_mg",)), (("r_oh",),))
        act(r_eg[:], r_eg[:], AF.Exp, (("r_eg",),), (("r_eg",),))
        S.op("dve", lambda e: e.tensor_reduce(out=r_sg[:], in_=r_eg[:], axis=AX.X, op=ALU.add), (("r_eg",),), (("r_sg",),))
        S.op("dve", lambda e: e.reciprocal(out=r_pg[:], in_=r_sg[:]), (("r_sg",),), (("r_pg",),))
        tt("dve", r_t4[:], le, r_oh[:].unsqueeze(3).to_broadcast([128, NB, 4, 8]), ALU.mult, (L, ("r_oh",)), (("r_t4",),))
        S.op("dve", lambda e: e.tensor_reduce(out=r_sel[:], in_=r_t4[:].rearrange("p b g e -> p b e g"), axis=AX.X,
                                              op=ALU.add), (("r_t4",),), (("r_sel",),))
        S.op("dve", lambda e: e.tensor_reduce(out=r_l1[:], in_=r_sel[:], axis=AX.X, op=ALU.max), (("r_sel",),), (("r_l1",),))
        tt("dve", r_oh1[:], r_sel[:], bc3(r_l1[:], 8), ALU.is_equal, (("r_sel",), ("r_l1",)), (("r_oh1",),))
        stt(r_msk[:], r_oh1[:], -1e30, r_sel[:], ALU.mult, ALU.add, (("r_oh1",), ("r_sel",)), (("r_msk",),))
        S.op("dve", lambda e: e.tensor_reduce(out=r_l2[:], in_=r_msk[:], axis=AX.X, op=ALU.max), (("r_msk",),), (("r_l2",),))
        tt("dve", r_oh2[:], r_msk[:], bc3(r_l2[:], 8), ALU.is_equal, (("r_msk",), ("r_l2",)), (("r_oh2",),))
        tt("dve", r_w1[:], r_l2[:], r_l1[:], ALU.subtract, (("r_l2",), ("r_l1",)), (("r_w1",),))
        act(r_w1[:], r_w1[:], AF.Exp, (("r_w1",),), (("r_w1",),))
        tscal("dve", r_w1[:], r_w1[:], 1.0, None, ALU.add, None, (("r_w1",),), (("r_w1",),))
        S.op("dve", lambda e: e.reciprocal(out=r_w1[:], in_=r_w1[:]), (("r_w1",),), (("r_w1",),))
        tscal("dve", r_w2[:], r_w1[:], -1.0, 1.0, ALU.mult, ALU.add, (("r_w1",),), (("r_w2",),))
        tt("dve", r_w1[:], r_w1[:], r_pg[:], ALU.mult, (("r_w1",), ("r_pg",)), (("r_w1",),))
        tt("dve", r_w2[:], r_w2[:], r_pg[:], ALU.mult, (("r_w2",), ("r_pg",)), (("r_w2",),))
        tt("dve", r_ce[:], r_oh1[:], bc3(r_w1[:], 8), ALU.mult, (("r_oh1",), ("r_w1",)), (("r_ce",),))
        tt("dve", r_oh2[:], r_oh2[:], bc3(r_w2[:], 8), ALU.mult, (("r_oh2",), ("r_w2",)), (("r_oh2",),))
        tt("dve", r_ce[:], r_ce[:], r_oh2[:], ALU.add, (("r_ce",), ("r_oh2",)), (("r_ce",),))
        comb4 = comb[:].rearrange("p b (g e) -> p b g e", e=8)
        for g in range(4):
            tt("dve", comb4[:, :, g, :], r_ce[:], bc3(r_oh[:, :, g], 8), ALU.mult, (("r_ce",), ("r_oh",)), (("comb",),))
        dbg("comb", comb[:], [128, NB, 32], F32, ("comb",))
        ckpt("R")

        if SPARSE:
            S.free("h2T")
            NSLOT = 48
            NROW = NSLOT * 128
            Mf = S.alloc("Mf", [128, NB, 32], F32)
            Mb = S.alloc("Mb", [128, NB, 32], BF16)
            Ub = S.alloc("Ub", [128, 128], BF16)
            rank = S.alloc("rank", [128, NB, 32], F32)
            cntt = S.alloc("cntt", [128, 32], F32)
            nst = S.alloc("nst", [128, 32], F32)
            cs = [S.alloc(f"cs{i}", [128, 32], F32) for i in range(2)]
            sot = S.alloc("sot", [128, 32], F32)
            pos = S.alloc("pos", [128, NB, 32], F32)
            posm = S.alloc("posm", [128, NB, 32], F32)
            ptmp = S.alloc("ptmp", [128, NB, 32], F32)
            pAf = S.alloc("pAf", [128, NB], F32)
            pBf = S.alloc("pBf", [128, NB], F32)
            pAi = S.alloc("pAi", [128, NB], I32)
            pBi = S.alloc("pBi", [128, NB], I32)
            wA = S.alloc("wA", [128, NB], F32)
            wB = S.alloc("wB", [128, NB], F32)
            siota = S.alloc("siota", [128, NSLOT], F32)
            ecmp = S.alloc("ecmp", [128, NSLOT, 32], F32)
            eidf = S.alloc("eidf", [128, NSLOT], F32)
            eidi = S.alloc("eidi", [128, NSLOT], I32)
            C = ("comb",)
            tscal("dve", Mf[:], comb[:], 0.0, None, ALU.is_gt, None, (C,), (("Mf",),))
            tcopy("dve", Mb[:], Mf[:], (("Mf",),), (("Mb",),))
            tscal("dve", Ub[:], iot[:], 0.0, None, ALU.is_gt, None, (("iot",),), (("Ub",),))
            S.op("pool", lambda e: e.iota(siota[:], pattern=[[1, NSLOT]], base=0, channel_multiplier=0,
                                          allow_small_or_imprecise_dtypes=True), (), (("siota",),))
            for b in range(NB):
                bk, bkey = nbank()
                items = [(ones_bf[:], Mb[:, b2, :], (("Mb",), ("ones_bf",))) for b2 in range(b)]
                items.append((Ub[:], Mb[:, b, :], (("Mb",), ("Ub",))))
                mm_group(bk[:, 0:32], bkey, items)
                tcopy("dve", rank[:, b, :], bk[:, 0:32], (bkey,), (("rank",),))
            bk, bkey = nbank()
            mm_group(bk[:, 0:32], bkey, [(ones_bf[:], Mb[:, b2, :], (("Mb",), ("ones_bf",))) for b2 in range(NB)])
            tcopy("dve", cntt[:], bk[:, 0:32], (bkey,), (("cntt",),))
            tscal("dve", nst[:], cntt[:], 0.0, None, ALU.is_gt, None, (("cntt",),), (("nst",),))
            for k in range(1, 8):
                stt(nst[:], cntt[:], 128.0 * k, nst[:], ALU.is_gt, ALU.add, (("cntt",), ("nst",)), (("nst",),))
            tcopy("dve", cs[0][:], nst[:], (("nst",),), (("cs0",),))
            cur = 0
            for dstep in (1, 2, 4, 8, 16):
                nxt = 1 - cur
                tcopy("dve", cs[nxt][:], cs[cur][:], ((f"cs{cur}",),), ((f"cs{nxt}",),))
                tt("dve", cs[nxt][:, dstep:32], cs[cur][:, dstep:32], cs[cur][:, 0:32 - dstep], ALU.add,
                   ((f"cs{cur}",),), ((f"cs{nxt}",),))
                cur = nxt
            tt("dve", sot[:], cs[cur][:], nst[:], ALU.subtract, ((f"cs{cur}",), ("nst",)), (("sot",),))
            for b in range(NB):
                stt(pos[:, b, :], sot[:], 128.0, rank[:, b, :], ALU.mult, ALU.add, (("sot",), ("rank",)), (("pos",),))
            tscal("dve", ptmp[:], Mf[:], -1.0e6, 1.0e6, ALU.mult, ALU.add, (("Mf",),), (("ptmp",),))
            tt("dve", posm[:], pos[:], ptmp[:], ALU.add, (("pos",), ("ptmp",)), (("posm",),))
            S.op("dve", lambda e: e.tensor_reduce(out=pAf[:], in_=posm[:], axis=AX.X, op=ALU.min), (("posm",),), (("pAf",),))
            tt("dve", ptmp[:], pos[:], Mf[:], ALU.mult, (("pos",), ("Mf",), ("posm",)), (("ptmp",),))
            S.op("dve", lambda e: e.tensor_reduce(out=pBf[:], in_=ptmp[:], axis=AX.X, op=ALU.max), (("ptmp",),), (("pBf",),))
            tcopy("dve", pAi[:], pAf[:], (("pAf",),), (("pAi",),))
            tcopy("dve", pBi[:], pBf[:], (("pBf",),), (("pBi",),))
            tt("dve", ptmp[:], posm[:], bc3(pAf[:], 32), ALU.is_equal, (("posm",), ("pAf",), ("pBf",)), (("ptmp",),))
            tt("dve", ptmp[:], ptmp[:], comb[:], ALU.mult, (("ptmp",), C), (("ptmp",),))
            S.op("dve", lambda e: e.tensor_reduce(out=wA[:], in_=ptmp[:], axis=AX.X, op=ALU.add), (("ptmp",),), (("wA",),))
            S.op("dve", lambda e: e.tensor_reduce(out=wB[:], in_=comb[:], axis=AX.X, op=ALU.add), (C,), (("wB",),))
            tt("dve", wB[:], wB[:], wA[:], ALU.subtract, (("wB",), ("wA",)), (("wB",),))
            tt("dve", ecmp[:], sot[:].unsqueeze(1).to_broadcast([128, NSLOT, 32]),
               siota[:].unsqueeze(2).to_broadcast([128, NSLOT, 32]), ALU.is_le, (("sot",), ("siota",)), (("ecmp",),))
            S.op("dve", lambda e: e.tensor_reduce(out=eidf[:], in_=ecmp[:], axis=AX.X, op=ALU.add), (("ecmp",),), (("eidf",),))
            tscal("dve", eidf[:], eidf[:], -1.0, None, ALU.add, None, (("eidf",),), (("eidf",),))
            tcopy("dve", eidi[:], eidf[:], (("eidf",),), (("eidi",),))
            idxf = S.alloc("idxf", [128, NSLOT, 2, 2], F32)
            idxw = S.alloc("idxw", [128, NSLOT, 2, 2], I32)
            pcol = S.alloc("pcol", [128, 1], F32)
            tscal("dve", pcol[:], iot[:, 0:1], -2.0, None, ALU.mult, None, (("iot",),), (("pcol",),))
            for hf in range(2):
                for pc in range(2):
                    tscal("dve", idxf[:, :, hf, pc], eidf[:], 512.0, 256.0 * hf + pc, ALU.mult, ALU.add,
                          (("eidf",),), (("idxf",),))
            tscal("dve", idxf[:], idxf[:], pcol[:], None, ALU.add, None, (("idxf",), ("pcol",)), (("idxf",),))
            tcopy("dve", idxw[:], idxf[:], (("idxf",),), (("idxw",),))
            S.free("idxf"); S.free("pcol")
            dbg("pAi", pAi[:], [128, NB], I32, ("pAi",))
            dbg("pBi", pBi[:], [128, NB], I32, ("pBi",))
            dbg("eidi", eidi[:], [128, NSLOT], I32, ("eidi",))
            dbg("wA", wA[:], [128, NB], F32, ("wA",))
            for nm in ("Mf", "Mb", "Ub", "rank", "cntt", "nst", "cs0", "cs1", "sot", "pos", "posm", "ptmp",
                       "pAf", "pBf", "siota", "ecmp", "eidf"):
                S.free(nm)

            xs_scr = nc.dram_tensor("xs_scr", [NROW, D], BF16, kind="Internal").ap()
            ys_scr = nc.dram_tensor("ys_scr", [NROW, D], F32, kind="Internal").ap()
            XSK, YSK = ("xs_scr",), ("ys_scr",)
            Xs = [S.alloc(f"Xs{i}", [128, D], BF16) for i in range(2)]
            S.op("dve", lambda e: e.memset(Xs[0][:], 0.0), (), (("Xs0",),))
            XALL = tuple(("xs_scr", q_) for q_ in range(NSLOT))
            for s_ in range(NSLOT):
                dma("sp", f"xz{s_ % 4}", xs_scr[s_ * 128:(s_ + 1) * 128, :], Xs[0][:], (("Xs0",),),
                    (("xs_scr", s_), ("xs_scr", "z", s_ % 4)))
            for b in range(NB):
                for (pi_, pk_) in ((pAi, ("pAi",)), (pBi, ("pBi",))):
                    S.dma("pool", "sc",
                          lambda e, b=b, pi_=pi_: e.indirect_dma_start(
                              out=xs_scr[:, :], out_offset=bass.IndirectOffsetOnAxis(ap=pi_[:, b:b + 1], axis=0),
                              in_=h2tok[:, b, :], in_offset=None),
                          (("h2tok", b), pk_), XALL)
            S.free("h2tok")

            NSL = 3
            wg = [S.alloc(f"wg{i}", [128, KC, 256], BF16) for i in range(NSL)]
            wu = [S.alloc(f"wu{i}", [128, KC, 256], BF16) for i in range(NSL)]
            wd = [S.alloc(f"wd{i}", [128, 2, D], BF16) for i in range(NSL)]
            XsT = S.alloc("XsT", [128, KC, 128], BF16)
            sgt = S.alloc("sgt", [128, 256], BF16)
            hidh = S.alloc("hidh", [128, 256], BF16)
            hidT = S.alloc("hidT", [128, 2, 128], BF16)
            Ys = S.alloc("Ys", [128, D], F32)

            def load_unit(u):
                s_, hf = u // 2, u % 2
                sl = u % NSL
                for (nm, dst, src) in (("wg", wg[sl], w_gate), ("wu", wu[sl], w_up), ("wd", wd[sl], w_down)):
                    for pc in range(2):
                        if nm == "wd":
                            d2 = dst[:, pc, :]
                        else:
                            d2 = dst[:, pc * 8:(pc + 1) * 8, :].rearrange("p a b -> p (a b)")
                        S.dma("pool", f"{nm}{sl}",
                              lambda e, d2=d2, src=src, s_=s_, hf=hf, pc=pc: e.indirect_dma_start(
                                  out=d2, out_offset=None, in_=src[:, :],
                                  in_offset=bass.IndirectOffsetOnAxis(ap=idxw[:, s_, hf, pc:pc + 1], axis=0)),
                              (("idxw",),), ((f"{nm}{sl}", pc), (f"{nm}{sl}", "ser")))

            load_unit(0)
            for s_ in range(NSLOT):
                xsl = s_ % 2
                dma("sp", f"Xs{xsl}", Xs[xsl][:], xs_scr[s_ * 128:(s_ + 1) * 128, :], (("xs_scr", s_),), ((f"Xs{xsl}",),))
                for half, (pt, pk) in enumerate(((ptA, ("ptA",)), (ptB, ("ptB",)))):
                    for q in range(8):
                        kc = half * 8 + q
                        transpose(pt[:, q * 128:(q + 1) * 128], pk, Xs[xsl][:, kc * 128:(kc + 1) * 128], (f"Xs{xsl}",),
                                  mark=(q == 7))
                    src = pt[:, :].rearrange("p (c q) -> p c q", q=128)
                    if half == 0:
                        tcopy("dve", XsT[:, 0:8, :], src, (pk,), (("XsT", 0),))
                    else:
                        act(XsT[:, 8:16, :], src, AF.Copy, (pk,), (("XsT", 1),))
                for hf in range(2):
                    u = 2 * s_ + hf
                    sl = u % NSL
                    if u + 1 < 2 * NSLOT:
                        load_unit(u + 1)
                    for c in range(2):
                        tt("pool", wd[sl][:, c, :], wd[sl][:, c, :], g2_b[:], ALU.mult, ((f"wd{sl}", c), ("g2_b",)),
                           ((f"wd{sl}", c),))
                    mm_group(pq0[:, 0:256], ("pq0",), [(XsT[:, kc, :], wg[sl][:, kc, :],
                                                        (("XsT", 0), ("XsT", 1), (f"wg{sl}", kc // 8))) for kc in range(KC)])
                    mm_group(pq1[:, 0:256], ("pq1",), [(XsT[:, kc, :], wu[sl][:, kc, :],
                                                        (("XsT", 0), ("XsT", 1), (f"wu{sl}", kc // 8))) for kc in range(KC)])
                    act(sgt[:], pq0[:, 0:256], AF.Silu, (("pq0",),), (("sgt",),))
                    tt("dve", hidh[:], sgt[:], pq1[:, 0:256], ALU.mult, (("sgt",), ("pq1",)), (("hidh",),))
                    for kf in range(2):
                        transpose(ptA[:, kf * 128:(kf + 1) * 128], ("ptA",), hidh[:, kf * 128:(kf + 1) * 128], ("hidh",),
                                  mark=(kf == 1))
                    tcopy("dve", hidT[:], ptA[:, 0:256].rearrange("p (c q) -> p c q", q=128), (("ptA",),), (("hidT",),))
                    for cg in range(4):
                        for kf in range(2):
                            first = (hf == 0 and kf == 0)
                            last = (hf == 1 and kf == 1)
                            mm(pbig[:, cg * 512:(cg + 1) * 512], hidT[:, kf, :], wd[sl][:, kf, cg * 512:(cg + 1) * 512],
                               first, last, (("hidT",), (f"wd{sl}", kf)),
                               (("pbig", cg),) if (first or last) else (), mark=last)
                for cg in range(4):
                    if cg % 2 == 0:
                        tcopy("dve", Ys[:, cg * 512:(cg + 1) * 512], pbig[:, cg * 512:(cg + 1) * 512], (("pbig", cg),),
                              (("Ys", 0),))
                    else:
                        act(Ys[:, cg * 512:(cg + 1) * 512], pbig[:, cg * 512:(cg + 1) * 512], AF.Copy, (("pbig", cg),),
                            (("Ys", 1),))
                dma("sp", "ysst", ys_scr[s_ * 128:(s_ + 1) * 128, :], Ys[:], (("Ys", 0), ("Ys", 1)), (YSK,))
            for i in range(NSL):
                S.free(f"wg{i}"); S.free(f"wu{i}"); S.free(f"wd{i}")
            for nm in ("Xs0", "Xs1", "XsT", "sgt", "hidh", "hidT", "Ys"):
                S.free(nm)
            yg = [S.alloc(f"yg{i}", [128, D], F32) for i in range(2)]
            gi = 0
            for b in range(NB):
                for (pi_, pk_, wt_, wk_) in ((pAi, ("pAi",), wA, ("wA",)), (pBi, ("pBi",), wB, ("wB",))):
                    g_ = gi % 2
                    gi += 1
                    S.dma("pool", f"yg{g_}",
                          lambda e, b=b, pi_=pi_, g_=g_: e.indirect_dma_start(
                              out=yg[g_][:], out_offset=None, in_=ys_scr[:, :],
                              in_offset=bass.IndirectOffsetOnAxis(ap=pi_[:, b:b + 1], axis=0)),
                          (YSK, pk_), ((f"yg{g_}",),))
                    stt(x1[:, b, :], yg[g_][:], wt_[:, b:b + 1], x1[:, b, :], ALU.mult, ALU.add,
                        ((f"yg{g_}",), wk_, ("x1", b)), (("x1", b),))
            for nm in ("yg0", "yg1", "pAi", "pBi", "wA", "wB", "eidi", "idxw"):
                S.free(nm)
        else:
            NSL = 3
            wg = [S.alloc(f"wg{i}", [128, KC, 256], BF16) for i in range(NSL)]
            wu = [S.alloc(f"wu{i}", [128, KC, 256], BF16) for i in range(NSL)]
            wd = [S.alloc(f"wd{i}", [128, 2, D], BF16) for i in range(NSL)]
            hid = [S.alloc(f"hid{i}", [128, 2, TOK], BF16) for i in range(2)]
            sgt = [S.alloc(f"sgt{i}", [128, 512], BF16) for i in range(2)]
            sgc = 0
            pqs = ((pq0, ("pq0",)), (pq1, ("pq1",)))
            pqc = 0
            def load_unit(u):
                e_, hf = u // 2, u % 2
                sl = u % NSL
                dma("pool", f"wg{sl}", wg[sl][:], w_gate[e_].rearrange("(kc p) n -> p kc n", p=128)[:, :, hf * 256:(hf + 1) * 256],
                    (), ((f"wg{sl}",),))
                dma("pool", f"wu{sl}", wu[sl][:], w_up[e_].rearrange("(kc p) n -> p kc n", p=128)[:, :, hf * 256:(hf + 1) * 256],
                    (), ((f"wu{sl}",),))
                dma("pool", f"wd{sl}", wd[sl][:], w_down[e_][hf * 256:(hf + 1) * 256, :].rearrange("(c p) n -> p c n", p=128),
                    (), ((f"wd{sl}",),))

            load_unit(0)
            for u in range(2 * NEXP):
                e_, hf = u // 2, u % 2
                sl = u % NSL
                hi = u % 2
                if u + 1 < 2 * NEXP:
                    load_unit(u + 1)
                for c in range(2):
                    tt("pool", wd[sl][:, c, :], wd[sl][:, c, :], g2_b[:], ALU.mult, ((f"wd{sl}",), ("g2_b",)), ((f"wd{sl}",),))
                for n in range(2):
                    for ffc in range(2):
                        bg, bgk = nbank()
                        mm_group(bg, bgk, [(wg[sl][:, kc, ffc * 128:(ffc + 1) * 128], h2T[:, kc, n * 512:(n + 1) * 512],
                                            ((f"wg{sl}",),) + k2(("h2T",))) for kc in range(KC)])
                        bu, buk = nbank()
                        mm_group(bu, buk, [(wu[sl][:, kc, ffc * 128:(ffc + 1) * 128], h2T[:, kc, n * 512:(n + 1) * 512],
                                            ((f"wu{sl}",),) + k2(("h2T",))) for kc in range(KC)])
                        si = sgc % 2
                        sgc += 1
                        act(sgt[si][:], bg, AF.Silu, (bgk,), ((f"sgt{si}",),))
                        tt("dve", hid[hi][:, ffc, n * 512:(n + 1) * 512], sgt[si][:], bu, ALU.mult,
                           ((f"sgt{si}",), buk), ((f"hid{hi}", n),))
                    for bb in range(4):
                        blk = n * 4 + bb
                        for cg in range(4):
                            pq, pqk = pqs[pqc % 2]
                            pqc += 1
                            mm_group(pq[:, :], pqk, [(hid[hi][:, c, blk * 128:(blk + 1) * 128], wd[sl][:, c, cg * 512:(cg + 1) * 512],
                                                      ((f"hid{hi}", n), (f"wd{sl}",))) for c in range(2)])
                            stt(x1[:, blk, cg * 512:(cg + 1) * 512], pq[:, :], comb[:, blk, e_:e_ + 1],
                                x1[:, blk, cg * 512:(cg + 1) * 512], ALU.mult, ALU.add,
                                (pqk, ("comb",), ("x1", blk)), (("x1", blk),))
            for i in range(NSL):
                S.free(f"wg{i}"); S.free(f"wu{i}"); S.free(f"wd{i}")
            S.free("hid0"); S.free("hid1"); S.free("sgt0"); S.free("sgt1"); S.free("h2T")

        lng2 = S.alloc("lng2", [128, D], F32)
        lnb2 = S.alloc("lnb2", [128, D], F32)
        dma("sp", "c_ln2g", lng2[:], ln2g_d[0:1, :].to_broadcast([128, D]), (), (("lng2",),))
        dma("sp", "c_ln2b", lnb2[:], ln2b_d[0:1, :].to_broadcast([128, D]), (), (("lnb2",),))
        for blk in range(NB):
            rstd, nmr, KR, KN = ln_stats(x1[:, blk, :], ("x1", blk), D)
            act(x1[:, blk, :], x1[:, blk, :], AF.Identity, (("x1", blk), KR, KN), (("x1", blk),),
                bias=nmr[:], scale=rstd[:])
            tt("pool", x1[:, blk, :], x1[:, blk, :], lng2[:], ALU.mult, (("x1", blk), ("lng2",)), (("x1", blk),))
            tt("dve", x1[:, blk, :], x1[:, blk, :], lnb2[:], ALU.add, (("x1", blk), ("lnb2",)), (("x1", blk),))
            dma("sp", "out", out_d[blk * 128:(blk + 1) * 128, :], x1[:, blk, :], (("x1", blk),), (("outd",),))

    try:
        body()
    except _Stop:
        pass
    S.wait_all("sp", list(S.dch.values()))

    import contextlib
    with contextlib.ExitStack() as es:
        for ch in list(S.chan.values()) + list(S.dch.values()):
            ch.sem = es.enter_context(nc.semaphore("s_" + ch.name))
        block = es.enter_context(nc.Block())

        def emit(name, e):
            for waits, fn, ch in S.ops[name]:
                for (c, v) in waits:
                    e.wait_ge(c.sem, v)
                if fn is None:
                    continue
                ins = fn(e)
                if ch is not None:
                    ins.then_inc(ch.sem, ch.inc)

        block.tensor(lambda e: emit("pe", e))
        block.scalar(lambda e: emit("act", e))
        block.vector(lambda e: emit("dve", e))
        block.gpsimd(lambda e: emit("pool", e))
        block.sync(lambda e: emit("sp", e))
    return nc, dbg_out, S


OWN = {0: [0, 3, 4, 7, 8, 11, 12, 15], 1: [1, 2, 5, 6, 9, 10, 13, 14]}


def make_in_maps(x, c, positions, w_ada, b_ada, w_in, q_norm_g, w_uq, kv_norm_g, w_ukv,
                 sgu_norm_g, sgu_norm_b, w_spatial, b_spatial, w_o, ln1_g, ln1_b,
                 w_router_group, b_router_group, w_router_expert, b_router_expert,
                 w_gate, w_up, w_down, ln2_g, ln2_b):
    f = lambda a: np.ascontiguousarray(np.asarray(a), dtype=np.float32)

    def relayout_gu(w):
        w = np.asarray(w, dtype=np.float32)
        if not SPARSE:
            return np.ascontiguousarray(w)
        E = w.shape[0]
        return np.ascontiguousarray(w.reshape(E, KC, 128, 2, 256).transpose(0, 3, 2, 1, 4)).reshape(E * 512, 2048)

    def relayout_d(w):
        w = np.asarray(w, dtype=np.float32)
        if not SPARSE:
            return np.ascontiguousarray(w)
        E = w.shape[0]
        return np.ascontiguousarray(w.reshape(E, 2, 2, 128, D).transpose(0, 1, 3, 2, 4)).reshape(E * 512, D)

    x = f(x); c = f(c)
    positions = np.asarray(positions).astype(np.int32)
    inv_freq = (1.0 / (10000.0 ** (np.arange(0, 64, 2, dtype=np.float32) / 64.0))).astype(np.float32)
    ropec = np.zeros((64, 2), np.float32)
    ropec[:, 0] = np.concatenate([inv_freq, inv_freq])
    ropec[:, 1] = np.concatenate([-np.ones(32, np.float32), np.ones(32, np.float32)])
    shared = {
        "ropec": ropec,
        "w_ada": f(w_ada[0]), "b_ada": f(b_ada[0]).reshape(1, -1), "w_in": f(w_in[0]),
        "qg": f(np.asarray(q_norm_g[0]).reshape(6, 128).T), "w_uq": f(w_uq[0]),
        "kvg": f(np.asarray(kv_norm_g[0]).reshape(4, 128).T), "w_ukv": f(w_ukv[0]),
        "sgug": f(sgu_norm_g[0]).reshape(1, -1), "sgub": f(sgu_norm_b[0]).reshape(1, -1),
        "wspT": f(np.asarray(w_spatial[0]).transpose(2, 0, 1)),
        "bsp": f(b_spatial[0]).reshape(1, -1),
        "w_o": f(w_o[0]), "ln1g": f(ln1_g[0]).reshape(1, -1), "ln1b": f(ln1_b[0]).reshape(1, -1),
        "wr": f(np.concatenate([np.asarray(w_router_group[0]), np.asarray(w_router_expert[0])], axis=1)),
        "br": f(np.concatenate([np.asarray(b_router_group[0]), np.asarray(b_router_expert[0])])).reshape(1, -1),
        "w_gate": relayout_gu(w_gate[0]), "w_up": relayout_gu(w_up[0]), "w_down": relayout_d(w_down[0]),
        "ln2g": f(ln2_g[0]).reshape(1, -1), "ln2b": f(ln2_b[0]).reshape(1, -1),
    }
    in_maps = []
    for core in range(8):
        b, p = core // 2, core % 2
        own, oth = OWN[p], OWN[1 - p]
        order = own + oth
        rows = np.concatenate([np.arange(k * 128, (k + 1) * 128) for k in order])
        m = dict(shared)
        m["x_loc"] = np.ascontiguousarray(x[b][rows])
        m["cT"] = np.ascontiguousarray(c[b].reshape(16, 128).T)
        m["pos"] = np.ascontiguousarray(positions[b][rows].reshape(1, -1))
        m["qidx"] = np.ascontiguousarray(
            (np.array(own, np.float32)[None, :] * 128 + np.arange(128, dtype=np.float32)[:, None]))
        m["oblk"] = np.ascontiguousarray(np.broadcast_to(np.array(oth, np.float32)[None, :] * 128, (128, 8)))
        in_maps.append(m)
    return in_maps


def assemble(results):
    out = np.zeros((4, 2048, 2048), np.float32)
    for core in range(8):
        b, p = core // 2, core % 2
        y = np.asarray(results[core]["out"])
        for j, k in enumerate(OWN[p]):
            out[b, k * 128:(k + 1) * 128] = y[j * 128:(j + 1) * 128]
    return out


def kernel(**inputs):
    nc, _, _ = build_nc()
    in_maps = make_in_maps(**inputs)
    res = run_bass_kernel_spmd(nc, in_maps, core_ids=list(range(8)))
    return assemble(res.results)
```

```python
import numpy as np
import concourse.bass as bass
import concourse.mybir as mybir
from concourse.bass_utils import run_bass_kernel_spmd

F32 = mybir.dt.float32
BF16 = mybir.dt.bfloat16
I32 = mybir.dt.int32
AF = mybir.ActivationFunctionType
ALU = mybir.AluOpType
AX = mybir.AxisListType

D = 2048
KC = 16
NB = 8
TOK = 1024
TL = 2048
H = 8
NEXP = 32
ALPHA = 2.0 ** 0.25
EPS = 1e-6
SM_SCALE = 192.0 ** -0.5
NEG = -30000.0
SBUF_BASE = 16640
PERSIST = 28672
SBUF_LIMIT = 229312

DEBUG = {}
SPARSE = True
PSUM_NAMES = ("pbig", "ptA", "ptB", "pq0", "pq1")


def dsize(dt):
    return {F32: 4, BF16: 2, I32: 4}[dt]


class Chan:
    def __init__(self, name, inc):
        self.name = name
        self.inc = inc
        self.count = 0
        self.sem = None


class Sched:
    ENG = ("pe", "act", "dve", "pool", "sp")

    def __init__(self, nc):
        self.nc = nc
        self.chan = {e: Chan(e, 1) for e in self.ENG}
        self.dch = {}
        self.ops = {e: [] for e in self.ENG}
        self.seen = {e: {} for e in self.ENG}
        self.last_w = {}
        self.readers = {}
        self.pending = {}
        self.allocs = []
        self.tomb = []
        self.tiles = {}
        self.peak = 0

    def alloc(self, name, shape, dt, base=SBUF_BASE + PERSIST, top=False):
        size = int(np.prod(shape[1:])) * dsize(dt)
        size = (size + 63) // 64 * 64
        self.allocs.sort()
        if top:
            off = SBUF_LIMIT // 64 * 64 - size
            for (o, s, _) in reversed(self.allocs):
                if o >= off + size:
                    continue
                if o + s <= off:
                    break
                off = o - size
            assert off >= base, f"SBUF overflow (top) allocating {name}"
        else:
            off = base
            for (o, s, _) in self.allocs:
                if o + s <= off:
                    continue
                if off + size <= o:
                    break
                off = o + s
        assert off + size <= SBUF_LIMIT, f"SBUF overflow allocating {name} size {size} at {off}"
        self.allocs.append((off, size, name))
        self.peak = max(self.peak, off + size)
        pend = {}
        keep = []
        for (o, s, deps) in self.tomb:
            if o < off + size and off < o + s:
                for ch, v in deps.items():
                    pend[ch] = max(pend.get(ch, 0), v)
            keep.append((o, s, deps))
        self.tomb = keep
        if pend:
            self.pending[name] = pend
        t = self.nc.alloc_sbuf_tensor_at(name, list(shape), dt, offset=off)
        self.tiles[name] = t
        return t

    def free(self, name):
        ent = [a for a in self.allocs if a[2] == name]
        assert len(ent) == 1, name
        self.allocs.remove(ent[0])
        deps = dict(self.pending.get(name, {}))
        for k, (ch, v) in self.last_w.items():
            if k[0] == name:
                deps[ch] = max(deps.get(ch, 0), v)
        for k, rd in self.readers.items():
            if k[0] == name:
                for ch, v in rd.items():
                    deps[ch] = max(deps.get(ch, 0), v)
        self.tomb.append((ent[0][0], ent[0][1], deps))

    def dchan(self, name):
        if name not in self.dch:
            self.dch[name] = Chan("d_" + name, 16)
        return self.dch[name]

    def _deps(self, eng, reads, writes):
        own = self.chan[eng]
        deps = {}

        def add(ch, v, raw):
            if ch is own and eng in ("pe", "sp"):
                return
            if deps.get(ch, 0) < v:
                deps[ch] = v

        for r in reads:
            lw = self.last_w.get(r)
            if lw:
                add(lw[0], lw[1], True)
            for ch, v in self.pending.get(r[0], {}).items():
                add(ch, v, True)
            if r[0] in PSUM_NAMES:
                for ch, v in self.readers.get(r, {}).items():
                    add(ch, v, False)
        for w in writes:
            lw = self.last_w.get(w)
            if lw:
                add(lw[0], lw[1], False)
            for ch, v in self.readers.get(w, {}).items():
                add(ch, v, False)
            for ch, v in self.pending.get(w[0], {}).items():
                add(ch, v, True)
        waits = []
        for ch, v in deps.items():
            if self.seen[eng].get(ch, 0) < v:
                waits.append((ch, v))
                self.seen[eng][ch] = v
        return waits

    def _record(self, ch, tick, reads, writes):
        for r in reads:
            rd = self.readers.setdefault(r, {})
            if rd.get(ch, 0) < tick:
                rd[ch] = tick
        for w in writes:
            self.last_w[w] = (ch, tick)
            self.readers[w] = {}

    def op(self, eng, fn, reads=(), writes=(), mark=True):
        waits = self._deps(eng, reads, writes)
        ch = self.chan[eng]
        if mark:
            ch.count += 1
            tick = ch.count
        else:
            tick = ch.count + 1
        self._record(ch, tick, reads, writes)
        self.ops[eng].append((waits, fn, ch if mark else None))

    def dma(self, queue, chname, fn, reads=(), writes=()):
        waits = self._deps(queue, reads, writes)
        ch = self.dchan(chname)
        ch.count += 16
        self._record(ch, ch.count, reads, writes)
        self.ops[queue].append((waits, fn, ch))

    def wait_all(self, eng, chans):
        waits = []
        for ch in chans:
            if ch.count > 0 and self.seen[eng].get(ch, 0) < ch.count:
                waits.append((ch, ch.count))
                self.seen[eng][ch] = ch.count
        self.ops[eng].append((waits, None, None))


class _Stop(Exception):
    pass


def build_nc(debug=(), stop=None, nexp_decl=NEXP):
    nc = bass.Bass("TRN2", target_bir_lowering=False)
    S = Sched(nc)
    BREG = {}

    def din(name, shape, dt=F32):
        return nc.dram_tensor(name, list(shape), dt, kind="ExternalInput").ap()

    x_loc = din("x_loc", [TL, D])
    cT_d = din("cT", [128, KC])
    pos_d = din("pos", [1, TL], I32)
    qidx_d = din("qidx", [128, NB])
    oblk_d = din("oblk", [128, NB])
    rope_d = din("ropec", [64, 2])
    w_ada = din("w_ada", [D, 6 * D])
    b_ada = din("b_ada", [1, 6 * D])
    w_in = din("w_in", [D, 3392])
    qg_d = din("qg", [128, 6])
    w_uq = din("w_uq", [768, 1536])
    kvg_d = din("kvg", [128, 4])
    w_ukv = din("w_ukv", [512, 2048])
    sgug_d = din("sgug", [1, 1024])
    sgub_d = din("sgub", [1, 1024])
    wspT_d = din("wspT", [128, 8, 128])
    bsp_d = din("bsp", [1, 1024])
    w_o = din("w_o", [D, D])
    ln1g_d = din("ln1g", [1, D])
    ln1b_d = din("ln1b", [1, D])
    wr_d = din("wr", [D, 36])
    br_d = din("br", [1, 36])
    if SPARSE:
        w_gate = din("w_gate", [nexp_decl * 512, 2048])
        w_up = din("w_up", [nexp_decl * 512, 2048])
        w_down = din("w_down", [nexp_decl * 512, D])
    else:
        w_gate = din("w_gate", [nexp_decl, D, 512])
        w_up = din("w_up", [nexp_decl, D, 512])
        w_down = din("w_down", [nexp_decl, 512, D])
    ln2g_d = din("ln2g", [1, D])
    ln2b_d = din("ln2b", [1, D])
    out_d = nc.dram_tensor("out", [TOK, D], F32, kind="ExternalOutput").ap()
    dbg_out = {}

    poff = [SBUF_BASE]

    def palloc(name, shape, dt):
        size = int(np.prod(shape[1:])) * dsize(dt)
        size = (size + 63) // 64 * 64
        t = nc.alloc_sbuf_tensor_at(name, list(shape), dt, offset=poff[0])
        poff[0] += size
        assert poff[0] <= SBUF_BASE + PERSIST
        return t

    ident = palloc("ident", [128, 128], BF16)
    ones_bf = palloc("ones_bf", [128, 128], BF16)
    onesf = palloc("onesf", [1, 128], F32)
    iot = palloc("iot", [128, 128], F32)
    tri = palloc("tri", [128, 128], F32)
    maskT = palloc("maskT", [128, 128], F32)
    cT = palloc("cT_sb", [128, KC], F32)
    cT_bf = palloc("cT_bf", [128, KC], BF16)
    modT = palloc("modT", [128, 64], F32)
    g1_b = palloc("g1_b", [128, D], F32)
    g2_b = palloc("g2_b", [128, D], F32)
    qidx = palloc("qidx_sb", [128, NB], F32)
    oblk = palloc("oblk_sb", [128, NB], F32)
    maskb = palloc("maskb", [128, NB, NB], F32)
    ropec = palloc("ropec_sb", [64, 2], F32)
    qg = palloc("qg_sb", [128, 6], F32)
    kvg = palloc("kvg_sb", [128, 4], F32)
    brb = palloc("brb", [128, 36], F32)
    logits = palloc("logits", [128, NB, 36], F32)
    comb = palloc("comb", [128, NB, 32], F32)
    st6s = [palloc(f"st6_{i}", [128, 4, 6], F32) for i in range(2)]
    mvs = [palloc(f"mv_{i}", [128, 2], F32) for i in range(2)]
    sds = [palloc(f"sd_{i}", [128, 1], F32) for i in range(2)]
    rstds = [palloc(f"rstd_{i}", [128, 1], F32) for i in range(2)]
    nmrs = [palloc(f"nmr_{i}", [128, 1], F32) for i in range(2)]
    lnc = [0]
    rs1 = palloc("rs1", [128, 1], F32)
    rs2 = palloc("rs2", [128, 1], F32)
    rs3 = palloc("rs3", [128, 1], F32)
    r_mg = palloc("r_mg", [128, NB], F32)
    r_sg = palloc("r_sg", [128, NB], F32)
    r_pg = palloc("r_pg", [128, NB], F32)
    r_eg = palloc("r_eg", [128, NB, 4], F32)
    r_oh = palloc("r_oh", [128, NB, 4], F32)
    r_t4 = palloc("r_t4", [128, NB, 4, 8], F32)
    r_sel = palloc("r_sel", [128, NB, 8], F32)
    r_l1 = palloc("r_l1", [128, NB], F32)
    r_l2 = palloc("r_l2", [128, NB], F32)
    r_oh1 = palloc("r_oh1", [128, NB, 8], F32)
    r_oh2 = palloc("r_oh2", [128, NB, 8], F32)
    r_msk = palloc("r_msk", [128, NB, 8], F32)
    r_w1 = palloc("r_w1", [128, NB], F32)
    r_w2 = palloc("r_w2", [128, NB], F32)
    r_ce = palloc("r_ce", [128, NB, 8], F32)

    pbig = nc.alloc_psum_tensor("pbig", [128, 2048], F32)
    ptA = nc.alloc_psum_tensor("ptA", [128, 1024], BF16)
    ptB = nc.alloc_psum_tensor("ptB", [128, 1024], BF16)
    pq0 = nc.alloc_psum_tensor("pq0", [128, 512], F32)
    pq1 = nc.alloc_psum_tensor("pq1", [128, 512], F32)

    def bank(i):
        return pbig[:, i * 512:(i + 1) * 512], ("pbig", i)

    def act(out, in_, func, reads, writes, bias=0.0, scale=1.0, accum=None):
        if accum is None:
            S.op("act", lambda e: e.activation(out=out, in_=in_, func=func, bias=bias, scale=scale),
                 reads, writes)
        else:
            S.op("act", lambda e: e.activation(out=out, in_=in_, func=func, bias=bias, scale=scale,
                                               accum_out=accum), reads, writes)

    def tscal(eng, out, in0, s1, s2, op0, op1, reads, writes):
        if s2 is None:
            S.op(eng, lambda e: e.tensor_scalar(out=out, in0=in0, scalar1=s1, scalar2=None, op0=op0),
                 reads, writes)
        else:
            S.op(eng, lambda e: e.tensor_scalar(out=out, in0=in0, scalar1=s1, scalar2=s2, op0=op0, op1=op1),
                 reads, writes)

    def tt(eng, out, in0, in1, op, reads, writes):
        S.op(eng, lambda e: e.tensor_tensor(out=out, in0=in0, in1=in1, op=op), reads, writes)

    def stt(out, in0, scalar, in1, op0, op1, reads, writes):
        S.op("dve", lambda e: e.scalar_tensor_tensor(out=out, in0=in0, scalar=scalar, in1=in1, op0=op0, op1=op1),
             reads, writes)

    def tcopy(eng, out, in_, reads, writes):
        S.op(eng, lambda e: e.tensor_copy(out=out, in_=in_), reads, writes)

    def mm(out, lhsT, rhs, start, stop, reads, writes=(), mark=False):
        S.op("pe", lambda e: e.matmul(out, lhsT=lhsT, rhs=rhs, start=start, stop=stop), reads, writes, mark=mark)

    def mm_group(out, okey, items):
        n = len(items)
        for i, (l, r, rk) in enumerate(items):
            first, last = (i == 0), (i == n - 1)
            mm(out, l, r, first, last, rk, writes=(okey,) if (first or last) else (), mark=last)

    def transpose(out, okey, in_, ikey, mark):
        S.op("pe", lambda e: e.transpose(out=out, in_=in_, identity=ident[:]), (ikey, ("ident",)),
             (okey,), mark=mark)

    def dma(queue, chname, out, in_, reads, writes):
        S.dma(queue, chname, lambda e: e.dma_start(out=out, in_=in_), reads, writes)

    def dbg(name, tile_ap, shape, dt, key):
        if name in debug:
            d = nc.dram_tensor("dbg_" + name, list(shape), dt, kind="ExternalOutput").ap()
            dbg_out[name] = d
            dma("sp", "dbg_" + name, d, tile_ap, (key,), ())

    def hk(blks):
        return tuple(("hT", b, par) for b in blks for par in (0, 1))

    def k2(base):
        return (base + (0,), base + (1,))

    def ckpt(name):
        if stop == name:
            raise _Stop()

    def evac(i, out, in_, reads, wkey):
        if i % 2 == 0:
            tcopy("dve", out, in_, reads, (wkey + (0,),))
        else:
            act(out, in_, AF.Copy, reads, (wkey + (1,),))

    def ln_stats(src, skey, n):
        k = lnc[0] % 2
        lnc[0] += 1
        st6, mv, sd, rstd, nmr = st6s[k], mvs[k], sds[k], rstds[k], nmrs[k]
        K6, KM, KS, KR, KN = (f"st6_{k}",), (f"mv_{k}",), (f"sd_{k}",), (f"rstd_{k}",), (f"nmr_{k}",)
        nch = n // 512
        for i in range(nch):
            S.op("dve", lambda e, i=i: e.bn_stats(out=st6[:, i, :], in_=src[:, i * 512:(i + 1) * 512]),
                 (skey,), (K6,))
        S.op("dve", lambda e: e.bn_aggr(out=mv[:], in_=st6[:, 0:nch, :]), (K6,), (KM,))
        act(sd[:], mv[:, 1:2], AF.Sqrt, (KM,), (KS,), bias=EPS, scale=1.0)
        S.op("dve", lambda e: e.reciprocal(out=rstd[:], in_=sd[:]), (KS,), (KR,))
        tscal("dve", nmr[:], mv[:, 0:1], rstd[:], -1.0, ALU.mult, ALU.mult, (KM, KR), (KN,))
        return rstd, nmr, KR, KN

    def body():
        S.op("pool", lambda e: e.iota(iot[:], pattern=[[1, 128]], base=0, channel_multiplier=-1,
                                      allow_small_or_imprecise_dtypes=True), (), (("iot",),))
        tscal("dve", ident[:], iot[:], 0.0, None, ALU.is_equal, None, (("iot",),), (("ident",),))
        tscal("dve", tri[:], iot[:], 0.0, NEG, ALU.is_gt, ALU.mult, (("iot",),), (("tri",),))
        tscal("dve", maskT[:], iot[:], 0.0, None, ALU.is_ge, None, (("iot",),), (("maskT",),))
        S.op("dve", lambda e: e.memset(ones_bf[:], 1.0), (), (("ones_bf",),))
        S.op("dve", lambda e: e.memset(onesf[:], 1.0), (), (("onesf",),))
        for (t, d_, nm) in ((cT, cT_d, "cT"), (qidx, qidx_d, "qidx"), (oblk, oblk_d, "oblk"), (ropec, rope_d, "ropec"),
                            (qg, qg_d, "qg"), (kvg, kvg_d, "kvg")):
            dma("sp", "c_" + nm, t[:], d_, (), ((nm,),))
        dma("sp", "c_brb", brb[:], br_d[0:1, :].to_broadcast([128, 36]), (), (("brb",),))
        tcopy("dve", cT_bf[:], cT[:], (("cT",),), (("cT_bf",),))
        for j in range(NB):
            tscal("dve", maskb[:, j, :], oblk[:], qidx[:, j:j + 1], NEG, ALU.is_gt, ALU.mult,
                  (("oblk",), ("qidx",)), (("maskb",),))

        cosT = S.alloc("cosT", [64, TL], BF16)
        sinT = S.alloc("sinT", [64, TL], BF16)
        posi = S.alloc("posi", [64, TL], I32)
        ang = S.alloc("ang", [64, TL], F32)
        tmpa = S.alloc("tmpa", [64, TL], F32)
        tmpi = S.alloc("tmpi", [64, TL], I32)
        dma("sp", "c_pos", posi[:], pos_d[0:1, :].to_broadcast([64, TL]), (), (("posi",),))
        tcopy("dve", ang[:], posi[:], (("posi",),), (("ang",),))
        tscal("dve", ang[:], ang[:], ropec[:, 0:1], None, ALU.mult, None, (("ang",), ("ropec",)), (("ang",),))
        TWO_PI = 2.0 * np.pi
        C1 = 6.28125
        C2 = TWO_PI - C1
        for which, shift, dst in (("sin", 0.0, sinT), ("cos", np.pi / 2, cosT)):
            tscal("dve", tmpa[:], ang[:], float(shift), None, ALU.add, None, (("ang",),), (("tmpa",),))
            tscal("dve", tmpi[:], tmpa[:], float(1.0 / TWO_PI), None, ALU.mult, None, (("tmpa",),), (("tmpi",),))
            kf = S.alloc("kf", [64, TL], F32)
            tcopy("dve", kf[:], tmpi[:], (("tmpi",),), (("kf",),))
            stt(tmpa[:], kf[:], -C1, tmpa[:], ALU.mult, ALU.add, (("kf",), ("tmpa",)), (("tmpa",),))
            stt(tmpa[:], kf[:], -C2, tmpa[:], ALU.mult, ALU.add, (("kf",), ("tmpa",)), (("tmpa",),))
            tscal("dve", kf[:], tmpa[:], float(np.pi), -TWO_PI, ALU.is_gt, ALU.mult, (("tmpa",),), (("kf",),))
            tt("dve", tmpa[:], tmpa[:], kf[:], ALU.add, (("tmpa",), ("kf",)), (("tmpa",),))
            tscal("dve", kf[:], tmpa[:], float(-np.pi), TWO_PI, ALU.is_lt, ALU.mult, (("tmpa",),), (("kf",),))
            tt("dve", tmpa[:], tmpa[:], kf[:], ALU.add, (("tmpa",), ("kf",)), (("tmpa",),))
            tscal("dve", tmpa[:], tmpa[:], float(np.pi), float(-np.pi), ALU.min, ALU.max, (("tmpa",),), (("tmpa",),))
            if which == "sin":
                act(kf[:], tmpa[:], AF.Sin, (("tmpa",),), (("kf",),))
                tscal("dve", dst[:], kf[:], ropec[:, 1:2], None, ALU.mult, None, (("kf",), ("ropec",)), ((which + "T",),))
            else:
                act(dst[:], tmpa[:], AF.Sin, (("tmpa",),), ((which + "T",),))
            S.free("kf")
        for nm in ("posi", "ang", "tmpa", "tmpi"):
            S.free(nm)
        dbg("cosT", cosT[:], [64, TL], BF16, ("cosT",))
        dbg("sinT", sinT[:], [64, TL], BF16, ("sinT",))
        dbg("maskb", maskb[:], [128, NB, NB], F32, ("maskb",))
        ckpt("const")
        RCOS, RSIN = ("cosT",), ("sinT",)

        wa = [S.alloc(f"wa{i}", [128, KC, 512], BF16) for i in range(2)]
        brow = [S.alloc(f"brow{i}", [1, 512], F32) for i in range(2)]
        rowsb = [S.alloc(f"rowsb{i}", [1, 512], F32) for i in range(2)]
        w_ada_v = w_ada.rearrange("(kc p) n -> p kc n", p=128)
        fm_slot = {0: 0, 1: 1, 3: 2, 4: 3}
        for j in range(8):
            sl = j % 2
            v, q4 = j // 4, j % 4
            dma("pool", f"wa{sl}", wa[sl][:], w_ada_v[:, :, j * 512:(j + 1) * 512], (), ((f"wa{sl}",),))
            dma("sp", f"brow{sl}", brow[sl][:], b_ada[0:1, j * 512:(j + 1) * 512], (), ((f"brow{sl}",),))
            mm_group(pq0[0:1, :], ("pq0",),
                     [(cT_bf[:, kc:kc + 1], wa[sl][:, kc, :], (("cT_bf",), (f"wa{sl}",))) for kc in range(KC)])
            tt("dve", rowsb[sl][:], pq0[0:1, :], brow[sl][:], ALU.add, (("pq0",), (f"brow{sl}",)), ((f"rowsb{sl}",),))
            base = fm_slot[v] * 16 + q4 * 4
            for q in range(4):
                mm(pq1[:, base + q:base + q + 1], rowsb[sl][0:1, q * 128:(q + 1) * 128], onesf[0:1, 0:1],
                   True, True, ((f"rowsb{sl}",), ("onesf",)), (("pq1",),), mark=True)
        tcopy("dve", modT[:, 0:32], pq1[:, 0:32], (("pq1",),), (("modT", 1),))
        tscal("dve", modT[:, 16:32], modT[:, 16:32], 1.0, None, ALU.add, None, (("modT", 1),), (("modT", 1),))
        for nm in ("wa0", "wa1", "brow0", "brow1", "rowsb0", "rowsb1"):
            S.free(nm)

        DCW = 256
        defer = {"c": 0}

        def mod_chunk_def(wa2, brow2, rowsb2):
            c = defer["c"]
            if c >= (4 * D) // DCW:
                return
            defer["c"] += 1
            sl = c % 2
            col0 = 2 * D + c * DCW
            v, off = col0 // D, col0 % D
            dma("pool", f"wb{sl}", wa2[sl][:], w_ada_v[:, :, col0:col0 + DCW], (), ((f"wb{sl}",),))
            dma("sp", f"brow2{sl}", brow2[sl][:], b_ada[0:1, col0:col0 + DCW], (), ((f"browb{sl}",),))
            mm_group(pq0[0:1, 0:DCW], ("pq0",),
                     [(cT_bf[:, kc:kc + 1], wa2[sl][:, kc, :], (("cT_bf",), (f"wb{sl}",))) for kc in range(KC)])
            tt("dve", rowsb2[sl][:], pq0[0:1, 0:DCW], brow2[sl][:], ALU.add, (("pq0",), (f"browb{sl}",)),
               ((f"rowsbb{sl}",),))
            if v in (3, 4):
                base = fm_slot[v] * 16 + off // 128
                for q in range(2):
                    mm(pq0[:, 256 + q:257 + q], rowsb2[sl][0:1, q * 128:(q + 1) * 128], onesf[0:1, 0:1],
                       True, True, ((f"rowsbb{sl}",), ("onesf",)), (("pq0",),), mark=True)
                tscal("dve", modT[:, base:base + 2], pq0[:, 256:258], 1.0 if v == 4 else 0.0, None, ALU.add, None,
                      (("pq0",),), (("modT", 2),))
            else:
                gb = g1_b if v == 2 else g2_b
                gk = ("g1_b",) if v == 2 else ("g2_b",)
                mm(pq0[:, 256:512], onesf[0:1, :], rowsb2[sl][:], True, True, ((f"rowsbb{sl}",), ("onesf",)),
                   (("pq0",),), mark=True)
                tcopy("dve", gb[:, off:off + DCW], pq0[:, 256:512], (("pq0",),), (gk,))

        ckpt("A")

        xs = [S.alloc(f"xs{i}", [128, D], F32) for i in range(2)]
        xn = [S.alloc(f"xn{i}", [128, D], BF16) for i in range(2)]
        XN = [xn]
        cnt = {"ln": 0, "ev": 0}

        def ln_mod_T(src, skey, dst_fn, dkey, moff):
            i = cnt["ln"] % 2
            cnt["ln"] += 1
            rstd, nmr, KR, KN = ln_stats(src, skey, D)
            xn = XN[0]
            act(xn[i][:], src, AF.Identity, (skey, KR, KN), ((f"xn{i}",),), bias=nmr[:], scale=rstd[:])
            for half, (pt, pk) in enumerate(((ptA, ("ptA",)), (ptB, ("ptB",)))):
                for q in range(8):
                    kc = half * 8 + q
                    transpose(pt[:, q * 128:(q + 1) * 128], pk, xn[i][:, kc * 128:(kc + 1) * 128], (f"xn{i}",),
                              mark=(q == 7))
                for q in range(8):
                    kc = half * 8 + q
                    sc = modT[:, moff + 16 + kc:moff + 17 + kc]
                    sh = modT[:, moff + kc:moff + kc + 1]
                    if half == 0:
                        tscal("dve", dst_fn(kc), pt[:, q * 128:(q + 1) * 128], sc, sh, ALU.mult, ALU.add,
                              (pk, ("modT", 1 if moff == 0 else 2)), (dkey + (0,),))
                    else:
                        act(dst_fn(kc), pt[:, q * 128:(q + 1) * 128], AF.Identity, (pk, ("modT", 1 if moff == 0 else 2)), (dkey + (1,),),
                            bias=sh, scale=sc)

        hT = S.alloc("hT", [128, KC, TOK], BF16)
        for blk in range(NB):
            sl = blk % 2
            dma("sp", f"xs{sl}", xs[sl][:], x_loc[blk * 128:(blk + 1) * 128, :], (), ((f"xs{sl}",),))
            ln_mod_T(xs[sl][:], (f"xs{sl}",), lambda kc, blk=blk: hT[:, kc, blk * 128:(blk + 1) * 128], ("hT", blk), 0)
        dbg("hT", hT[:], [128, KC, TOK], BF16, ("hT", 7, 1))
        ckpt("hT")
        for nm in ("xs0", "xs1", "xn0", "xn1"):
            S.free(nm)

        wi = [S.alloc(f"wi{i}", [128, KC, 512], BF16) for i in range(2)]
        w_in_v = w_in.rearrange("(kc p) n -> p kc n", p=128)
        wcnt = [0]

        def load_win(c0, ncol):
            sl = wcnt[0] % 2
            wcnt[0] += 1
            dma("pool", f"wi{sl}", wi[sl][:, :, 0:ncol], w_in_v[:, :, c0:c0 + ncol], (), ((f"wi{sl}",),))
            return wi[sl], (f"wi{sl}",)

        bcnt = [0]

        def nbank():
            b = bcnt[0] % 4
            bcnt[0] += 1
            return bank(b)

        mixT = S.alloc("mixT", [128, 16, TOK], BF16, top=True)
        uT = S.alloc("uT", [128, 8, TOK], BF16)
        vsg = S.alloc("vsg", [128, NB, 1024], BF16)
        sgug_b = S.alloc("sgug_b", [128, 1024], F32)
        sgub_b = S.alloc("sgub_b", [128, 1024], F32)
        bsp_b = S.alloc("bsp_b", [128, 1024], F32)
        wsT = S.alloc("wsT", [128, 8, 128], BF16)
        wsT_f = S.alloc("wsT_f", [128, 8, 128], F32)
        dma("sp", "c_sgug", sgug_b[:], sgug_d[0:1, :].to_broadcast([128, 1024]), (), (("sgug_b",),))
        dma("sp", "c_sgub", sgub_b[:], sgub_d[0:1, :].to_broadcast([128, 1024]), (), (("sgub_b",),))
        dma("sp", "c_bsp", bsp_b[:], bsp_d[0:1, :].to_broadcast([128, 1024]), (), (("bsp_b",),))
        dma("sp", "c_wsT", wsT_f[:], wspT_d, (), (("wsT_f",),))
        tt("dve", wsT[:], wsT_f[:], maskT[:].unsqueeze(1).to_broadcast([128, 8, 128]), ALU.mult,
           (("wsT_f",), ("maskT",)), (("wsT",),))
        S.free("wsT_f")
        ckpt("B3a")

        def gelu_evac(out, bk, bkey, wkey):
            act(out, bk, AF.Gelu, (bkey,), (wkey,))

        for ch in range(2):
            wt, wk = load_win(1344 + ch * 512, 512)
            for m in range(4):
                for n in range(2):
                    bk, bkey = nbank()
                    mm_group(bk, bkey, [(wt[:, kc, m * 128:(m + 1) * 128], hT[:, kc, n * 512:(n + 1) * 512],
                                         (wk,) + hk(range(4 * n, 4 * n + 4))) for kc in range(KC)])
                    gelu_evac(uT[:, ch * 4 + m, n * 512:(n + 1) * 512], bk, bkey, ("uT",))
        ckpt("B3b")
        for ch in range(2):
            wt, wk = load_win(2368 + ch * 512, 512)
            for blk in range(NB):
                bk, bkey = nbank()
                mm_group(bk, bkey, [(hT[:, kc, blk * 128:(blk + 1) * 128], wt[:, kc, 0:512],
                                     (wk,) + hk([blk])) for kc in range(KC)])
                gelu_evac(vsg[:, blk, ch * 512:(ch + 1) * 512], bk, bkey, ("vsg", blk))
        dbg("uT", uT[:], [128, 8, TOK], BF16, ("uT",))
        dbg("vsg", vsg[:], [128, NB, 1024], BF16, ("vsg", 0))
        ckpt("B3c")
        vtmp = [S.alloc(f"vtmp{i}", [128, 1024], F32) for i in range(2)]
        vsn = [S.alloc(f"vsn{i}", [128, 1024], BF16) for i in range(2)]
        for blk in range(NB):
            i = blk % 2
            rstd, nmr, KR, KN = ln_stats(vsg[:, blk, :], ("vsg", blk), 1024)
            act(vtmp[i][:], vsg[:, blk, :], AF.Identity, (("vsg", blk), KR, KN), ((f"vtmp{i}",),),
                bias=nmr[:], scale=rstd[:])
            tt("dve", vtmp[i][:], vtmp[i][:], sgug_b[:], ALU.mult, ((f"vtmp{i}",), ("sgug_b",)), ((f"vtmp{i}",),))
            tt("dve", vsn[i][:], vtmp[i][:], sgub_b[:], ALU.add, ((f"vtmp{i}",), ("sgub_b",)), ((f"vsn{i}",),))
            for half in range(2):
                bk, bkey = nbank()
                for gg in range(4):
                    g = half * 4 + gg
                    mm(bk[:, gg * 128:(gg + 1) * 128], vsn[i][:, g * 128:(g + 1) * 128], wsT[:, g, :], True, True,
                       ((f"vsn{i}",), ("wsT",)), (bkey,), mark=(gg == 3))
                tt("dve", vtmp[i][:, half * 512:(half + 1) * 512], bk, bsp_b[:, half * 512:(half + 1) * 512], ALU.add,
                   (bkey, ("bsp_b",)), ((f"vtmp{i}",),))
                tt("dve", mixT[:, 8 + half * 4:12 + half * 4, blk * 128:(blk + 1) * 128],
                   vtmp[i][:, half * 512:(half + 1) * 512].rearrange("p (g t) -> p g t", g=4),
                   uT[:, half * 4:half * 4 + 4, blk * 128:(blk + 1) * 128], ALU.mult,
                   ((f"vtmp{i}",), ("uT",)), (("mixT", "s"),))
        for nm in ("uT", "vsg", "sgug_b", "sgub_b", "bsp_b", "wsT", "vtmp0", "vtmp1", "vsn0", "vsn1"):
            S.free(nm)
        dbg("sguT", mixT[:, 8:16, :], [128, 8, TOK], BF16, ("mixT", "s"))
        ckpt("B3")

        sqt = [S.alloc(f"sqt{i}", [128, 512], BF16) for i in range(2)]
        rsb = S.alloc("rsb", [128, 512], F32)
        rt1 = S.alloc("rt1", [64, 512], F32)
        rt2 = S.alloc("rt2", [64, 512], F32)
        sqc = [0]

        def rms_feature_major(dstT, dkey, nchunk, wt_fn, src_fn, skeys, gain, ntot, gkey):
            for m in range(nchunk):
                bk, bkey = nbank()
                wt, wk, c0 = wt_fn(m)
                mm_group(bk, bkey, [(wt[:, kc, c0:c0 + 128], src_fn(kc), (wk,) + skeys) for kc in range(KC)])
                tcopy("dve", dstT[:, m, :], bk, (bkey,), (dkey,))
                i = sqc[0] % 2
                sqc[0] += 1
                act(sqt[i][:], bk, AF.Square, (bkey,), ((f"sqt{i}",),))
                mm(pq0[:, :], ones_bf[:], sqt[i][:], m == 0, m == nchunk - 1, ((f"sqt{i}",), ("ones_bf",)),
                   (("pq0",),) if (m == 0 or m == nchunk - 1) else (), mark=(m == nchunk - 1))
            act(rsb[:], pq0[:, :], AF.Sqrt, (("pq0",),), (("rsb",),), bias=EPS, scale=1.0 / ntot)
            S.op("dve", lambda e: e.reciprocal(out=rsb[:], in_=rsb[:]), (("rsb",),), (("rsb",),))
            for m in range(nchunk):
                stt(dstT[:, m, :], dstT[:, m, :], gain[:, m:m + 1], rsb[:], ALU.mult, ALU.mult,
                    (dkey, ("rsb",), gkey), (dkey,))

        def rope_evac(pe_ps, pe_key, sw_ps, sw_key, tok0, dst, dkey):
            tt("dve", rt1[:], pe_ps, cosT[:, tok0:tok0 + 512], ALU.mult, (pe_key, RCOS), (("rt1",),))
            tt("dve", rt2[:], sw_ps, sinT[:, tok0:tok0 + 512], ALU.mult, (sw_key, RSIN), (("rt2",),))
            tt("dve", dst, rt1[:], rt2[:], ALU.add, (("rt1",), ("rt2",)), (dkey,))

        qnT = S.alloc("qnT", [128, H, TOK], BF16, top=True)
        qrT = S.alloc("qrT", [64, H, TOK], BF16, top=True)
        cqT = S.alloc("cqT", [128, 6, TOK], BF16)
        wqn = S.alloc("wqn", [128, 6, H, 128], BF16)
        wqp = S.alloc("wqp", [128, 6, H, 96], BF16)
        w_uq_v = w_uq.rearrange("(kc p) (h d) -> p kc h d", p=128, d=192)
        for kc in range(6):
            dma("pool", "wq", wqn[:, kc, :, :], w_uq_v[:, kc, :, 0:128], (), (("wqn", kc), ("wqn", "ser")))
            dma("pool", "wq", wqp[:, kc, :, 0:64], w_uq_v[:, kc, :, 128:192], (), (("wqp", kc, 0), ("wqn", "ser")))
            dma("pool", "wq", wqp[:, kc, :, 64:96], w_uq_v[:, kc, :, 128:160], (), (("wqp", kc, 1), ("wqn", "ser")))
        wA, wAk = load_win(0, 512)
        wB, wBk = load_win(512, 256)
        for n in range(2):
            rms_feature_major(cqT[:, :, n * 512:(n + 1) * 512], ("cqT", n), 6,
                              lambda m: (wA, wAk, m * 128) if m < 4 else (wB, wBk, (m - 4) * 128),
                              lambda kc, n=n: hT[:, kc, n * 512:(n + 1) * 512], hk(range(4 * n, 4 * n + 4)), qg, 768.0, ("qg",))
        for n in range(2):
            for h in range(H):
                bk, bkey = nbank()
                mm_group(bk, bkey, [(wqn[:, kc, h, :], cqT[:, kc, n * 512:(n + 1) * 512], (("wqn", kc), ("cqT", n)))
                                    for kc in range(6)])
                evac(h, qnT[:, h, n * 512:(n + 1) * 512], bk, (bkey,), ("qnT",))
                b1, b1k = nbank()
                mm_group(b1[0:64, :], b1k, [(wqp[:, kc, h, 0:64], cqT[:, kc, n * 512:(n + 1) * 512],
                                             (("wqp", kc, 0), ("wqp", kc, 1), ("cqT", n))) for kc in range(6)])
                b2, b2k = nbank()
                mm_group(b2[0:64, :], b2k, [(wqp[:, kc, h, 32:96], cqT[:, kc, n * 512:(n + 1) * 512],
                                             (("wqp", kc, 0), ("wqp", kc, 1), ("cqT", n))) for kc in range(6)])
                rope_evac(b1[0:64, :], b1k, b2[0:64, :], b2k, n * 512, qrT[:, h, n * 512:(n + 1) * 512], ("qrT",))
        for nm in ("cqT", "wqn", "wqp"):
            S.free(nm)
        dbg("qnT", qnT[:], [128, H, TOK], BF16, ("qnT", 1))
        dbg("qrT", qrT[:], [64, H, TOK], BF16, ("qrT",))
        ckpt("B2")

        ckvT = S.alloc("ckvT", [128, 4, TL], BF16, top=True)
        krT = S.alloc("krT", [64, TL], BF16, top=True)
        wkp = S.alloc("wkp", [128, KC, 96], BF16, top=True)
        wC, wCk = load_win(768, 512)
        dma("pool", "wkpa", wkp[:, :, 0:64], w_in_v[:, :, 1280:1344], (), (("wkp", 0),))
        dma("pool", "wkpb", wkp[:, :, 64:96], w_in_v[:, :, 1280:1312], (), (("wkp", 1),))

        def kv_group(n, src_fn, skeys):
            rms_feature_major(ckvT[:, :, n * 512:(n + 1) * 512], ("ckvT", n), 4,
                              lambda m: (wC, wCk, m * 128), src_fn, skeys, kvg, 512.0, ("kvg",))
            b1, b1k = nbank()
            mm_group(b1[0:64, :], b1k, [(wkp[:, kc, 0:64], src_fn(kc), (("wkp", 0), ("wkp", 1)) + skeys) for kc in range(KC)])
            b2, b2k = nbank()
            mm_group(b2[0:64, :], b2k, [(wkp[:, kc, 32:96], src_fn(kc), (("wkp", 0), ("wkp", 1)) + skeys) for kc in range(KC)])
            rope_evac(b1[0:64, :], b1k, b2[0:64, :], b2k, n * 512, krT[:, n * 512:(n + 1) * 512], ("krT",))

        for n in range(2):
            kv_group(n, lambda kc, n=n: hT[:, kc, n * 512:(n + 1) * 512], hk(range(4 * n, 4 * n + 4)))
        S.free("hT")
        S.free("wi0" if wCk == ("wi1",) else "wi1")
        xs = [S.alloc(f"xs{i}", [128, D], F32) for i in range(2)]
        xn = [S.alloc(f"xn{i}", [128, D], BF16) for i in range(2)]
        XN[0] = xn
        hTo = [S.alloc(f"hTo{i}", [128, KC, 512], BF16) for i in range(2)]
        for n in range(2, 4):
            i = n % 2
            for bb in range(4):
                blk = n * 4 + bb
                sl = blk % 2
                dma("sp", f"xs{sl}", xs[sl][:], x_loc[blk * 128:(blk + 1) * 128, :], (), ((f"xs{sl}",),))
                ln_mod_T(xs[sl][:], (f"xs{sl}",), lambda kc, i=i, bb=bb: hTo[i][:, kc, bb * 128:(bb + 1) * 128],
                         (f"hTo{i}",), 0)
            kv_group(n, lambda kc, i=i: hTo[i][:, kc, :], k2((f"hTo{i}",)))
        for nm in ("hTo0", "hTo1", wCk[0], "wkp", "xs0", "xs1", "xn0", "xn1",
                   "sqt0", "sqt1", "rsb", "rt1", "rt2", "cosT", "sinT"):
            S.free(nm)
        dbg("ckvT", ckvT[:], [128, 4, TL], BF16, ("ckvT", 0))
        dbg("krT", krT[:], [64, TL], BF16, ("krT",))
        ckpt("B1b")

        wkk = S.alloc("wkk", [128, 4, H, 128], BF16)
        wvv = S.alloc("wvv", [128, 4, H, 128], BF16)
        w_ukv_v = w_ukv.rearrange("(kc p) (h two d) -> p kc h two d", p=128, two=2, d=128)
        for kc in range(4):
            dma("pool", "wkv", wkk[:, kc, :, :], w_ukv_v[:, kc, :, 0, :], (), (("wkk", kc), ("wkk", "ser")))
            dma("pool", "wkv", wvv[:, kc, :, :], w_ukv_v[:, kc, :, 1, :], (), (("wvv", kc), ("wkk", "ser")))
        knT = S.alloc("knT", [128, H, TL], BF16, top=True)
        V = S.alloc("V", [128, 16, H * 128], BF16, top=True)
        ec = 0
        for n in range(4):
            for h in range(H):
                bk, bkey = nbank()
                mm_group(bk, bkey, [(wkk[:, kc, h, :], ckvT[:, kc, n * 512:(n + 1) * 512], (("wkk", kc), ("ckvT", n)))
                                    for kc in range(4)])
                evac(ec, knT[:, h, n * 512:(n + 1) * 512], bk, (bkey,), ("knT",))
                ec += 1
            for bb in range(4):
                lb = n * 4 + bb
                for g in range(2):
                    bk, bkey = nbank()
                    mm_group(bk, bkey, [(ckvT[:, kc, lb * 128:(lb + 1) * 128],
                                         wvv[:, kc, g * 4:(g + 1) * 4, :], (("wvv", kc), ("ckvT", n))) for kc in range(4)])
                    evac(ec, V[:, lb, g * 512:(g + 1) * 512], bk, (bkey,), ("V",))
                    ec += 1
        for nm in ("ckvT", "wkk", "wvv"):
            S.free(nm)
        dbg("knT", knT[:], [128, H, TL], BF16, ("knT", 1))
        dbg("V", V[:], [128, 16, H * 128], BF16, ("V", 1))
        ckpt("B1c")

        Pm = [S.alloc(f"Pm{i}", [128, TL], BF16) for i in range(2)]
        PT = [S.alloc(f"PT{i}", [128, 16, 128], BF16) for i in range(2)]
        attn = [S.alloc(f"attn{i}", [128, H * 128], BF16) for i in range(2)]
        mx = S.alloc("mx", [128, 1], F32)
        nb_ = S.alloc("nb_", [128, 1], F32)
        rsum = S.alloc("rsum", [128, 1], F32)
        rinv = S.alloc("rinv", [128, 1], F32)
        wa2 = [S.alloc(f"wb{i}", [128, KC, DCW], BF16) for i in range(2)]
        brow2 = [S.alloc(f"browb{i}", [1, DCW], F32) for i in range(2)]
        rowsb2 = [S.alloc(f"rowsbb{i}", [1, DCW], F32) for i in range(2)]
        it = 0
        for j in range(NB):
            nk = j + 1
            W = nk * 128
            ai = j % 2
            for h in range(H):
                pi = it % 2
                it += 1
                if it % 2 == 0:
                    mod_chunk_def(wa2, brow2, rowsb2)
                segs = []
                for side in range(2):
                    k0 = side * 1024
                    c = 0
                    while c < W:
                        w_ = min(512 - ((side * W + c) % 512), W - c)
                        segs.append((side * W + c, k0 + c, w_))
                        c += w_
                for (col, key0, w_) in segs:
                    bnk = col // 512
                    assert (col + w_ - 1) // 512 == bnk
                    mm(pbig[:, col:col + w_], qnT[:, h, j * 128:(j + 1) * 128], knT[:, h, key0:key0 + w_], True, False,
                       k2(("qnT",)) + k2(("knT",)), (("pbig", bnk),), mark=False)
                    mm(pbig[:, col:col + w_], qrT[:, h, j * 128:(j + 1) * 128], krT[:, key0:key0 + w_], False, True,
                       (("qrT",), ("krT",)), (("pbig", bnk),), mark=True)
                banks = tuple(("pbig", b) for b in range((2 * W + 511) // 512))
                dcol = j * 128
                tt("dve", pbig[:, dcol:dcol + 128], pbig[:, dcol:dcol + 128], tri[:], ALU.add,
                   (("pbig", dcol // 512), ("tri",)), (("pbig", dcol // 512),))
                tt("dve", pbig[:, W:2 * W].rearrange("p (b k) -> p b k", k=128),
                   pbig[:, W:2 * W].rearrange("p (b k) -> p b k", k=128),
                   maskb[:, j, 0:nk].unsqueeze(2).to_broadcast([128, nk, 128]), ALU.add,
                   banks + (("maskb",),), banks)
                S.op("dve", lambda e, W=W: e.reduce_max(out=mx[:], in_=pbig[:, 0:2 * W], axis=AX.X), banks, (("mx",),))
                tscal("dve", nb_[:], mx[:], -SM_SCALE, None, ALU.mult, None, (("mx",),), (("nb_",),))
                act(Pm[pi][:, 0:2 * W], pbig[:, 0:2 * W], AF.Exp, banks + (("nb_",),), ((f"Pm{pi}",), ("rsum",)),
                    bias=nb_[:], scale=SM_SCALE, accum=rsum[:])
                S.op("dve", lambda e: e.reciprocal(out=rinv[:], in_=rsum[:]), (("rsum",),), (("rinv",),))
                nblk = 2 * nk
                for bi, b0 in enumerate(range(0, nblk, 8)):
                    pt, pk = (ptA, ("ptA",)) if bi % 2 == 0 else (ptB, ("ptB",))
                    nb8 = min(8, nblk - b0)
                    for q in range(nb8):
                        transpose(pt[:, q * 128:(q + 1) * 128], pk, Pm[pi][:, (b0 + q) * 128:(b0 + q + 1) * 128],
                                  (f"Pm{pi}",), mark=(q == nb8 - 1))
                    src = pt[:, 0:nb8 * 128].rearrange("p (b q) -> p b q", q=128)
                    if bi % 2 == 0:
                        tcopy("dve", PT[pi][:, b0:b0 + nb8, :], src, (pk,), ((f"PT{pi}",),))
                    else:
                        act(PT[pi][:, b0:b0 + nb8, :], src, AF.Copy, (pk,), ((f"PT{pi}",),))
                items = []
                for b in range(nblk):
                    lb = b if b < nk else 8 + (b - nk)
                    items.append((PT[pi][:, b, :], V[:, lb, h * 128:(h + 1) * 128], ((f"PT{pi}",),) + k2(("V",))))
                mm_group(pq1[:, 0:128], ("pq1",), items)
                tscal("dve", attn[ai][:, h * 128:(h + 1) * 128], pq1[:, 0:128], rinv[:], None, ALU.mult, None,
                      (("pq1",), ("rinv",)), ((f"attn{ai}",),))
            for q in range(8):
                transpose(ptA[:, q * 128:(q + 1) * 128], ("ptA",), attn[ai][:, q * 128:(q + 1) * 128], (f"attn{ai}",),
                          mark=(q == 7))
            tcopy("dve", mixT[:, 0:8, j * 128:(j + 1) * 128], ptA[:, :].rearrange("p (c q) -> p c q", q=128),
                  (("ptA",),), (("mixT", "a"),))
        while defer["c"] < (4 * D) // DCW:
            mod_chunk_def(wa2, brow2, rowsb2)
        for nm in ("wb0", "wb1", "browb0", "browb1", "rowsbb0", "rowsbb1"):
            S.free(nm)
        for nm in ("Pm0", "Pm1", "PT0", "PT1", "attn0", "attn1", "mx", "nb_", "rsum", "rinv",
                   "qnT", "qrT", "knT", "krT", "V"):
            S.free(nm)
        dbg("mixT", mixT[:], [128, 16, TOK], BF16, ("mixT", "a"))
        ckpt("C")

        x1 = S.alloc("x1", [128, NB, D], F32, top=True)
        xs = [S.alloc(f"xs{i}", [128, D], F32) for i in range(2)]
        wo = [S.alloc(f"wo{i}", [128, KC, 512], BF16) for i in range(2)]
        lng = S.alloc("lng", [128, D], F32)
        lnb = S.alloc("lnb", [128, D], F32)
        dma("sp", "c_ln1g", lng[:], ln1g_d[0:1, :].to_broadcast([128, D]), (), (("lng",),))
        dma("sp", "c_ln1b", lnb[:], ln1b_d[0:1, :].to_broadcast([128, D]), (), (("lnb",),))
        w_o_v = w_o.rearrange("(kc p) n -> p kc n", p=128)
        for cg in range(4):
            sl = cg % 2
            dma("pool", f"wo{sl}", wo[sl][:], w_o_v[:, :, cg * 512:(cg + 1) * 512], (), ((f"wo{sl}",),))
            for blk in range(NB):
                bk, bkey = nbank()
                mm_group(bk, bkey, [(mixT[:, kc, blk * 128:(blk + 1) * 128], wo[sl][:, kc, :],
                                     ((f"wo{sl}",), ("mixT", "a"), ("mixT", "s"))) for kc in range(KC)])
                tt("dve", x1[:, blk, cg * 512:(cg + 1) * 512], bk, g1_b[:, cg * 512:(cg + 1) * 512], ALU.mult,
                   (bkey, ("g1_b",)), (("x1", blk),))
        for blk in range(NB):
            sl = blk % 2
            dma("sp", f"xs{sl}", xs[sl][:], x_loc[blk * 128:(blk + 1) * 128, :], (), ((f"xs{sl}",),))
            stt(x1[:, blk, :], xs[sl][:], ALPHA, x1[:, blk, :], ALU.mult, ALU.add, ((f"xs{sl}",), ("x1", blk)),
                (("x1", blk),))
            rstd, nmr, KR, KN = ln_stats(x1[:, blk, :], ("x1", blk), D)
            act(x1[:, blk, :], x1[:, blk, :], AF.Identity, (("x1", blk), KR, KN), (("x1", blk),),
                bias=nmr[:], scale=rstd[:])
            tt("pool", x1[:, blk, :], x1[:, blk, :], lng[:], ALU.mult, (("x1", blk), ("lng",)), (("x1", blk),))
            tt("dve", x1[:, blk, :], x1[:, blk, :], lnb[:], ALU.add, (("x1", blk), ("lnb",)), (("x1", blk),))
        for nm in ("mixT", "wo0", "wo1", "lng", "lnb", "xs0", "xs1"):
            S.free(nm)
        dbg("x1", x1[:], [128, NB, D], F32, ("x1", 0))
        ckpt("D")

        h2T = S.alloc("h2T", [128, KC, TOK], BF16, top=True)
        xn = [S.alloc(f"xn{i}", [128, D], BF16) for i in range(2)]
        XN[0] = xn
        wr = S.alloc("wr", [128, KC, 36], BF16)
        dma("pool", "wr", wr[:], wr_d.rearrange("(kc p) n -> p kc n", p=128), (), (("wr",),))
        if SPARSE:
            h2tok = S.alloc("h2tok", [128, NB, D], BF16, top=True)
            sc2_b = S.alloc("sc2_b", [128, D], F32)
            sh2_b = S.alloc("sh2_b", [128, D], F32)
            identf = S.alloc("identf", [128, 128], F32)
            onesF = S.alloc("onesF", [128, 128], F32)
            dg = [S.alloc(f"dg{i}", [128, 128], F32) for i in range(2)]
            h2tmp = S.alloc("h2tmp", [128, D], F32)
            tscal("dve", identf[:], iot[:], 0.0, None, ALU.is_equal, None, (("iot",),), (("identf",),))
            S.op("dve", lambda e: e.memset(onesF[:], 1.0), (), (("onesF",),))
            dgc = 0
            for (dst, dk, c0) in ((sh2_b, ("sh2_b",), 32), (sc2_b, ("sc2_b",), 48)):
                for kq in range(4):
                    bk, bkey = nbank()
                    for q in range(4):
                        kc = kq * 4 + q
                        di = dgc % 2
                        dgc += 1
                        tscal("dve", dg[di][:], identf[:], modT[:, c0 + kc:c0 + kc + 1], None, ALU.mult, None,
                              (("identf",), ("modT", 2)), ((f"dg{di}",),))
                        mm(bk[:, q * 128:(q + 1) * 128], onesF[:], dg[di][:], True, True,
                           ((f"dg{di}",), ("onesF",)), (bkey,), mark=True)
                    tcopy("dve", dst[:, kq * 512:(kq + 1) * 512], bk, (bkey,), (dk,))
        for blk in range(NB):
            ln_mod_T(x1[:, blk, :], ("x1", blk), lambda kc, blk=blk: h2T[:, kc, blk * 128:(blk + 1) * 128],
                     ("h2T",), 32)
            if SPARSE:
                xi = (cnt["ln"] - 1) % 2
                tt("pool", h2tmp[:], XN[0][xi][:], sc2_b[:], ALU.mult, ((f"xn{xi}",), ("sc2_b",)), (("h2tmp",),))
                tt("dve", h2tok[:, blk, :], h2tmp[:], sh2_b[:], ALU.add, (("h2tmp",), ("sh2_b",)), (("h2tok", blk),))
            mm_group(pq0[:, 0:36], ("pq0",), [(h2T[:, kc, blk * 128:(blk + 1) * 128], wr[:, kc, :],
                                               k2(("h2T",)) + (("wr",),)) for kc in range(KC)])
            tt("dve", logits[:, blk, :], pq0[:, 0:36], brb[:], ALU.add, (("pq0",), ("brb",)), (("logits",),))
            tscal("pool", x1[:, blk, :], x1[:, blk, :], ALPHA, None, ALU.mult, None, (("x1", blk),), (("x1", blk),))
        S.free("xn0"); S.free("xn1"); S.free("wr")
        if SPARSE:
            for nm in ("sc2_b", "sh2_b", "identf", "onesF", "dg0", "dg1", "h2tmp"):
                S.free(nm)
        dbg("h2T", h2T[:], [128, KC, TOK], BF16, ("h2T", 1))
        dbg("logits", logits[:], [128, NB, 36], F32, ("logits",))

        L = ("logits",)
        lg = logits[:, :, 0:4]
        le = logits[:, :, 4:36].rearrange("p b (g e) -> p b g e", e=8)

        def bc3(t2, n):
            return t2.unsqueeze(2).to_broadcast([128, NB, n])

        S.op("dve", lambda e: e.tensor_reduce(out=r_mg[:], in_=lg, axis=AX.X, op=ALU.max), (L,), (("r_mg",),))
        tt("dve", r_eg[:], lg, bc3(r_mg[:], 4), ALU.subtract, (L, ("r_mg",)), (("r_eg",),))
        tt("dve", r_oh[:], lg, bc3(r_mg[:], 4), ALU.is_equal, (L, ("r_mg",)), (("r_oh",),))
        act(r_eg[:], r_eg[:], AF.Exp, (("r_eg",),), (("r_eg",),))
        S.op("dve", lambda e: e.tensor_reduce(out=r_sg[:], in_=r_eg[:], axis=AX.X, op=ALU.add), (("r_eg",),), (("r_sg",),))
        S.op("dve", lambda e: e.reciprocal(out=r_pg[:], in_=r_sg[:]), (("r_sg",),), (("r_pg",),))
        tt("dve", r_t4[:], le, r_oh[:].unsqueeze(3).to_broadcast([128, NB, 4, 8]), ALU.mult, (L, ("r_oh",)), (("r_t4",),))
        S.op("dve", lambda e: e.tensor_reduce(out=r_sel[:], in_=r_t4[:].rearrange("p b g e -> p b e g"), axis=AX.X,
                                              op=ALU.add), (("r_t4",),), (("r_sel",),))
        S.op("dve", lambda e: e.tensor_reduce(out=r_l1[:], in_=r_sel[:], axis=AX.X, op=ALU.max), (("r_sel",),), (("r_l1",),))
        tt("dve", r_oh1[:], r_sel[:], bc3(r_l1[:], 8), ALU.is_equal, (("r_sel",), ("r_l1",)), (("r_oh1",),))
        stt(r_msk[:], r_oh1[:], -1e30, r_sel[:], ALU.mult, ALU.add, (("r_oh1",), ("r_sel",)), (("r_msk",),))
        S.op("dve", lambda e: e.tensor_reduce(out=r_l2[:], in_=r_msk[:], axis=AX.X, op=ALU.max), (("r_msk",),), (("r_l2",),))
        tt("dve", r_oh2[:], r_msk[:], bc3(r_l2[:], 8), ALU.is_equal, (("r_msk",), ("r_l2",)), (("r_oh2",),))
        tt("dve", r_w1[:], r_l2[:], r_l1[:], ALU.subtract, (("r_l2",), ("r_l1",)), (("r_w1",),))
        act(r_w1[:], r_w1[:], AF.Exp, (("r_w1",),), (("r_w1",),))
        tscal("dve", r_w1[:], r_w1[:], 1.0, None, ALU.add, None, (("r_w1",),), (("r_w1",),))
        S.op("dve", lambda e: e.reciprocal(out=r_w1[:], in_=r_w1[:]), (("r_w1",),), (("r_w1",),))
        tscal("dve", r_w2[:], r_w1[:], -1.0, 1.0, ALU.mult, ALU.add, (("r_w1",),), (("r_w2",),))
        tt("dve", r_w1[:], r_w1[:], r_pg[:], ALU.mult, (("r_w1",), ("r_pg",)), (("r_w1",),))
        tt("dve", r_w2[:], r_w2[:], r_pg[:], ALU.mult, (("r_w2",), ("r_pg",)), (("r_w2",),))
        tt("dve", r_ce[:], r_oh1[:], bc3(r_w1[:], 8), ALU.mult, (("r_oh1",), ("r_w1",)), (("r_ce",),))
        tt("dve", r_oh2[:], r_oh2[:], bc3(r_w2[:], 8), ALU.mult, (("r_oh2",), ("r_w2",)), (("r_oh2",),))
        tt("dve", r_ce[:], r_ce[:], r_oh2[:], ALU.add, (("r_ce",), ("r_oh2",)), (("r_ce",),))
        comb4 = comb[:].rearrange("p b (g e) -> p b g e", e=8)
        for g in range(4):
            tt("dve", comb4[:, :, g, :], r_ce[:], bc3(r_oh[:, :, g], 8), ALU.mult, (("r_ce",), ("r_oh",)), (("comb",),))
        dbg("comb", comb[:], [128, NB, 32], F32, ("comb",))
        ckpt("R")

        if SPARSE:
            S.free("h2T")
            NSLOT = 48
            NROW = NSLOT * 128
            Mf = S.alloc("Mf", [128, NB, 32], F32)
            Mb = S.alloc("Mb", [128, NB, 32], BF16)
            Ub = S.alloc("Ub", [128, 128], BF16)
            rank = S.alloc("rank", [128, NB, 32], F32)
            cntt = S.alloc("cntt", [128, 32], F32)
            nst = S.alloc("nst", [128, 32], F32)
            cs = [S.alloc(f"cs{i}", [128, 32], F32) for i in range(2)]
            sot = S.alloc("sot", [128, 32], F32)
            pos = S.alloc("pos", [128, NB, 32], F32)
            posm = S.alloc("posm", [128, NB, 32], F32)
            ptmp = S.alloc("ptmp", [128, NB, 32], F32)
            pAf = S.alloc("pAf", [128, NB], F32)
            pBf = S.alloc("pBf", [128, NB], F32)
            pAi = S.alloc("pAi", [128, NB], I32)
            pBi = S.alloc("pBi", [128, NB], I32)
            wA = S.alloc("wA", [128, NB], F32)
            wB = S.alloc("wB", [128, NB], F32)
            siota = S.alloc("siota", [128, NSLOT], F32)
            ecmp = S.alloc("ecmp", [128, NSLOT, 32], F32)
            eidf = S.alloc("eidf", [128, NSLOT], F32)
            eidi = S.alloc("eidi", [128, NSLOT], I32)
            C = ("comb",)
            tscal("dve", Mf[:], comb[:], 0.0, None, ALU.is_gt, None, (C,), (("Mf",),))
            tcopy("dve", Mb[:], Mf[:], (("Mf",),), (("Mb",),))
            tscal("dve", Ub[:], iot[:], 0.0, None, ALU.is_gt, None, (("iot",),), (("Ub",),))
            S.op("pool", lambda e: e.iota(siota[:], pattern=[[1, NSLOT]], base=0, channel_multiplier=0,
                                          allow_small_or_imprecise_dtypes=True), (), (("siota",),))
            for b in range(NB):
                bk, bkey = nbank()
                items = [(ones_bf[:], Mb[:, b2, :], (("Mb",), ("ones_bf",))) for b2 in range(b)]
                items.append((Ub[:], Mb[:, b, :], (("Mb",), ("Ub",))))
                mm_group(bk[:, 0:32], bkey, items)
                tcopy("dve", rank[:, b, :], bk[:, 0:32], (bkey,), (("rank",),))
            bk, bkey = nbank()
            mm_group(bk[:, 0:32], bkey, [(ones_bf[:], Mb[:, b2, :], (("Mb",), ("ones_bf",))) for b2 in range(NB)])
            tcopy("dve", cntt[:], bk[:, 0:32], (bkey,), (("cntt",),))
            tscal("dve", nst[:], cntt[:], 0.0, None, ALU.is_gt, None, (("cntt",),), (("nst",),))
            for k in range(1, 8):
                stt(nst[:], cntt[:], 128.0 * k, nst[:], ALU.is_gt, ALU.add, (("cntt",), ("nst",)), (("nst",),))
            tcopy("dve", cs[0][:], nst[:], (("nst",),), (("cs0",),))
            cur = 0
            for dstep in (1, 2, 4, 8, 16):
                nxt = 1 - cur
                tcopy("dve", cs[nxt][:], cs[cur][:], ((f"cs{cur}",),), ((f"cs{nxt}",),))
                tt("dve", cs[nxt][:, dstep:32], cs[cur][:, dstep:32], cs[cur][:, 0:32 - dstep], ALU.add,
                   ((f"cs{cur}",),), ((f"cs{nxt}",),))
                cur = nxt
            tt("dve", sot[:], cs[cur][:], nst[:], ALU.subtract, ((f"cs{cur}",), ("nst",)), (("sot",),))
            for b in range(NB):
                stt(pos[:, b, :], sot[:], 128.0, rank[:, b, :], ALU.mult, ALU.add, (("sot",), ("rank",)), (("pos",),))
            tscal("dve", ptmp[:], Mf[:], -1.0e6, 1.0e6, ALU.mult, ALU.add, (("Mf",),), (("ptmp",),))
            tt("dve", posm[:], pos[:], ptmp[:], ALU.add, (("pos",), ("ptmp",)), (("posm",),))
            S.op("dve", lambda e: e.tensor_reduce(out=pAf[:], in_=posm[:], axis=AX.X, op=ALU.min), (("posm",),), (("pAf",),))
            tt("dve", ptmp[:], pos[:], Mf[:], ALU.mult, (("pos",), ("Mf",), ("posm",)), (("ptmp",),))
            S.op("dve", lambda e: e.tensor_reduce(out=pBf[:], in_=ptmp[:], axis=AX.X, op=ALU.max), (("ptmp",),), (("pBf",),))
            tcopy("dve", pAi[:], pAf[:], (("pAf",),), (("pAi",),))
            tcopy("dve", pBi[:], pBf[:], (("pBf",),), (("pBi",),))
            tt("dve", ptmp[:], posm[:], bc3(pAf[:], 32), ALU.is_equal, (("posm",), ("pAf",), ("pBf",)), (("ptmp",),))
            tt("dve", ptmp[:], ptmp[:], comb[:], ALU.mult, (("ptmp",), C), (("ptmp",),))
            S.op("dve", lambda e: e.tensor_reduce(out=wA[:], in_=ptmp[:], axis=AX.X, op=ALU.add), (("ptmp",),), (("wA",),))
            S.op("dve", lambda e: e.tensor_reduce(out=wB[:], in_=comb[:], axis=AX.X, op=ALU.add), (C,), (("wB",),))
            tt("dve", wB[:], wB[:], wA[:], ALU.subtract, (("wB",), ("wA",)), (("wB",),))
            tt("dve", ecmp[:], sot[:].unsqueeze(1).to_broadcast([128, NSLOT, 32]),
               siota[:].unsqueeze(2).to_broadcast([128, NSLOT, 32]), ALU.is_le, (("sot",), ("siota",)), (("ecmp",),))
            S.op("dve", lambda e: e.tensor_reduce(out=eidf[:], in_=ecmp[:], axis=AX.X, op=ALU.add), (("ecmp",),), (("eidf",),))
            tscal("dve", eidf[:], eidf[:], -1.0, None, ALU.add, None, (("eidf",),), (("eidf",),))
            tcopy("dve", eidi[:], eidf[:], (("eidf",),), (("eidi",),))
            idxf = S.alloc("idxf", [128, NSLOT, 2, 2], F32)
            idxw = S.alloc("idxw", [128, NSLOT, 2, 2], I32)
            pcol = S.alloc("pcol", [128, 1], F32)
            tscal("dve", pcol[:], iot[:, 0:1], -2.0, None, ALU.mult, None, (("iot",),), (("pcol",),))
            for hf in range(2):
                for pc in range(2):
                    tscal("dve", idxf[:, :, hf, pc], eidf[:], 512.0, 256.0 * hf + pc, ALU.mult, ALU.add,
                          (("eidf",),), (("idxf",),))
            tscal("dve", idxf[:], idxf[:], pcol[:], None, ALU.add, None, (("idxf",), ("pcol",)), (("idxf",),))
            emp = S.alloc("emp", [128, NSLOT], F32)
            tscal("dve", emp[:], siota[:], cs[cur][:, 31:32], 32768.0, ALU.is_ge, ALU.mult,
                  (("siota",), (f"cs{cur}",)), (("emp",),))
            tt("dve", idxf[:].rearrange("p s a b -> p s (a b)"), idxf[:].rearrange("p s a b -> p s (a b)"),
               emp[:].unsqueeze(2).to_broadcast([128, NSLOT, 4]), ALU.add, (("idxf",), ("emp",)), (("idxf",),))
            S.free("emp")
            tcopy("dve", idxw[:], idxf[:], (("idxf",),), (("idxw",),))
            S.free("idxf"); S.free("pcol")
            dbg("pAi", pAi[:], [128, NB], I32, ("pAi",))
            dbg("pBi", pBi[:], [128, NB], I32, ("pBi",))
            dbg("eidi", eidi[:], [128, NSLOT], I32, ("eidi",))
            dbg("wA", wA[:], [128, NB], F32, ("wA",))
            for nm in ("Mf", "Mb", "Ub", "rank", "cntt", "nst", "cs0", "cs1", "sot", "pos", "posm", "ptmp",
                       "pAf", "pBf", "siota", "ecmp", "eidf"):
                S.free(nm)

            xs_scr = nc.dram_tensor("xs_scr", [NROW, D], BF16, kind="Internal").ap()
            ys_scr = nc.dram_tensor("ys_scr", [NROW, D], F32, kind="Internal").ap()
            XSK, YSK = ("xs_scr",), ("ys_scr",)
            Xs = [S.alloc(f"Xs{i}", [128, D], BF16) for i in range(2)]
            S.op("dve", lambda e: e.memset(Xs[0][:], 0.0), (), (("Xs0",),))
            XALL = tuple(("xs_scr", q_) for q_ in range(NSLOT))
            for s_ in range(NSLOT):
                dma("sp", f"xz{s_ % 4}", xs_scr[s_ * 128:(s_ + 1) * 128, :], Xs[0][:], (("Xs0",),),
                    (("xs_scr", s_), ("xs_scr", "z", s_ % 4)))
            for b in range(NB):
                for (pi_, pk_) in ((pAi, ("pAi",)), (pBi, ("pBi",))):
                    S.dma("pool", "sc",
                          lambda e, b=b, pi_=pi_: e.indirect_dma_start(
                              out=xs_scr[:, :], out_offset=bass.IndirectOffsetOnAxis(ap=pi_[:, b:b + 1], axis=0),
                              in_=h2tok[:, b, :], in_offset=None),
                          (("h2tok", b), pk_), XALL)
            S.free("h2tok")

            NSL = 3
            wg = [S.alloc(f"wg{i}", [128, KC, 256], BF16) for i in range(NSL)]
            wu = [S.alloc(f"wu{i}", [128, KC, 256], BF16) for i in range(NSL)]
            wd = [S.alloc(f"wd{i}", [128, 2, D], BF16) for i in range(NSL)]
            XsT = S.alloc("XsT", [128, KC, 128], BF16)
            sgt = S.alloc("sgt", [128, 256], BF16)
            hidh = S.alloc("hidh", [128, 256], BF16)
            hidT = S.alloc("hidT", [128, 2, 128], BF16)
            Ys = S.alloc("Ys", [128, D], F32)

            def load_unit(u):
                s_, hf = u // 2, u % 2
                sl = u % NSL
                for (nm, dst, src) in (("wg", wg[sl], w_gate), ("wu", wu[sl], w_up), ("wd", wd[sl], w_down)):
                    for pc in range(2):
                        if nm == "wd":
                            d2 = dst[:, pc, :]
                        else:
                            d2 = dst[:, pc * 8:(pc + 1) * 8, :].rearrange("p a b -> p (a b)")
                        S.dma("pool", f"{nm}{sl}{pc}",
                              lambda e, d2=d2, src=src, s_=s_, hf=hf, pc=pc: e.indirect_dma_start(
                                  out=d2, out_offset=None, in_=src[:, :],
                                  in_offset=bass.IndirectOffsetOnAxis(ap=idxw[:, s_, hf, pc:pc + 1], axis=0),
                                  bounds_check=BREG["r"], oob_is_err=False),
                              (("idxw",),), ((f"{nm}{sl}", pc),))

            load_unit(0)
            for s_ in range(NSLOT):
                xsl = s_ % 2
                dma("sp", f"Xs{xsl}", Xs[xsl][:], xs_scr[s_ * 128:(s_ + 1) * 128, :], (("xs_scr", s_),), ((f"Xs{xsl}",),))
                for half, (pt, pk) in enumerate(((ptA, ("ptA",)), (ptB, ("ptB",)))):
                    for q in range(8):
                        kc = half * 8 + q
                        transpose(pt[:, q * 128:(q + 1) * 128], pk, Xs[xsl][:, kc * 128:(kc + 1) * 128], (f"Xs{xsl}",),
                                  mark=(q == 7))
                    src = pt[:, :].rearrange("p (c q) -> p c q", q=128)
                    if half == 0:
                        tcopy("dve", XsT[:, 0:8, :], src, (pk,), (("XsT", 0),))
                    else:
                        act(XsT[:, 8:16, :], src, AF.Copy, (pk,), (("XsT", 1),))
                for hf in range(2):
                    u = 2 * s_ + hf
                    sl = u % NSL
                    if u + 1 < 2 * NSLOT:
                        load_unit(u + 1)
                    for c in range(2):
                        tt("pool", wd[sl][:, c, :], wd[sl][:, c, :], g2_b[:], ALU.mult, ((f"wd{sl}", c), ("g2_b",)),
                           ((f"wd{sl}", c),))
                    mm_group(pq0[:, 0:256], ("pq0",), [(XsT[:, kc, :], wg[sl][:, kc, :],
                                                        (("XsT", 0), ("XsT", 1), (f"wg{sl}", kc // 8))) for kc in range(KC)])
                    mm_group(pq1[:, 0:256], ("pq1",), [(XsT[:, kc, :], wu[sl][:, kc, :],
                                                        (("XsT", 0), ("XsT", 1), (f"wu{sl}", kc // 8))) for kc in range(KC)])
                    act(sgt[:], pq0[:, 0:256], AF.Silu, (("pq0",),), (("sgt",),))
                    tt("dve", hidh[:], sgt[:], pq1[:, 0:256], ALU.mult, (("sgt",), ("pq1",)), (("hidh",),))
                    for kf in range(2):
                        transpose(ptA[:, kf * 128:(kf + 1) * 128], ("ptA",), hidh[:, kf * 128:(kf + 1) * 128], ("hidh",),
                                  mark=(kf == 1))
                    tcopy("dve", hidT[:], ptA[:, 0:256].rearrange("p (c q) -> p c q", q=128), (("ptA",),), (("hidT",),))
                    for cg in range(4):
                        for kf in range(2):
                            first = (hf == 0 and kf == 0)
                            last = (hf == 1 and kf == 1)
                            mm(pbig[:, cg * 512:(cg + 1) * 512], hidT[:, kf, :], wd[sl][:, kf, cg * 512:(cg + 1) * 512],
                               first, last, (("hidT",), (f"wd{sl}", kf)),
                               (("pbig", cg),) if (first or last) else (), mark=last)
                for cg in range(4):
                    if cg % 2 == 0:
                        tcopy("dve", Ys[:, cg * 512:(cg + 1) * 512], pbig[:, cg * 512:(cg + 1) * 512], (("pbig", cg),),
                              (("Ys", 0),))
                    else:
                        act(Ys[:, cg * 512:(cg + 1) * 512], pbig[:, cg * 512:(cg + 1) * 512], AF.Copy, (("pbig", cg),),
                            (("Ys", 1),))
                dma("sp", "ysst", ys_scr[s_ * 128:(s_ + 1) * 128, :], Ys[:], (("Ys", 0), ("Ys", 1)), (YSK,))
            for i in range(NSL):
                S.free(f"wg{i}"); S.free(f"wu{i}"); S.free(f"wd{i}")
            for nm in ("Xs0", "Xs1", "XsT", "sgt", "hidh", "hidT", "Ys"):
                S.free(nm)
            yg = [S.alloc(f"yg{i}", [128, D], F32) for i in range(2)]
            gi = 0
            for b in range(NB):
                for (pi_, pk_, wt_, wk_) in ((pAi, ("pAi",), wA, ("wA",)), (pBi, ("pBi",), wB, ("wB",))):
                    g_ = gi % 2
                    gi += 1
                    S.dma("pool", f"yg{g_}",
                          lambda e, b=b, pi_=pi_, g_=g_: e.indirect_dma_start(
                              out=yg[g_][:], out_offset=None, in_=ys_scr[:, :],
                              in_offset=bass.IndirectOffsetOnAxis(ap=pi_[:, b:b + 1], axis=0)),
                          (YSK, pk_), ((f"yg{g_}",),))
                    stt(x1[:, b, :], yg[g_][:], wt_[:, b:b + 1], x1[:, b, :], ALU.mult, ALU.add,
                        ((f"yg{g_}",), wk_, ("x1", b)), (("x1", b),))
            for nm in ("yg0", "yg1", "pAi", "pBi", "wA", "wB", "eidi", "idxw"):
                S.free(nm)
        else:
            NSL = 3
            wg = [S.alloc(f"wg{i}", [128, KC, 256], BF16) for i in range(NSL)]
            wu = [S.alloc(f"wu{i}", [128, KC, 256], BF16) for i in range(NSL)]
            wd = [S.alloc(f"wd{i}", [128, 2, D], BF16) for i in range(NSL)]
            hid = [S.alloc(f"hid{i}", [128, 2, TOK], BF16) for i in range(2)]
            sgt = [S.alloc(f"sgt{i}", [128, 512], BF16) for i in range(2)]
            sgc = 0
            pqs = ((pq0, ("pq0",)), (pq1, ("pq1",)))
            pqc = 0
            def load_unit(u):
                e_, hf = u // 2, u % 2
                sl = u % NSL
                dma("pool", f"wg{sl}", wg[sl][:], w_gate[e_].rearrange("(kc p) n -> p kc n", p=128)[:, :, hf * 256:(hf + 1) * 256],
                    (), ((f"wg{sl}",),))
                dma("pool", f"wu{sl}", wu[sl][:], w_up[e_].rearrange("(kc p) n -> p kc n", p=128)[:, :, hf * 256:(hf + 1) * 256],
                    (), ((f"wu{sl}",),))
                dma("pool", f"wd{sl}", wd[sl][:], w_down[e_][hf * 256:(hf + 1) * 256, :].rearrange("(c p) n -> p c n", p=128),
                    (), ((f"wd{sl}",),))

            load_unit(0)
            for u in range(2 * NEXP):
                e_, hf = u // 2, u % 2
                sl = u % NSL
                hi = u % 2
                if u + 1 < 2 * NEXP:
                    load_unit(u + 1)
                for c in range(2):
                    tt("pool", wd[sl][:, c, :], wd[sl][:, c, :], g2_b[:], ALU.mult, ((f"wd{sl}",), ("g2_b",)), ((f"wd{sl}",),))
                for n in range(2):
                    for ffc in range(2):
                        bg, bgk = nbank()
                        mm_group(bg, bgk, [(wg[sl][:, kc, ffc * 128:(ffc + 1) * 128], h2T[:, kc, n * 512:(n + 1) * 512],
                                            ((f"wg{sl}",),) + k2(("h2T",))) for kc in range(KC)])
                        bu, buk = nbank()
                        mm_group(bu, buk, [(wu[sl][:, kc, ffc * 128:(ffc + 1) * 128], h2T[:, kc, n * 512:(n + 1) * 512],
                                            ((f"wu{sl}",),) + k2(("h2T",))) for kc in range(KC)])
                        si = sgc % 2
                        sgc += 1
                        act(sgt[si][:], bg, AF.Silu, (bgk,), ((f"sgt{si}",),))
                        tt("dve", hid[hi][:, ffc, n * 512:(n + 1) * 512], sgt[si][:], bu, ALU.mult,
                           ((f"sgt{si}",), buk), ((f"hid{hi}", n),))
                    for bb in range(4):
                        blk = n * 4 + bb
                        for cg in range(4):
                            pq, pqk = pqs[pqc % 2]
                            pqc += 1
                            mm_group(pq[:, :], pqk, [(hid[hi][:, c, blk * 128:(blk + 1) * 128], wd[sl][:, c, cg * 512:(cg + 1) * 512],
                                                      ((f"hid{hi}", n), (f"wd{sl}",))) for c in range(2)])
                            stt(x1[:, blk, cg * 512:(cg + 1) * 512], pq[:, :], comb[:, blk, e_:e_ + 1],
                                x1[:, blk, cg * 512:(cg + 1) * 512], ALU.mult, ALU.add,
                                (pqk, ("comb",), ("x1", blk)), (("x1", blk),))
            for i in range(NSL):
                S.free(f"wg{i}"); S.free(f"wu{i}"); S.free(f"wd{i}")
            S.free("hid0"); S.free("hid1"); S.free("sgt0"); S.free("sgt1"); S.free("h2T")

        lng2 = S.alloc("lng2", [128, D], F32)
        lnb2 = S.alloc("lnb2", [128, D], F32)
        dma("sp", "c_ln2g", lng2[:], ln2g_d[0:1, :].to_broadcast([128, D]), (), (("lng2",),))
        dma("sp", "c_ln2b", lnb2[:], ln2b_d[0:1, :].to_broadcast([128, D]), (), (("lnb2",),))
        for blk in range(NB):
            rstd, nmr, KR, KN = ln_stats(x1[:, blk, :], ("x1", blk), D)
            act(x1[:, blk, :], x1[:, blk, :], AF.Identity, (("x1", blk), KR, KN), (("x1", blk),),
                bias=nmr[:], scale=rstd[:])
            tt("pool", x1[:, blk, :], x1[:, blk, :], lng2[:], ALU.mult, (("x1", blk), ("lng2",)), (("x1", blk),))
            tt("dve", x1[:, blk, :], x1[:, blk, :], lnb2[:], ALU.add, (("x1", blk), ("lnb2",)), (("x1", blk),))
            dma("sp", "out", out_d[blk * 128:(blk + 1) * 128, :], x1[:, blk, :], (("x1", blk),), (("outd",),))

    try:
        body()
    except _Stop:
        pass
    S.wait_all("sp", list(S.dch.values()))

    import contextlib
    with contextlib.ExitStack() as es:
        for ch in list(S.chan.values()) + list(S.dch.values()):
            ch.sem = es.enter_context(nc.semaphore("s_" + ch.name))
        block = es.enter_context(nc.Block())

        def emit(name, e):
            if name == "pool" and SPARSE:
                BREG["r"] = e.alloc_register("bnd_reg")
                e.reg_mov(BREG["r"], NEXP * 512 - 1)
            for waits, fn, ch in S.ops[name]:
                for (c, v) in waits:
                    e.wait_ge(c.sem, v)
                if fn is None:
                    continue
                ins = fn(e)
                if ch is not None:
                    ins.then_inc(ch.sem, ch.inc)

        block.tensor(lambda e: emit("pe", e))
        block.scalar(lambda e: emit("act", e))
        block.vector(lambda e: emit("dve", e))
        block.gpsimd(lambda e: emit("pool", e))
        block.sync(lambda e: emit("sp", e))
    return nc, dbg_out, S


OWN = {0: [0, 3, 4, 7, 8, 11, 12, 15], 1: [1, 2, 5, 6, 9, 10, 13, 14]}


def make_in_maps(x, c, positions, w_ada, b_ada, w_in, q_norm_g, w_uq, kv_norm_g, w_ukv,
                 sgu_norm_g, sgu_norm_b, w_spatial, b_spatial, w_o, ln1_g, ln1_b,
                 w_router_group, b_router_group, w_router_expert, b_router_expert,
                 w_gate, w_up, w_down, ln2_g, ln2_b):
    f = lambda a: np.ascontiguousarray(np.asarray(a), dtype=np.float32)

    def relayout_gu(w):
        w = np.asarray(w, dtype=np.float32)
        if not SPARSE:
            return np.ascontiguousarray(w)
        E = w.shape[0]
        return np.ascontiguousarray(w.reshape(E, KC, 128, 2, 256).transpose(0, 3, 2, 1, 4)).reshape(E * 512, 2048)

    def relayout_d(w):
        w = np.asarray(w, dtype=np.float32)
        if not SPARSE:
            return np.ascontiguousarray(w)
        E = w.shape[0]
        return np.ascontiguousarray(w.reshape(E, 2, 2, 128, D).transpose(0, 1, 3, 2, 4)).reshape(E * 512, D)

    x = f(x); c = f(c)
    positions = np.asarray(positions).astype(np.int32)
    inv_freq = (1.0 / (10000.0 ** (np.arange(0, 64, 2, dtype=np.float32) / 64.0))).astype(np.float32)
    ropec = np.zeros((64, 2), np.float32)
    ropec[:, 0] = np.concatenate([inv_freq, inv_freq])
    ropec[:, 1] = np.concatenate([-np.ones(32, np.float32), np.ones(32, np.float32)])
    shared = {
        "ropec": ropec,
        "w_ada": f(w_ada[0]), "b_ada": f(b_ada[0]).reshape(1, -1), "w_in": f(w_in[0]),
        "qg": f(np.asarray(q_norm_g[0]).reshape(6, 128).T), "w_uq": f(w_uq[0]),
        "kvg": f(np.asarray(kv_norm_g[0]).reshape(4, 128).T), "w_ukv": f(w_ukv[0]),
        "sgug": f(sgu_norm_g[0]).reshape(1, -1), "sgub": f(sgu_norm_b[0]).reshape(1, -1),
        "wspT": f(np.asarray(w_spatial[0]).transpose(2, 0, 1)),
        "bsp": f(b_spatial[0]).reshape(1, -1),
        "w_o": f(w_o[0]), "ln1g": f(ln1_g[0]).reshape(1, -1), "ln1b": f(ln1_b[0]).reshape(1, -1),
        "wr": f(np.concatenate([np.asarray(w_router_group[0]), np.asarray(w_router_expert[0])], axis=1)),
        "br": f(np.concatenate([np.asarray(b_router_group[0]), np.asarray(b_router_expert[0])])).reshape(1, -1),
        "w_gate": relayout_gu(w_gate[0]), "w_up": relayout_gu(w_up[0]), "w_down": relayout_d(w_down[0]),
        "ln2g": f(ln2_g[0]).reshape(1, -1), "ln2b": f(ln2_b[0]).reshape(1, -1),
    }
    in_maps = []
    for core in range(8):
        b, p = core // 2, core % 2
        own, oth = OWN[p], OWN[1 - p]
        order = own + oth
        rows = np.concatenate([np.arange(k * 128, (k + 1) * 128) for k in order])
        m = dict(shared)
        m["x_loc"] = np.ascontiguousarray(x[b][rows])
        m["cT"] = np.ascontiguousarray(c[b].reshape(16, 128).T)
        m["pos"] = np.ascontiguousarray(positions[b][rows].reshape(1, -1))
        m["qidx"] = np.ascontiguousarray(
            (np.array(own, np.float32)[None, :] * 128 + np.arange(128, dtype=np.float32)[:, None]))
        m["oblk"] = np.ascontiguousarray(np.broadcast_to(np.array(oth, np.float32)[None, :] * 128, (128, 8)))
        in_maps.append(m)
    return in_maps


def assemble(results):
    out = np.zeros((4, 2048, 2048), np.float32)
    for core in range(8):
        b, p = core // 2, core % 2
        y = np.asarray(results[core]["out"])
        for j, k in enumerate(OWN[p]):
            out[b, k * 128:(k + 1) * 128] = y[j * 128:(j + 1) * 128]
    return out


def kernel(**inputs):
    nc, _, _ = build_nc()
    in_maps = make_in_maps(**inputs)
    res = run_bass_kernel_spmd(nc, in_maps, core_ids=list(range(8)))
    return assemble(res.results)
```

```python
import numpy as np
import concourse.bass as bass
import concourse.mybir as mybir
from concourse.bass_utils import run_bass_kernel_spmd

F32 = mybir.dt.float32
BF16 = mybir.dt.bfloat16
I32 = mybir.dt.int32
AF = mybir.ActivationFunctionType
ALU = mybir.AluOpType
AX = mybir.AxisListType

D = 2048
KC = 16
NB = 8
TOK = 1024
TL = 2048
H = 8
NEXP = 32
ALPHA = 2.0 ** 0.25
EPS = 1e-6
SM_SCALE = 192.0 ** -0.5
NEG = -30000.0
SBUF_BASE = 16640
PERSIST = 28672
SBUF_LIMIT = 229312

DEBUG = {}
SPARSE = True
PSUM_NAMES = ("pbig", "ptA", "ptB", "pq0", "pq1")


def dsize(dt):
    return {F32: 4, BF16: 2, I32: 4}[dt]


class Chan:
    def __init__(self, name, inc):
        self.name = name
        self.inc = inc
        self.count = 0
        self.sem = None


class Sched:
    ENG = ("pe", "act", "dve", "pool", "sp")

    def __init__(self, nc):
        self.nc = nc
        self.chan = {e: Chan(e, 1) for e in self.ENG}
        self.dch = {}
        self.ops = {e: [] for e in self.ENG}
        self.seen = {e: {} for e in self.ENG}
        self.last_w = {}
        self.readers = {}
        self.pending = {}
        self.allocs = []
        self.tomb = []
        self.tiles = {}
        self.peak = 0

    def alloc(self, name, shape, dt, base=SBUF_BASE + PERSIST, top=False):
        size = int(np.prod(shape[1:])) * dsize(dt)
        size = (size + 63) // 64 * 64
        self.allocs.sort()
        if top:
            off = SBUF_LIMIT // 64 * 64 - size
            for (o, s, _) in reversed(self.allocs):
                if o >= off + size:
                    continue
                if o + s <= off:
                    break
                off = o - size
            assert off >= base, f"SBUF overflow (top) allocating {name}"
        else:
            off = base
            for (o, s, _) in self.allocs:
                if o + s <= off:
                    continue
                if off + size <= o:
                    break
                off = o + s
        assert off + size <= SBUF_LIMIT, f"SBUF overflow allocating {name} size {size} at {off}"
        self.allocs.append((off, size, name))
        self.peak = max(self.peak, off + size)
        pend = {}
        keep = []
        for (o, s, deps) in self.tomb:
            if o < off + size and off < o + s:
                for ch, v in deps.items():
                    pend[ch] = max(pend.get(ch, 0), v)
            keep.append((o, s, deps))
        self.tomb = keep
        if pend:
            self.pending[name] = pend
        t = self.nc.alloc_sbuf_tensor_at(name, list(shape), dt, offset=off)
        self.tiles[name] = t
        return t

    def free(self, name):
        ent = [a for a in self.allocs if a[2] == name]
        assert len(ent) == 1, name
        self.allocs.remove(ent[0])
        deps = dict(self.pending.get(name, {}))
        for k, (ch, v) in self.last_w.items():
            if k[0] == name:
                deps[ch] = max(deps.get(ch, 0), v)
        for k, rd in self.readers.items():
            if k[0] == name:
                for ch, v in rd.items():
                    deps[ch] = max(deps.get(ch, 0), v)
        self.tomb.append((ent[0][0], ent[0][1], deps))

    def dchan(self, name):
        if name not in self.dch:
            self.dch[name] = Chan("d_" + name, 16)
        return self.dch[name]

    def _deps(self, eng, reads, writes):
        own = self.chan[eng]
        deps = {}

        def add(ch, v, raw):
            if ch is own and eng in ("pe", "sp"):
                return
            if deps.get(ch, 0) < v:
                deps[ch] = v

        for r in reads:
            lw = self.last_w.get(r)
            if lw:
                add(lw[0], lw[1], True)
            for ch, v in self.pending.get(r[0], {}).items():
                add(ch, v, True)
            if r[0] in PSUM_NAMES:
                for ch, v in self.readers.get(r, {}).items():
                    add(ch, v, False)
        for w in writes:
            lw = self.last_w.get(w)
            if lw:
                add(lw[0], lw[1], False)
            for ch, v in self.readers.get(w, {}).items():
                add(ch, v, False)
            for ch, v in self.pending.get(w[0], {}).items():
                add(ch, v, True)
        waits = []
        for ch, v in deps.items():
            if self.seen[eng].get(ch, 0) < v:
                waits.append((ch, v))
                self.seen[eng][ch] = v
        return waits

    def _record(self, ch, tick, reads, writes):
        for r in reads:
            rd = self.readers.setdefault(r, {})
            if rd.get(ch, 0) < tick:
                rd[ch] = tick
        for w in writes:
            self.last_w[w] = (ch, tick)
            self.readers[w] = {}

    def op(self, eng, fn, reads=(), writes=(), mark=True):
        waits = self._deps(eng, reads, writes)
        ch = self.chan[eng]
        if mark:
            ch.count += 1
            tick = ch.count
        else:
            tick = ch.count + 1
        self._record(ch, tick, reads, writes)
        self.ops[eng].append((waits, fn, ch if mark else None))

    def dma(self, queue, chname, fn, reads=(), writes=()):
        waits = self._deps(queue, reads, writes)
        ch = self.dchan(chname)
        ch.count += 16
        self._record(ch, ch.count, reads, writes)
        self.ops[queue].append((waits, fn, ch))

    def wait_all(self, eng, chans):
        waits = []
        for ch in chans:
            if ch.count > 0 and self.seen[eng].get(ch, 0) < ch.count:
                waits.append((ch, ch.count))
                self.seen[eng][ch] = ch.count
        self.ops[eng].append((waits, None, None))


class _Stop(Exception):
    pass


def build_nc(debug=(), stop=None, nexp_decl=NEXP):
    nc = bass.Bass("TRN2", target_bir_lowering=False)
    S = Sched(nc)
    BREG = {}

    def din(name, shape, dt=F32):
        return nc.dram_tensor(name, list(shape), dt, kind="ExternalInput").ap()

    x_loc = din("x_loc", [TL, D])
    cT_d = din("cT", [128, KC])
    pos_d = din("pos", [1, TL], I32)
    qidx_d = din("qidx", [128, NB])
    oblk_d = din("oblk", [128, NB])
    rope_d = din("ropec", [64, 2])
    w_ada = din("w_ada", [D, 6 * D])
    b_ada = din("b_ada", [1, 6 * D])
    w_in = din("w_in", [D, 3392])
    qg_d = din("qg", [128, 6])
    w_uq = din("w_uq", [768, 1536])
    kvg_d = din("kvg", [128, 4])
    w_ukv = din("w_ukv", [512, 2048])
    sgug_d = din("sgug", [1, 1024])
    sgub_d = din("sgub", [1, 1024])
    wspT_d = din("wspT", [128, 8, 128])
    bsp_d = din("bsp", [1, 1024])
    w_o = din("w_o", [D, D])
    ln1g_d = din("ln1g", [1, D])
    ln1b_d = din("ln1b", [1, D])
    wr_d = din("wr", [D, 36])
    br_d = din("br", [1, 36])
    if SPARSE:
        w_gate = din("w_gate", [nexp_decl * 512, 2048])
        w_up = din("w_up", [nexp_decl * 512, 2048])
        w_down = din("w_down", [nexp_decl * 512, D])
    else:
        w_gate = din("w_gate", [nexp_decl, D, 512])
        w_up = din("w_up", [nexp_decl, D, 512])
        w_down = din("w_down", [nexp_decl, 512, D])
    ln2g_d = din("ln2g", [1, D])
    ln2b_d = din("ln2b", [1, D])
    out_d = nc.dram_tensor("out", [TOK, D], F32, kind="ExternalOutput").ap()
    dbg_out = {}

    poff = [SBUF_BASE]

    def palloc(name, shape, dt):
        size = int(np.prod(shape[1:])) * dsize(dt)
        size = (size + 63) // 64 * 64
        t = nc.alloc_sbuf_tensor_at(name, list(shape), dt, offset=poff[0])
        poff[0] += size
        assert poff[0] <= SBUF_BASE + PERSIST
        return t

    ident = palloc("ident", [128, 128], BF16)
    ones_bf = palloc("ones_bf", [128, 128], BF16)
    onesf = palloc("onesf", [1, 128], F32)
    iot = palloc("iot", [128, 128], F32)
    tri = palloc("tri", [128, 128], F32)
    maskT = palloc("maskT", [128, 128], F32)
    cT = palloc("cT_sb", [128, KC], F32)
    cT_bf = palloc("cT_bf", [128, KC], BF16)
    modT = palloc("modT", [128, 64], F32)
    g1_b = palloc("g1_b", [128, D], F32)
    g2_b = palloc("g2_b", [128, D], F32)
    qidx = palloc("qidx_sb", [128, NB], F32)
    oblk = palloc("oblk_sb", [128, NB], F32)
    maskb = palloc("maskb", [128, NB, NB], F32)
    ropec = palloc("ropec_sb", [64, 2], F32)
    qg = palloc("qg_sb", [128, 6], F32)
    kvg = palloc("kvg_sb", [128, 4], F32)
    brb = palloc("brb", [128, 36], F32)
    logits = palloc("logits", [128, NB, 36], F32)
    comb = palloc("comb", [128, NB, 32], F32)
    st6s = [palloc(f"st6_{i}", [128, 4, 6], F32) for i in range(2)]
    mvs = [palloc(f"mv_{i}", [128, 2], F32) for i in range(2)]
    sds = [palloc(f"sd_{i}", [128, 1], F32) for i in range(2)]
    rstds = [palloc(f"rstd_{i}", [128, 1], F32) for i in range(2)]
    nmrs = [palloc(f"nmr_{i}", [128, 1], F32) for i in range(2)]
    lnc = [0]
    rs1 = palloc("rs1", [128, 1], F32)
    rs2 = palloc("rs2", [128, 1], F32)
    rs3 = palloc("rs3", [128, 1], F32)
    r_mg = palloc("r_mg", [128, NB], F32)
    r_sg = palloc("r_sg", [128, NB], F32)
    r_pg = palloc("r_pg", [128, NB], F32)
    r_eg = palloc("r_eg", [128, NB, 4], F32)
    r_oh = palloc("r_oh", [128, NB, 4], F32)
    r_t4 = palloc("r_t4", [128, NB, 4, 8], F32)
    r_sel = palloc("r_sel", [128, NB, 8], F32)
    r_l1 = palloc("r_l1", [128, NB], F32)
    r_l2 = palloc("r_l2", [128, NB], F32)
    r_oh1 = palloc("r_oh1", [128, NB, 8], F32)
    r_oh2 = palloc("r_oh2", [128, NB, 8], F32)
    r_msk = palloc("r_msk", [128, NB, 8], F32)
    r_w1 = palloc("r_w1", [128, NB], F32)
    r_w2 = palloc("r_w2", [128, NB], F32)
    r_ce = palloc("r_ce", [128, NB, 8], F32)

    pbig = nc.alloc_psum_tensor("pbig", [128, 2048], F32)
    ptA = nc.alloc_psum_tensor("ptA", [128, 1024], BF16)
    ptB = nc.alloc_psum_tensor("ptB", [128, 1024], BF16)
    pq0 = nc.alloc_psum_tensor("pq0", [128, 512], F32)
    pq1 = nc.alloc_psum_tensor("pq1", [128, 512], F32)

    def bank(i):
        return pbig[:, i * 512:(i + 1) * 512], ("pbig", i)

    def act(out, in_, func, reads, writes, bias=0.0, scale=1.0, accum=None):
        if accum is None:
            S.op("act", lambda e: e.activation(out=out, in_=in_, func=func, bias=bias, scale=scale),
                 reads, writes)
        else:
            S.op("act", lambda e: e.activation(out=out, in_=in_, func=func, bias=bias, scale=scale,
                                               accum_out=accum), reads, writes)

    def tscal(eng, out, in0, s1, s2, op0, op1, reads, writes):
        if s2 is None:
            S.op(eng, lambda e: e.tensor_scalar(out=out, in0=in0, scalar1=s1, scalar2=None, op0=op0),
                 reads, writes)
        else:
            S.op(eng, lambda e: e.tensor_scalar(out=out, in0=in0, scalar1=s1, scalar2=s2, op0=op0, op1=op1),
                 reads, writes)

    def tt(eng, out, in0, in1, op, reads, writes):
        S.op(eng, lambda e: e.tensor_tensor(out=out, in0=in0, in1=in1, op=op), reads, writes)

    def stt(out, in0, scalar, in1, op0, op1, reads, writes):
        S.op("dve", lambda e: e.scalar_tensor_tensor(out=out, in0=in0, scalar=scalar, in1=in1, op0=op0, op1=op1),
             reads, writes)

    def tcopy(eng, out, in_, reads, writes):
        S.op(eng, lambda e: e.tensor_copy(out=out, in_=in_), reads, writes)

    def mm(out, lhsT, rhs, start, stop, reads, writes=(), mark=False):
        S.op("pe", lambda e: e.matmul(out, lhsT=lhsT, rhs=rhs, start=start, stop=stop), reads, writes, mark=mark)

    def mm_group(out, okey, items):
        n = len(items)
        for i, (l, r, rk) in enumerate(items):
            first, last = (i == 0), (i == n - 1)
            mm(out, l, r, first, last, rk, writes=(okey,) if (first or last) else (), mark=last)

    def transpose(out, okey, in_, ikey, mark):
        S.op("pe", lambda e: e.transpose(out=out, in_=in_, identity=ident[:]), (ikey, ("ident",)),
             (okey,), mark=mark)

    def dma(queue, chname, out, in_, reads, writes):
        S.dma(queue, chname, lambda e: e.dma_start(out=out, in_=in_), reads, writes)

    def dbg(name, tile_ap, shape, dt, key):
        if name in debug:
            d = nc.dram_tensor("dbg_" + name, list(shape), dt, kind="ExternalOutput").ap()
            dbg_out[name] = d
            dma("sp", "dbg_" + name, d, tile_ap, (key,), ())

    def hk(blks):
        return tuple(("hT", b, par) for b in blks for par in (0, 1))

    def k2(base):
        return (base + (0,), base + (1,))

    def ckpt(name):
        if stop == name:
            raise _Stop()

    def evac(i, out, in_, reads, wkey):
        if i % 2 == 0:
            tcopy("dve", out, in_, reads, (wkey + (0,),))
        else:
            act(out, in_, AF.Copy, reads, (wkey + (1,),))

    def ln_stats(src, skey, n):
        k = lnc[0] % 2
        lnc[0] += 1
        st6, mv, sd, rstd, nmr = st6s[k], mvs[k], sds[k], rstds[k], nmrs[k]
        K6, KM, KS, KR, KN = (f"st6_{k}",), (f"mv_{k}",), (f"sd_{k}",), (f"rstd_{k}",), (f"nmr_{k}",)
        nch = n // 512
        for i in range(nch):
            S.op("dve", lambda e, i=i: e.bn_stats(out=st6[:, i, :], in_=src[:, i * 512:(i + 1) * 512]),
                 (skey,), (K6,))
        S.op("dve", lambda e: e.bn_aggr(out=mv[:], in_=st6[:, 0:nch, :]), (K6,), (KM,))
        act(sd[:], mv[:, 1:2], AF.Sqrt, (KM,), (KS,), bias=EPS, scale=1.0)
        S.op("dve", lambda e: e.reciprocal(out=rstd[:], in_=sd[:]), (KS,), (KR,))
        tscal("dve", nmr[:], mv[:, 0:1], rstd[:], -1.0, ALU.mult, ALU.mult, (KM, KR), (KN,))
        return rstd, nmr, KR, KN

    def body():
        S.op("pool", lambda e: e.iota(iot[:], pattern=[[1, 128]], base=0, channel_multiplier=-1,
                                      allow_small_or_imprecise_dtypes=True), (), (("iot",),))
        tscal("dve", ident[:], iot[:], 0.0, None, ALU.is_equal, None, (("iot",),), (("ident",),))
        tscal("dve", tri[:], iot[:], 0.0, NEG, ALU.is_gt, ALU.mult, (("iot",),), (("tri",),))
        tscal("dve", maskT[:], iot[:], 0.0, None, ALU.is_ge, None, (("iot",),), (("maskT",),))
        S.op("dve", lambda e: e.memset(ones_bf[:], 1.0), (), (("ones_bf",),))
        S.op("dve", lambda e: e.memset(onesf[:], 1.0), (), (("onesf",),))
        for (t, d_, nm) in ((cT, cT_d, "cT"), (qidx, qidx_d, "qidx"), (oblk, oblk_d, "oblk"), (ropec, rope_d, "ropec"),
                            (qg, qg_d, "qg"), (kvg, kvg_d, "kvg")):
            dma("sp", "c_" + nm, t[:], d_, (), ((nm,),))
        dma("sp", "c_brb", brb[:], br_d[0:1, :].to_broadcast([128, 36]), (), (("brb",),))
        tcopy("dve", cT_bf[:], cT[:], (("cT",),), (("cT_bf",),))
        for j in range(NB):
            tscal("dve", maskb[:, j, :], oblk[:], qidx[:, j:j + 1], NEG, ALU.is_gt, ALU.mult,
                  (("oblk",), ("qidx",)), (("maskb",),))

        cosT = S.alloc("cosT", [64, TL], BF16)
        sinT = S.alloc("sinT", [64, TL], BF16)
        posi = S.alloc("posi", [64, TL], I32)
        ang = S.alloc("ang", [64, TL], F32)
        tmpa = S.alloc("tmpa", [64, TL], F32)
        tmpi = S.alloc("tmpi", [64, TL], I32)
        dma("sp", "c_pos", posi[:], pos_d[0:1, :].to_broadcast([64, TL]), (), (("posi",),))
        tcopy("dve", ang[:], posi[:], (("posi",),), (("ang",),))
        tscal("dve", ang[:], ang[:], ropec[:, 0:1], None, ALU.mult, None, (("ang",), ("ropec",)), (("ang",),))
        TWO_PI = 2.0 * np.pi
        C1 = 6.28125
        C2 = TWO_PI - C1
        for which, shift, dst in (("sin", 0.0, sinT), ("cos", np.pi / 2, cosT)):
            tscal("dve", tmpa[:], ang[:], float(shift), None, ALU.add, None, (("ang",),), (("tmpa",),))
            tscal("dve", tmpi[:], tmpa[:], float(1.0 / TWO_PI), None, ALU.mult, None, (("tmpa",),), (("tmpi",),))
            kf = S.alloc("kf", [64, TL], F32)
            tcopy("dve", kf[:], tmpi[:], (("tmpi",),), (("kf",),))
            stt(tmpa[:], kf[:], -C1, tmpa[:], ALU.mult, ALU.add, (("kf",), ("tmpa",)), (("tmpa",),))
            stt(tmpa[:], kf[:], -C2, tmpa[:], ALU.mult, ALU.add, (("kf",), ("tmpa",)), (("tmpa",),))
            tscal("dve", kf[:], tmpa[:], float(np.pi), -TWO_PI, ALU.is_gt, ALU.mult, (("tmpa",),), (("kf",),))
            tt("dve", tmpa[:], tmpa[:], kf[:], ALU.add, (("tmpa",), ("kf",)), (("tmpa",),))
            tscal("dve", kf[:], tmpa[:], float(-np.pi), TWO_PI, ALU.is_lt, ALU.mult, (("tmpa",),), (("kf",),))
            tt("dve", tmpa[:], tmpa[:], kf[:], ALU.add, (("tmpa",), ("kf",)), (("tmpa",),))
            tscal("dve", tmpa[:], tmpa[:], float(np.pi), float(-np.pi), ALU.min, ALU.max, (("tmpa",),), (("tmpa",),))
            if which == "sin":
                act(kf[:], tmpa[:], AF.Sin, (("tmpa",),), (("kf",),))
                tscal("dve", dst[:], kf[:], ropec[:, 1:2], None, ALU.mult, None, (("kf",), ("ropec",)), ((which + "T",),))
            else:
                act(dst[:], tmpa[:], AF.Sin, (("tmpa",),), ((which + "T",),))
            S.free("kf")
        for nm in ("posi", "ang", "tmpa", "tmpi"):
            S.free(nm)
        dbg("cosT", cosT[:], [64, TL], BF16, ("cosT",))
        dbg("sinT", sinT[:], [64, TL], BF16, ("sinT",))
        dbg("maskb", maskb[:], [128, NB, NB], F32, ("maskb",))
        ckpt("const")
        RCOS, RSIN = ("cosT",), ("sinT",)

        wa = [S.alloc(f"wa{i}", [128, KC, 512], BF16) for i in range(2)]
        brow = [S.alloc(f"brow{i}", [1, 512], F32) for i in range(2)]
        rowsb = [S.alloc(f"rowsb{i}", [1, 512], F32) for i in range(2)]
        w_ada_v = w_ada.rearrange("(kc p) n -> p kc n", p=128)
        fm_slot = {0: 0, 1: 1, 3: 2, 4: 3}
        for j in range(8):
            sl = j % 2
            v, q4 = j // 4, j % 4
            dma("pool", f"wa{sl}", wa[sl][:], w_ada_v[:, :, j * 512:(j + 1) * 512], (), ((f"wa{sl}",),))
            dma("sp", f"brow{sl}", brow[sl][:], b_ada[0:1, j * 512:(j + 1) * 512], (), ((f"brow{sl}",),))
            mm_group(pq0[0:1, :], ("pq0",),
                     [(cT_bf[:, kc:kc + 1], wa[sl][:, kc, :], (("cT_bf",), (f"wa{sl}",))) for kc in range(KC)])
            tt("dve", rowsb[sl][:], pq0[0:1, :], brow[sl][:], ALU.add, (("pq0",), (f"brow{sl}",)), ((f"rowsb{sl}",),))
            base = fm_slot[v] * 16 + q4 * 4
            for q in range(4):
                mm(pq1[:, base + q:base + q + 1], rowsb[sl][0:1, q * 128:(q + 1) * 128], onesf[0:1, 0:1],
                   True, True, ((f"rowsb{sl}",), ("onesf",)), (("pq1",),), mark=True)
        tcopy("dve", modT[:, 0:32], pq1[:, 0:32], (("pq1",),), (("modT", 1),))
        tscal("dve", modT[:, 16:32], modT[:, 16:32], 1.0, None, ALU.add, None, (("modT", 1),), (("modT", 1),))
        for nm in ("wa0", "wa1", "brow0", "brow1", "rowsb0", "rowsb1"):
            S.free(nm)

        DCW = 256
        defer = {"c": 0}

        def mod_chunk_def(wa2, brow2, rowsb2):
            c = defer["c"]
            if c >= (4 * D) // DCW:
                return
            defer["c"] += 1
            sl = c % 2
            col0 = 2 * D + c * DCW
            v, off = col0 // D, col0 % D
            dma("pool", f"wb{sl}", wa2[sl][:], w_ada_v[:, :, col0:col0 + DCW], (), ((f"wb{sl}",),))
            dma("sp", f"brow2{sl}", brow2[sl][:], b_ada[0:1, col0:col0 + DCW], (), ((f"browb{sl}",),))
            mm_group(pq0[0:1, 0:DCW], ("pq0",),
                     [(cT_bf[:, kc:kc + 1], wa2[sl][:, kc, :], (("cT_bf",), (f"wb{sl}",))) for kc in range(KC)])
            tt("dve", rowsb2[sl][:], pq0[0:1, 0:DCW], brow2[sl][:], ALU.add, (("pq0",), (f"browb{sl}",)),
               ((f"rowsbb{sl}",),))
            if v in (3, 4):
                base = fm_slot[v] * 16 + off // 128
                for q in range(2):
                    mm(pq0[:, 256 + q:257 + q], rowsb2[sl][0:1, q * 128:(q + 1) * 128], onesf[0:1, 0:1],
                       True, True, ((f"rowsbb{sl}",), ("onesf",)), (("pq0",),), mark=True)
                tscal("dve", modT[:, base:base + 2], pq0[:, 256:258], 1.0 if v == 4 else 0.0, None, ALU.add, None,
                      (("pq0",),), (("modT", 2),))
            else:
                gb = g1_b if v == 2 else g2_b
                gk = ("g1_b",) if v == 2 else ("g2_b",)
                mm(pq0[:, 256:512], onesf[0:1, :], rowsb2[sl][:], True, True, ((f"rowsbb{sl}",), ("onesf",)),
                   (("pq0",),), mark=True)
                tcopy("dve", gb[:, off:off + DCW], pq0[:, 256:512], (("pq0",),), (gk,))

        ckpt("A")

        xs = [S.alloc(f"xs{i}", [128, D], F32) for i in range(2)]
        xn = [S.alloc(f"xn{i}", [128, D], BF16) for i in range(2)]
        XN = [xn]
        cnt = {"ln": 0, "ev": 0}

        def ln_mod_T(src, skey, dst_fn, dkey, moff):
            i = cnt["ln"] % 2
            cnt["ln"] += 1
            rstd, nmr, KR, KN = ln_stats(src, skey, D)
            xn = XN[0]
            act(xn[i][:], src, AF.Identity, (skey, KR, KN), ((f"xn{i}",),), bias=nmr[:], scale=rstd[:])
            for half, (pt, pk) in enumerate(((ptA, ("ptA",)), (ptB, ("ptB",)))):
                for q in range(8):
                    kc = half * 8 + q
                    transpose(pt[:, q * 128:(q + 1) * 128], pk, xn[i][:, kc * 128:(kc + 1) * 128], (f"xn{i}",),
                              mark=(q == 7))
                for q in range(8):
                    kc = half * 8 + q
                    sc = modT[:, moff + 16 + kc:moff + 17 + kc]
                    sh = modT[:, moff + kc:moff + kc + 1]
                    if half == 0:
                        tscal("dve", dst_fn(kc), pt[:, q * 128:(q + 1) * 128], sc, sh, ALU.mult, ALU.add,
                              (pk, ("modT", 1 if moff == 0 else 2)), (dkey + (0,),))
                    else:
                        act(dst_fn(kc), pt[:, q * 128:(q + 1) * 128], AF.Identity, (pk, ("modT", 1 if moff == 0 else 2)), (dkey + (1,),),
                            bias=sh, scale=sc)

        hT = S.alloc("hT", [128, KC, TOK], BF16)
        for blk in range(NB):
            sl = blk % 2
            dma("sp", f"xs{sl}", xs[sl][:], x_loc[blk * 128:(blk + 1) * 128, :], (), ((f"xs{sl}",),))
            ln_mod_T(xs[sl][:], (f"xs{sl}",), lambda kc, blk=blk: hT[:, kc, blk * 128:(blk + 1) * 128], ("hT", blk), 0)
        dbg("hT", hT[:], [128, KC, TOK], BF16, ("hT", 7, 1))
        ckpt("hT")
        for nm in ("xs0", "xs1", "xn0", "xn1"):
            S.free(nm)

        wi = [S.alloc(f"wi{i}", [128, KC, 512], BF16) for i in range(2)]
        w_in_v = w_in.rearrange("(kc p) n -> p kc n", p=128)
        wcnt = [0]

        def load_win(c0, ncol):
            sl = wcnt[0] % 2
            wcnt[0] += 1
            dma("pool", f"wi{sl}", wi[sl][:, :, 0:ncol], w_in_v[:, :, c0:c0 + ncol], (), ((f"wi{sl}",),))
            return wi[sl], (f"wi{sl}",)

        bcnt = [0]

        def nbank():
            b = bcnt[0] % 4
            bcnt[0] += 1
            return bank(b)

        mixT = S.alloc("mixT", [128, 16, TOK], BF16, top=True)
        uT = S.alloc("uT", [128, 8, TOK], BF16)
        vsg = S.alloc("vsg", [128, NB, 1024], BF16)
        sgug_b = S.alloc("sgug_b", [128, 1024], F32)
        sgub_b = S.alloc("sgub_b", [128, 1024], F32)
        bsp_b = S.alloc("bsp_b", [128, 1024], F32)
        wsT = S.alloc("wsT", [128, 8, 128], BF16)
        wsT_f = S.alloc("wsT_f", [128, 8, 128], F32)
        dma("sp", "c_sgug", sgug_b[:], sgug_d[0:1, :].to_broadcast([128, 1024]), (), (("sgug_b",),))
        dma("sp", "c_sgub", sgub_b[:], sgub_d[0:1, :].to_broadcast([128, 1024]), (), (("sgub_b",),))
        dma("sp", "c_bsp", bsp_b[:], bsp_d[0:1, :].to_broadcast([128, 1024]), (), (("bsp_b",),))
        dma("sp", "c_wsT", wsT_f[:], wspT_d, (), (("wsT_f",),))
        tt("dve", wsT[:], wsT_f[:], maskT[:].unsqueeze(1).to_broadcast([128, 8, 128]), ALU.mult,
           (("wsT_f",), ("maskT",)), (("wsT",),))
        S.free("wsT_f")
        ckpt("B3a")

        def gelu_evac(out, bk, bkey, wkey):
            act(out, bk, AF.Gelu, (bkey,), (wkey,))

        for ch in range(2):
            wt, wk = load_win(1344 + ch * 512, 512)
            for m in range(4):
                for n in range(2):
                    bk, bkey = nbank()
                    mm_group(bk, bkey, [(wt[:, kc, m * 128:(m + 1) * 128], hT[:, kc, n * 512:(n + 1) * 512],
                                         (wk,) + hk(range(4 * n, 4 * n + 4))) for kc in range(KC)])
                    gelu_evac(uT[:, ch * 4 + m, n * 512:(n + 1) * 512], bk, bkey, ("uT",))
        ckpt("B3b")
        for ch in range(2):
            wt, wk = load_win(2368 + ch * 512, 512)
            for blk in range(NB):
                bk, bkey = nbank()
                mm_group(bk, bkey, [(hT[:, kc, blk * 128:(blk + 1) * 128], wt[:, kc, 0:512],
                                     (wk,) + hk([blk])) for kc in range(KC)])
                gelu_evac(vsg[:, blk, ch * 512:(ch + 1) * 512], bk, bkey, ("vsg", blk))
        dbg("uT", uT[:], [128, 8, TOK], BF16, ("uT",))
        dbg("vsg", vsg[:], [128, NB, 1024], BF16, ("vsg", 0))
        ckpt("B3c")
        vtmp = [S.alloc(f"vtmp{i}", [128, 1024], F32) for i in range(2)]
        vsn = [S.alloc(f"vsn{i}", [128, 1024], BF16) for i in range(2)]
        for blk in range(NB):
            i = blk % 2
            rstd, nmr, KR, KN = ln_stats(vsg[:, blk, :], ("vsg", blk), 1024)
            act(vtmp[i][:], vsg[:, blk, :], AF.Identity, (("vsg", blk), KR, KN), ((f"vtmp{i}",),),
                bias=nmr[:], scale=rstd[:])
            tt("dve", vtmp[i][:], vtmp[i][:], sgug_b[:], ALU.mult, ((f"vtmp{i}",), ("sgug_b",)), ((f"vtmp{i}",),))
            tt("dve", vsn[i][:], vtmp[i][:], sgub_b[:], ALU.add, ((f"vtmp{i}",), ("sgub_b",)), ((f"vsn{i}",),))
            for half in range(2):
                bk, bkey = nbank()
                for gg in range(4):
                    g = half * 4 + gg
                    mm(bk[:, gg * 128:(gg + 1) * 128], vsn[i][:, g * 128:(g + 1) * 128], wsT[:, g, :], True, True,
                       ((f"vsn{i}",), ("wsT",)), (bkey,), mark=(gg == 3))
                tt("dve", vtmp[i][:, half * 512:(half + 1) * 512], bk, bsp_b[:, half * 512:(half + 1) * 512], ALU.add,
                   (bkey, ("bsp_b",)), ((f"vtmp{i}",),))
                tt("dve", mixT[:, 8 + half * 4:12 + half * 4, blk * 128:(blk + 1) * 128],
                   vtmp[i][:, half * 512:(half + 1) * 512].rearrange("p (g t) -> p g t", g=4),
                   uT[:, half * 4:half * 4 + 4, blk * 128:(blk + 1) * 128], ALU.mult,
                   ((f"vtmp{i}",), ("uT",)), (("mixT", "s"),))
        for nm in ("uT", "vsg", "sgug_b", "sgub_b", "bsp_b", "wsT", "vtmp0", "vtmp1", "vsn0", "vsn1"):
            S.free(nm)
        dbg("sguT", mixT[:, 8:16, :], [128, 8, TOK], BF16, ("mixT", "s"))
        ckpt("B3")

        sqt = [S.alloc(f"sqt{i}", [128, 512], BF16) for i in range(2)]
        rsb = S.alloc("rsb", [128, 512], F32)
        rt1 = S.alloc("rt1", [64, 512], F32)
        rt2 = S.alloc("rt2", [64, 512], F32)
        sqc = [0]

        def rms_feature_major(dstT, dkey, nchunk, wt_fn, src_fn, skeys, gain, ntot, gkey):
            for m in range(nchunk):
                bk, bkey = nbank()
                wt, wk, c0 = wt_fn(m)
                mm_group(bk, bkey, [(wt[:, kc, c0:c0 + 128], src_fn(kc), (wk,) + skeys) for kc in range(KC)])
                tcopy("dve", dstT[:, m, :], bk, (bkey,), (dkey,))
                i = sqc[0] % 2
                sqc[0] += 1
                act(sqt[i][:], bk, AF.Square, (bkey,), ((f"sqt{i}",),))
                mm(pq0[:, :], ones_bf[:], sqt[i][:], m == 0, m == nchunk - 1, ((f"sqt{i}",), ("ones_bf",)),
                   (("pq0",),) if (m == 0 or m == nchunk - 1) else (), mark=(m == nchunk - 1))
            act(rsb[:], pq0[:, :], AF.Sqrt, (("pq0",),), (("rsb",),), bias=EPS, scale=1.0 / ntot)
            S.op("dve", lambda e: e.reciprocal(out=rsb[:], in_=rsb[:]), (("rsb",),), (("rsb",),))
            for m in range(nchunk):
                stt(dstT[:, m, :], dstT[:, m, :], gain[:, m:m + 1], rsb[:], ALU.mult, ALU.mult,
                    (dkey, ("rsb",), gkey), (dkey,))

        def rope_evac(pe_ps, pe_key, sw_ps, sw_key, tok0, dst, dkey):
            tt("dve", rt1[:], pe_ps, cosT[:, tok0:tok0 + 512], ALU.mult, (pe_key, RCOS), (("rt1",),))
            tt("dve", rt2[:], sw_ps, sinT[:, tok0:tok0 + 512], ALU.mult, (sw_key, RSIN), (("rt2",),))
            tt("dve", dst, rt1[:], rt2[:], ALU.add, (("rt1",), ("rt2",)), (dkey,))

        qnT = S.alloc("qnT", [128, H, TOK], BF16, top=True)
        qrT = S.alloc("qrT", [64, H, TOK], BF16, top=True)
        cqT = S.alloc("cqT", [128, 6, TOK], BF16)
        wqn = S.alloc("wqn", [128, 6, H, 128], BF16)
        wqp = S.alloc("wqp", [128, 6, H, 96], BF16)
        w_uq_v = w_uq.rearrange("(kc p) (h d) -> p kc h d", p=128, d=192)
        for kc in range(6):
            dma("pool", "wq", wqn[:, kc, :, :], w_uq_v[:, kc, :, 0:128], (), (("wqn", kc), ("wqn", "ser")))
            dma("pool", "wq", wqp[:, kc, :, 0:64], w_uq_v[:, kc, :, 128:192], (), (("wqp", kc, 0), ("wqn", "ser")))
            dma("pool", "wq", wqp[:, kc, :, 64:96], w_uq_v[:, kc, :, 128:160], (), (("wqp", kc, 1), ("wqn", "ser")))
        wA, wAk = load_win(0, 512)
        wB, wBk = load_win(512, 256)
        for n in range(2):
            rms_feature_major(cqT[:, :, n * 512:(n + 1) * 512], ("cqT", n), 6,
                              lambda m: (wA, wAk, m * 128) if m < 4 else (wB, wBk, (m - 4) * 128),
                              lambda kc, n=n: hT[:, kc, n * 512:(n + 1) * 512], hk(range(4 * n, 4 * n + 4)), qg, 768.0, ("qg",))
        for n in range(2):
            for h in range(H):
                bk, bkey = nbank()
                mm_group(bk, bkey, [(wqn[:, kc, h, :], cqT[:, kc, n * 512:(n + 1) * 512], (("wqn", kc), ("cqT", n)))
                                    for kc in range(6)])
                evac(h, qnT[:, h, n * 512:(n + 1) * 512], bk, (bkey,), ("qnT",))
                b1, b1k = nbank()
                mm_group(b1[0:64, :], b1k, [(wqp[:, kc, h, 0:64], cqT[:, kc, n * 512:(n + 1) * 512],
                                             (("wqp", kc, 0), ("wqp", kc, 1), ("cqT", n))) for kc in range(6)])
                b2, b2k = nbank()
                mm_group(b2[0:64, :], b2k, [(wqp[:, kc, h, 32:96], cqT[:, kc, n * 512:(n + 1) * 512],
                                             (("wqp", kc, 0), ("wqp", kc, 1), ("cqT", n))) for kc in range(6)])
                rope_evac(b1[0:64, :], b1k, b2[0:64, :], b2k, n * 512, qrT[:, h, n * 512:(n + 1) * 512], ("qrT",))
        for nm in ("cqT", "wqn", "wqp"):
            S.free(nm)
        dbg("qnT", qnT[:], [128, H, TOK], BF16, ("qnT", 1))
        dbg("qrT", qrT[:], [64, H, TOK], BF16, ("qrT",))
        ckpt("B2")

        ckvT = S.alloc("ckvT", [128, 4, TL], BF16, top=True)
        krT = S.alloc("krT", [64, TL], BF16, top=True)
        wkp = S.alloc("wkp", [128, KC, 96], BF16, top=True)
        wC, wCk = load_win(768, 512)
        dma("pool", "wkpa", wkp[:, :, 0:64], w_in_v[:, :, 1280:1344], (), (("wkp", 0),))
        dma("pool", "wkpb", wkp[:, :, 64:96], w_in_v[:, :, 1280:1312], (), (("wkp", 1),))

        def kv_group(n, src_fn, skeys):
            rms_feature_major(ckvT[:, :, n * 512:(n + 1) * 512], ("ckvT", n), 4,
                              lambda m: (wC, wCk, m * 128), src_fn, skeys, kvg, 512.0, ("kvg",))
            b1, b1k = nbank()
            mm_group(b1[0:64, :], b1k, [(wkp[:, kc, 0:64], src_fn(kc), (("wkp", 0), ("wkp", 1)) + skeys) for kc in range(KC)])
            b2, b2k = nbank()
            mm_group(b2[0:64, :], b2k, [(wkp[:, kc, 32:96], src_fn(kc), (("wkp", 0), ("wkp", 1)) + skeys) for kc in range(KC)])
            rope_evac(b1[0:64, :], b1k, b2[0:64, :], b2k, n * 512, krT[:, n * 512:(n + 1) * 512], ("krT",))

        for n in range(2):
            kv_group(n, lambda kc, n=n: hT[:, kc, n * 512:(n + 1) * 512], hk(range(4 * n, 4 * n + 4)))
        S.free("hT")
        S.free("wi0" if wCk == ("wi1",) else "wi1")
        xs = [S.alloc(f"xs{i}", [128, D], F32) for i in range(2)]
        xn = [S.alloc(f"xn{i}", [128, D], BF16) for i in range(2)]
        XN[0] = xn
        hTo = [S.alloc(f"hTo{i}", [128, KC, 512], BF16) for i in range(2)]
        for n in range(2, 4):
            i = n % 2
            for bb in range(4):
                blk = n * 4 + bb
                sl = blk % 2
                dma("sp", f"xs{sl}", xs[sl][:], x_loc[blk * 128:(blk + 1) * 128, :], (), ((f"xs{sl}",),))
                ln_mod_T(xs[sl][:], (f"xs{sl}",), lambda kc, i=i, bb=bb: hTo[i][:, kc, bb * 128:(bb + 1) * 128],
                         (f"hTo{i}",), 0)
            kv_group(n, lambda kc, i=i: hTo[i][:, kc, :], k2((f"hTo{i}",)))
        for nm in ("hTo0", "hTo1", wCk[0], "wkp", "xs0", "xs1", "xn0", "xn1",
                   "sqt0", "sqt1", "rsb", "rt1", "rt2", "cosT", "sinT"):
            S.free(nm)
        dbg("ckvT", ckvT[:], [128, 4, TL], BF16, ("ckvT", 0))
        dbg("krT", krT[:], [64, TL], BF16, ("krT",))
        ckpt("B1b")

        wkk = S.alloc("wkk", [128, 4, H, 128], BF16)
        wvv = S.alloc("wvv", [128, 4, H, 128], BF16)
        w_ukv_v = w_ukv.rearrange("(kc p) (h two d) -> p kc h two d", p=128, two=2, d=128)
        for kc in range(4):
            dma("pool", "wkv", wkk[:, kc, :, :], w_ukv_v[:, kc, :, 0, :], (), (("wkk", kc), ("wkk", "ser")))
            dma("pool", "wkv", wvv[:, kc, :, :], w_ukv_v[:, kc, :, 1, :], (), (("wvv", kc), ("wkk", "ser")))
        knT = S.alloc("knT", [128, H, TL], BF16, top=True)
        V = S.alloc("V", [128, 16, H * 128], BF16, top=True)
        ec = 0
        for n in range(4):
            for h in range(H):
                bk, bkey = nbank()
                mm_group(bk, bkey, [(wkk[:, kc, h, :], ckvT[:, kc, n * 512:(n + 1) * 512], (("wkk", kc), ("ckvT", n)))
                                    for kc in range(4)])
                evac(ec, knT[:, h, n * 512:(n + 1) * 512], bk, (bkey,), ("knT",))
                ec += 1
            for bb in range(4):
                lb = n * 4 + bb
                for g in range(2):
                    bk, bkey = nbank()
                    mm_group(bk, bkey, [(ckvT[:, kc, lb * 128:(lb + 1) * 128],
                                         wvv[:, kc, g * 4:(g + 1) * 4, :], (("wvv", kc), ("ckvT", n))) for kc in range(4)])
                    evac(ec, V[:, lb, g * 512:(g + 1) * 512], bk, (bkey,), ("V",))
                    ec += 1
        for nm in ("ckvT", "wkk", "wvv"):
            S.free(nm)
        dbg("knT", knT[:], [128, H, TL], BF16, ("knT", 1))
        dbg("V", V[:], [128, 16, H * 128], BF16, ("V", 1))
        ckpt("B1c")

        Pm = [S.alloc(f"Pm{i}", [128, TL], BF16) for i in range(2)]
        PT = [S.alloc(f"PT{i}", [128, 16, 128], BF16) for i in range(2)]
        attn = [S.alloc(f"attn{i}", [128, H * 128], BF16) for i in range(2)]
        mx = S.alloc("mx", [128, 1], F32)
        nb_ = S.alloc("nb_", [128, 1], F32)
        rsum = S.alloc("rsum", [128, 1], F32)
        rinv = S.alloc("rinv", [128, 1], F32)
        wa2 = [S.alloc(f"wb{i}", [128, KC, DCW], BF16) for i in range(2)]
        brow2 = [S.alloc(f"browb{i}", [1, DCW], F32) for i in range(2)]
        rowsb2 = [S.alloc(f"rowsbb{i}", [1, DCW], F32) for i in range(2)]
        it = 0
        for j in range(NB):
            nk = j + 1
            W = nk * 128
            ai = j % 2
            for h in range(H):
                pi = it % 2
                it += 1
                if it % 2 == 0:
                    mod_chunk_def(wa2, brow2, rowsb2)
                segs = []
                for side in range(2):
                    k0 = side * 1024
                    c = 0
                    while c < W:
                        w_ = min(512 - ((side * W + c) % 512), W - c)
                        segs.append((side * W + c, k0 + c, w_))
                        c += w_
                for (col, key0, w_) in segs:
                    bnk = col // 512
                    assert (col + w_ - 1) // 512 == bnk
                    mm(pbig[:, col:col + w_], qnT[:, h, j * 128:(j + 1) * 128], knT[:, h, key0:key0 + w_], True, False,
                       k2(("qnT",)) + k2(("knT",)), (("pbig", bnk),), mark=False)
                    mm(pbig[:, col:col + w_], qrT[:, h, j * 128:(j + 1) * 128], krT[:, key0:key0 + w_], False, True,
                       (("qrT",), ("krT",)), (("pbig", bnk),), mark=True)
                banks = tuple(("pbig", b) for b in range((2 * W + 511) // 512))
                dcol = j * 128
                tt("dve", pbig[:, dcol:dcol + 128], pbig[:, dcol:dcol + 128], tri[:], ALU.add,
                   (("pbig", dcol // 512), ("tri",)), (("pbig", dcol // 512),))
                tt("dve", pbig[:, W:2 * W].rearrange("p (b k) -> p b k", k=128),
                   pbig[:, W:2 * W].rearrange("p (b k) -> p b k", k=128),
                   maskb[:, j, 0:nk].unsqueeze(2).to_broadcast([128, nk, 128]), ALU.add,
                   banks + (("maskb",),), banks)
                S.op("dve", lambda e, W=W: e.reduce_max(out=mx[:], in_=pbig[:, 0:2 * W], axis=AX.X), banks, (("mx",),))
                tscal("dve", nb_[:], mx[:], -SM_SCALE, None, ALU.mult, None, (("mx",),), (("nb_",),))
                act(Pm[pi][:, 0:2 * W], pbig[:, 0:2 * W], AF.Exp, banks + (("nb_",),), ((f"Pm{pi}",), ("rsum",)),
                    bias=nb_[:], scale=SM_SCALE, accum=rsum[:])
                S.op("dve", lambda e: e.reciprocal(out=rinv[:], in_=rsum[:]), (("rsum",),), (("rinv",),))
                nblk = 2 * nk
                for bi, b0 in enumerate(range(0, nblk, 8)):
                    pt, pk = (ptA, ("ptA",)) if bi % 2 == 0 else (ptB, ("ptB",))
                    nb8 = min(8, nblk - b0)
                    for q in range(nb8):
                        transpose(pt[:, q * 128:(q + 1) * 128], pk, Pm[pi][:, (b0 + q) * 128:(b0 + q + 1) * 128],
                                  (f"Pm{pi}",), mark=(q == nb8 - 1))
                    src = pt[:, 0:nb8 * 128].rearrange("p (b q) -> p b q", q=128)
                    if bi % 2 == 0:
                        tcopy("dve", PT[pi][:, b0:b0 + nb8, :], src, (pk,), ((f"PT{pi}",),))
                    else:
                        act(PT[pi][:, b0:b0 + nb8, :], src, AF.Copy, (pk,), ((f"PT{pi}",),))
                items = []
                for b in range(nblk):
                    lb = b if b < nk else 8 + (b - nk)
                    items.append((PT[pi][:, b, :], V[:, lb, h * 128:(h + 1) * 128], ((f"PT{pi}",),) + k2(("V",))))
                mm_group(pq1[:, 0:128], ("pq1",), items)
                tscal("dve", attn[ai][:, h * 128:(h + 1) * 128], pq1[:, 0:128], rinv[:], None, ALU.mult, None,
                      (("pq1",), ("rinv",)), ((f"attn{ai}",),))
            for q in range(8):
                transpose(ptA[:, q * 128:(q + 1) * 128], ("ptA",), attn[ai][:, q * 128:(q + 1) * 128], (f"attn{ai}",),
                          mark=(q == 7))
            tcopy("dve", mixT[:, 0:8, j * 128:(j + 1) * 128], ptA[:, :].rearrange("p (c q) -> p c q", q=128),
                  (("ptA",),), (("mixT", "a"),))
        while defer["c"] < (4 * D) // DCW:
            mod_chunk_def(wa2, brow2, rowsb2)
        for nm in ("wb0", "wb1", "browb0", "browb1", "rowsbb0", "rowsbb1"):
            S.free(nm)
        for nm in ("Pm0", "Pm1", "PT0", "PT1", "attn0", "attn1", "mx", "nb_", "rsum", "rinv",
                   "qnT", "qrT", "knT", "krT", "V"):
            S.free(nm)
        dbg("mixT", mixT[:], [128, 16, TOK], BF16, ("mixT", "a"))
        ckpt("C")

        x1 = S.alloc("x1", [128, NB, D], F32, top=True)
        xs = [S.alloc(f"xs{i}", [128, D], F32) for i in range(2)]
        wo = [S.alloc(f"wo{i}", [128, KC, 512], BF16) for i in range(2)]
        lng = S.alloc("lng", [128, D], F32)
        lnb = S.alloc("lnb", [128, D], F32)
        dma("sp", "c_ln1g", lng[:], ln1g_d[0:1, :].to_broadcast([128, D]), (), (("lng",),))
        dma("sp", "c_ln1b", lnb[:], ln1b_d[0:1, :].to_broadcast([128, D]), (), (("lnb",),))
        w_o_v = w_o.rearrange("(kc p) n -> p kc n", p=128)
        for cg in range(4):
            sl = cg % 2
            dma("pool", f"wo{sl}", wo[sl][:], w_o_v[:, :, cg * 512:(cg + 1) * 512], (), ((f"wo{sl}",),))
            for blk in range(NB):
                bk, bkey = nbank()
                mm_group(bk, bkey, [(mixT[:, kc, blk * 128:(blk + 1) * 128], wo[sl][:, kc, :],
                                     ((f"wo{sl}",), ("mixT", "a"), ("mixT", "s"))) for kc in range(KC)])
                tt("dve", x1[:, blk, cg * 512:(cg + 1) * 512], bk, g1_b[:, cg * 512:(cg + 1) * 512], ALU.mult,
                   (bkey, ("g1_b",)), (("x1", blk),))
        for blk in range(NB):
            sl = blk % 2
            dma("sp", f"xs{sl}", xs[sl][:], x_loc[blk * 128:(blk + 1) * 128, :], (), ((f"xs{sl}",),))
            stt(x1[:, blk, :], xs[sl][:], ALPHA, x1[:, blk, :], ALU.mult, ALU.add, ((f"xs{sl}",), ("x1", blk)),
                (("x1", blk),))
            rstd, nmr, KR, KN = ln_stats(x1[:, blk, :], ("x1", blk), D)
            act(x1[:, blk, :], x1[:, blk, :], AF.Identity, (("x1", blk), KR, KN), (("x1", blk),),
                bias=nmr[:], scale=rstd[:])
            tt("pool", x1[:, blk, :], x1[:, blk, :], lng[:], ALU.mult, (("x1", blk), ("lng",)), (("x1", blk),))
            tt("dve", x1[:, blk, :], x1[:, blk, :], lnb[:], ALU.add, (("x1", blk), ("lnb",)), (("x1", blk),))
        for nm in ("mixT", "wo0", "wo1", "lng", "lnb", "xs0", "xs1"):
            S.free(nm)
        dbg("x1", x1[:], [128, NB, D], F32, ("x1", 0))
        ckpt("D")

        h2T = S.alloc("h2T", [128, KC, TOK], BF16, top=True)
        xn = [S.alloc(f"xn{i}", [128, D], BF16) for i in range(2)]
        XN[0] = xn
        wr = S.alloc("wr", [128, KC, 36], BF16)
        dma("pool", "wr", wr[:], wr_d.rearrange("(kc p) n -> p kc n", p=128), (), (("wr",),))
        if SPARSE:
            h2tok = S.alloc("h2tok", [128, NB, D], BF16, top=True)
            sc2_b = S.alloc("sc2_b", [128, D], F32)
            sh2_b = S.alloc("sh2_b", [128, D], F32)
            identf = S.alloc("identf", [128, 128], F32)
            onesF = S.alloc("onesF", [128, 128], F32)
            dg = [S.alloc(f"dg{i}", [128, 128], F32) for i in range(2)]
            h2tmp = S.alloc("h2tmp", [128, D], F32)
            tscal("dve", identf[:], iot[:], 0.0, None, ALU.is_equal, None, (("iot",),), (("identf",),))
            S.op("dve", lambda e: e.memset(onesF[:], 1.0), (), (("onesF",),))
            dgc = 0
            for (dst, dk, c0) in ((sh2_b, ("sh2_b",), 32), (sc2_b, ("sc2_b",), 48)):
                for kq in range(4):
                    bk, bkey = nbank()
                    for q in range(4):
                        kc = kq * 4 + q
                        di = dgc % 2
                        dgc += 1
                        tscal("dve", dg[di][:], identf[:], modT[:, c0 + kc:c0 + kc + 1], None, ALU.mult, None,
                              (("identf",), ("modT", 2)), ((f"dg{di}",),))
                        mm(bk[:, q * 128:(q + 1) * 128], onesF[:], dg[di][:], True, True,
                           ((f"dg{di}",), ("onesF",)), (bkey,), mark=True)
                    tcopy("dve", dst[:, kq * 512:(kq + 1) * 512], bk, (bkey,), (dk,))
        for blk in range(NB):
            ln_mod_T(x1[:, blk, :], ("x1", blk), lambda kc, blk=blk: h2T[:, kc, blk * 128:(blk + 1) * 128],
                     ("h2T",), 32)
            if SPARSE:
                xi = (cnt["ln"] - 1) % 2
                tt("pool", h2tmp[:], XN[0][xi][:], sc2_b[:], ALU.mult, ((f"xn{xi}",), ("sc2_b",)), (("h2tmp",),))
                tt("dve", h2tok[:, blk, :], h2tmp[:], sh2_b[:], ALU.add, (("h2tmp",), ("sh2_b",)), (("h2tok", blk),))
            mm_group(pq0[:, 0:36], ("pq0",), [(h2T[:, kc, blk * 128:(blk + 1) * 128], wr[:, kc, :],
                                               k2(("h2T",)) + (("wr",),)) for kc in range(KC)])
            tt("dve", logits[:, blk, :], pq0[:, 0:36], brb[:], ALU.add, (("pq0",), ("brb",)), (("logits",),))
            tscal("pool", x1[:, blk, :], x1[:, blk, :], ALPHA, None, ALU.mult, None, (("x1", blk),), (("x1", blk),))
        S.free("xn0"); S.free("xn1"); S.free("wr")
        if SPARSE:
            for nm in ("sc2_b", "sh2_b", "identf", "onesF", "dg0", "dg1", "h2tmp"):
                S.free(nm)
        dbg("h2T", h2T[:], [128, KC, TOK], BF16, ("h2T", 1))
        dbg("logits", logits[:], [128, NB, 36], F32, ("logits",))

        L = ("logits",)
        lg = logits[:, :, 0:4]
        le = logits[:, :, 4:36].rearrange("p b (g e) -> p b g e", e=8)

        def bc3(t2, n):
            return t2.unsqueeze(2).to_broadcast([128, NB, n])

        S.op("dve", lambda e: e.tensor_reduce(out=r_mg[:], in_=lg, axis=AX.X, op=ALU.max), (L,), (("r_mg",),))
        tt("dve", r_eg[:], lg, bc3(r_mg[:], 4), ALU.subtract, (L, ("r_mg",)), (("r_eg",),))
        tt("dve", r_oh[:], lg, bc3(r_mg[:], 4), ALU.is_equal, (L, ("r_mg",)), (("r_oh",),))
        act(r_eg[:], r_eg[:], AF.Exp, (("r_eg",),), (("r_eg",),))
        S.op("dve", lambda e: e.tensor_reduce(out=r_sg[:], in_=r_eg[:], axis=AX.X, op=ALU.add), (("r_eg",),), (("r_sg",),))
        S.op("dve", lambda e: e.reciprocal(out=r_pg[:], in_=r_sg[:]), (("r_sg",),), (("r_pg",),))
        tt("dve", r_t4[:], le, r_oh[:].unsqueeze(3).to_broadcast([128, NB, 4, 8]), ALU.mult, (L, ("r_oh",)), (("r_t4",),))
        S.op("dve", lambda e: e.tensor_reduce(out=r_sel[:], in_=r_t4[:].rearrange("p b g e -> p b e g"), axis=AX.X,
                                              op=ALU.add), (("r_t4",),), (("r_sel",),))
        S.op("dve", lambda e: e.tensor_reduce(out=r_l1[:], in_=r_sel[:], axis=AX.X, op=ALU.max), (("r_sel",),), (("r_l1",),))
        tt("dve", r_oh1[:], r_sel[:], bc3(r_l1[:], 8), ALU.is_equal, (("r_sel",), ("r_l1",)), (("r_oh1",),))
        stt(r_msk[:], r_oh1[:], -1e30, r_sel[:], ALU.mult, ALU.add, (("r_oh1",), ("r_sel",)), (("r_msk",),))
        S.op("dve", lambda e: e.tensor_reduce(out=r_l2[:], in_=r_msk[:], axis=AX.X, op=ALU.max), (("r_msk",),), (("r_l2",),))
        tt("dve", r_oh2[:], r_msk[:], bc3(r_l2[:], 8), ALU.is_equal, (("r_msk",), ("r_l2",)), (("r_oh2",),))
        tt("dve", r_w1[:], r_l2[:], r_l1[:], ALU.subtract, (("r_l2",), ("r_l1",)), (("r_w1",),))
        act(r_w1[:], r_w1[:], AF.Exp, (("r_w1",),), (("r_w1",),))
        tscal("dve", r_w1[:], r_w1[:], 1.0, None, ALU.add, None, (("r_w1",),), (("r_w1",),))
        S.op("dve", lambda e: e.reciprocal(out=r_w1[:], in_=r_w1[:]), (("r_w1",),), (("r_w1",),))
        tscal("dve", r_w2[:], r_w1[:], -1.0, 1.0, ALU.mult, ALU.add, (("r_w1",),), (("r_w2",),))
        tt("dve", r_w1[:], r_w1[:], r_pg[:], ALU.mult, (("r_w1",), ("r_pg",)), (("r_w1",),))
        tt("dve", r_w2[:], r_w2[:], r_pg[:], ALU.mult, (("r_w2",), ("r_pg",)), (("r_w2",),))
        tt("dve", r_ce[:], r_oh1[:], bc3(r_w1[:], 8), ALU.mult, (("r_oh1",), ("r_w1",)), (("r_ce",),))
        tt("dve", r_oh2[:], r_oh2[:], bc3(r_w2[:], 8), ALU.mult, (("r_oh2",), ("r_w2",)), (("r_oh2",),))
        tt("dve", r_ce[:], r_ce[:], r_oh2[:], ALU.add, (("r_ce",), ("r_oh2",)), (("r_ce",),))
        comb4 = comb[:].rearrange("p b (g e) -> p b g e", e=8)
        for g in range(4):
            tt("dve", comb4[:, :, g, :], r_ce[:], bc3(r_oh[:, :, g], 8), ALU.mult, (("r_ce",), ("r_oh",)), (("comb",),))
        dbg("comb", comb[:], [128, NB, 32], F32, ("comb",))
        ckpt("R")

        if SPARSE:
            S.free("h2T")
            NSLOT = 48
            NROW = NSLOT * 128
            Mf = S.alloc("Mf", [128, NB, 32], F32)
            Mb = S.alloc("Mb", [128, NB, 32], BF16)
            Ub = S.alloc("Ub", [128, 128], BF16)
            rank = S.alloc("rank", [128, NB, 32], F32)
            cntt = S.alloc("cntt", [128, 32], F32)
            nst = S.alloc("nst", [128, 32], F32)
            cs = [S.alloc(f"cs{i}", [128, 32], F32) for i in range(2)]
            sot = S.alloc("sot", [128, 32], F32)
            pos = S.alloc("pos", [128, NB, 32], F32)
            posm = S.alloc("posm", [128, NB, 32], F32)
            ptmp = S.alloc("ptmp", [128, NB, 32], F32)
            pAf = S.alloc("pAf", [128, NB], F32)
            pBf = S.alloc("pBf", [128, NB], F32)
            pAi = S.alloc("pAi", [128, NB], I32)
            pBi = S.alloc("pBi", [128, NB], I32)
            wA = S.alloc("wA", [128, NB], F32)
            wB = S.alloc("wB", [128, NB], F32)
            siota = S.alloc("siota", [128, NSLOT], F32)
            ecmp = S.alloc("ecmp", [128, NSLOT, 32], F32)
            eidf = S.alloc("eidf", [128, NSLOT], F32)
            eidi = S.alloc("eidi", [128, NSLOT], I32)
            C = ("comb",)
            tscal("dve", Mf[:], comb[:], 0.0, None, ALU.is_gt, None, (C,), (("Mf",),))
            tcopy("dve", Mb[:], Mf[:], (("Mf",),), (("Mb",),))
            tscal("dve", Ub[:], iot[:], 0.0, None, ALU.is_gt, None, (("iot",),), (("Ub",),))
            S.op("pool", lambda e: e.iota(siota[:], pattern=[[1, NSLOT]], base=0, channel_multiplier=0,
                                          allow_small_or_imprecise_dtypes=True), (), (("siota",),))
            for b in range(NB):
                bk, bkey = nbank()
                items = [(ones_bf[:], Mb[:, b2, :], (("Mb",), ("ones_bf",))) for b2 in range(b)]
                items.append((Ub[:], Mb[:, b, :], (("Mb",), ("Ub",))))
                mm_group(bk[:, 0:32], bkey, items)
                tcopy("dve", rank[:, b, :], bk[:, 0:32], (bkey,), (("rank",),))
            bk, bkey = nbank()
            mm_group(bk[:, 0:32], bkey, [(ones_bf[:], Mb[:, b2, :], (("Mb",), ("ones_bf",))) for b2 in range(NB)])
            tcopy("dve", cntt[:], bk[:, 0:32], (bkey,), (("cntt",),))
            tscal("dve", nst[:], cntt[:], 0.0, None, ALU.is_gt, None, (("cntt",),), (("nst",),))
            for k in range(1, 8):
                stt(nst[:], cntt[:], 128.0 * k, nst[:], ALU.is_gt, ALU.add, (("cntt",), ("nst",)), (("nst",),))
            tcopy("dve", cs[0][:], nst[:], (("nst",),), (("cs0",),))
            cur = 0
            for dstep in (1, 2, 4, 8, 16):
                nxt = 1 - cur
                tcopy("dve", cs[nxt][:], cs[cur][:], ((f"cs{cur}",),), ((f"cs{nxt}",),))
                tt("dve", cs[nxt][:, dstep:32], cs[cur][:, dstep:32], cs[cur][:, 0:32 - dstep], ALU.add,
                   ((f"cs{cur}",),), ((f"cs{nxt}",),))
                cur = nxt
            tt("dve", sot[:], cs[cur][:], nst[:], ALU.subtract, ((f"cs{cur}",), ("nst",)), (("sot",),))
            for b in range(NB):
                stt(pos[:, b, :], sot[:], 128.0, rank[:, b, :], ALU.mult, ALU.add, (("sot",), ("rank",)), (("pos",),))
            tscal("dve", ptmp[:], Mf[:], -1.0e6, 1.0e6, ALU.mult, ALU.add, (("Mf",),), (("ptmp",),))
            tt("dve", posm[:], pos[:], ptmp[:], ALU.add, (("pos",), ("ptmp",)), (("posm",),))
            S.op("dve", lambda e: e.tensor_reduce(out=pAf[:], in_=posm[:], axis=AX.X, op=ALU.min), (("posm",),), (("pAf",),))
            tt("dve", ptmp[:], pos[:], Mf[:], ALU.mult, (("pos",), ("Mf",), ("posm",)), (("ptmp",),))
            S.op("dve", lambda e: e.tensor_reduce(out=pBf[:], in_=ptmp[:], axis=AX.X, op=ALU.max), (("ptmp",),), (("pBf",),))
            tcopy("dve", pAi[:], pAf[:], (("pAf",),), (("pAi",),))
            tcopy("dve", pBi[:], pBf[:], (("pBf",),), (("pBi",),))
            tt("dve", ptmp[:], posm[:], bc3(pAf[:], 32), ALU.is_equal, (("posm",), ("pAf",), ("pBf",)), (("ptmp",),))
            tt("dve", ptmp[:], ptmp[:], comb[:], ALU.mult, (("ptmp",), C), (("ptmp",),))
            S.op("dve", lambda e: e.tensor_reduce(out=wA[:], in_=ptmp[:], axis=AX.X, op=ALU.add), (("ptmp",),), (("wA",),))
            S.op("dve", lambda e: e.tensor_reduce(out=wB[:], in_=comb[:], axis=AX.X, op=ALU.add), (C,), (("wB",),))
            tt("dve", wB[:], wB[:], wA[:], ALU.subtract, (("wB",), ("wA",)), (("wB",),))
            tt("dve", ecmp[:], sot[:].unsqueeze(1).to_broadcast([128, NSLOT, 32]),
               siota[:].unsqueeze(2).to_broadcast([128, NSLOT, 32]), ALU.is_le, (("sot",), ("siota",)), (("ecmp",),))
            S.op("dve", lambda e: e.tensor_reduce(out=eidf[:], in_=ecmp[:], axis=AX.X, op=ALU.add), (("ecmp",),), (("eidf",),))
            tscal("dve", eidf[:], eidf[:], -1.0, None, ALU.add, None, (("eidf",),), (("eidf",),))
            tcopy("dve", eidi[:], eidf[:], (("eidf",),), (("eidi",),))
            idxf = S.alloc("idxf", [128, NSLOT, 2, 2], F32)
            idxw = S.alloc("idxw", [128, NSLOT, 2, 2], I32)
            pcol = S.alloc("pcol", [128, 1], F32)
            tscal("dve", pcol[:], iot[:, 0:1], -2.0, None, ALU.mult, None, (("iot",),), (("pcol",),))
            for hf in range(2):
                for pc in range(2):
                    tscal("dve", idxf[:, :, hf, pc], eidf[:], 512.0, 256.0 * hf + pc, ALU.mult, ALU.add,
                          (("eidf",),), (("idxf",),))
            tscal("dve", idxf[:], idxf[:], pcol[:], None, ALU.add, None, (("idxf",), ("pcol",)), (("idxf",),))
            emp = S.alloc("emp", [128, NSLOT], F32)
            tscal("dve", emp[:], siota[:], cs[cur][:, 31:32], 32768.0, ALU.is_ge, ALU.mult,
                  (("siota",), (f"cs{cur}",)), (("emp",),))
            tt("dve", idxf[:].rearrange("p s a b -> p s (a b)"), idxf[:].rearrange("p s a b -> p s (a b)"),
               emp[:].unsqueeze(2).to_broadcast([128, NSLOT, 4]), ALU.add, (("idxf",), ("emp",)), (("idxf",),))
            S.free("emp")
            tcopy("dve", idxw[:], idxf[:], (("idxf",),), (("idxw",),))
            S.free("idxf"); S.free("pcol")
            dbg("pAi", pAi[:], [128, NB], I32, ("pAi",))
            dbg("pBi", pBi[:], [128, NB], I32, ("pBi",))
            dbg("eidi", eidi[:], [128, NSLOT], I32, ("eidi",))
            dbg("wA", wA[:], [128, NB], F32, ("wA",))
            for nm in ("Mf", "Mb", "Ub", "rank", "cntt", "nst", "cs0", "cs1", "sot", "pos", "posm", "ptmp",
                       "pAf", "pBf", "siota", "ecmp", "eidf"):
                S.free(nm)

            xs_scr = nc.dram_tensor("xs_scr", [NROW, D], BF16, kind="Internal").ap()
            ys_scr = nc.dram_tensor("ys_scr", [NROW, D], F32, kind="Internal").ap()
            XSK, YSK = ("xs_scr",), ("ys_scr",)
            Xs = [S.alloc(f"Xs{i}", [128, D], BF16) for i in range(2)]
            S.op("dve", lambda e: e.memset(Xs[0][:], 0.0), (), (("Xs0",),))
            XALL = tuple(("xs_scr", q_) for q_ in range(NSLOT))
            for s_ in range(NSLOT):
                dma("sp", f"xz{s_ % 4}", xs_scr[s_ * 128:(s_ + 1) * 128, :], Xs[0][:], (("Xs0",),),
                    (("xs_scr", s_), ("xs_scr", "z", s_ % 4)))
            SCK = []
            for b in range(NB):
                for ab, (pi_, pk_) in enumerate(((pAi, ("pAi",)), (pBi, ("pBi",)))):
                    sk = ("xs_scr", "sc", b * 2 + ab)
                    SCK.append(sk)
                    S.dma("pool", f"sc{b * 2 + ab}",
                          lambda e, b=b, pi_=pi_: e.indirect_dma_start(
                              out=xs_scr[:, :], out_offset=bass.IndirectOffsetOnAxis(ap=pi_[:, b:b + 1], axis=0),
                              in_=h2tok[:, b, :], in_offset=None),
                          (("h2tok", b), pk_) + XALL, (sk,))
            SCK = tuple(SCK)
            S.free("h2tok")

            NSL = 3
            wg = [S.alloc(f"wg{i}", [128, KC, 256], BF16) for i in range(NSL)]
            wu = [S.alloc(f"wu{i}", [128, KC, 256], BF16) for i in range(NSL)]
            wd = [S.alloc(f"wd{i}", [128, 2, D], BF16) for i in range(NSL)]
            XsT = S.alloc("XsT", [128, KC, 128], BF16)
            sgt = S.alloc("sgt", [128, 256], BF16)
            hidh = S.alloc("hidh", [128, 256], BF16)
            hidT = S.alloc("hidT", [128, 2, 128], BF16)
            Ys = S.alloc("Ys", [128, D], F32)

            def load_unit(u):
                s_, hf = u // 2, u % 2
                sl = u % NSL
                for (nm, dst, src) in (("wg", wg[sl], w_gate), ("wu", wu[sl], w_up), ("wd", wd[sl], w_down)):
                    for pc in range(2):
                        if nm == "wd":
                            d2 = dst[:, pc, :]
                        else:
                            d2 = dst[:, pc * 8:(pc + 1) * 8, :].rearrange("p a b -> p (a b)")
                        S.dma("pool", f"{nm}{sl}{pc}",
                              lambda e, d2=d2, src=src, s_=s_, hf=hf, pc=pc: e.indirect_dma_start(
                                  out=d2, out_offset=None, in_=src[:, :],
                                  in_offset=bass.IndirectOffsetOnAxis(ap=idxw[:, s_, hf, pc:pc + 1], axis=0),
                                  bounds_check=BREG["r"], oob_is_err=False),
                              (("idxw",),), ((f"{nm}{sl}", pc),))

            load_unit(0)
            for s_ in range(NSLOT):
                xsl = s_ % 2
                dma("sp", f"Xs{xsl}", Xs[xsl][:], xs_scr[s_ * 128:(s_ + 1) * 128, :], (("xs_scr", s_),) + SCK, ((f"Xs{xsl}",),))
                for half, (pt, pk) in enumerate(((ptA, ("ptA",)), (ptB, ("ptB",)))):
                    for q in range(8):
                        kc = half * 8 + q
                        transpose(pt[:, q * 128:(q + 1) * 128], pk, Xs[xsl][:, kc * 128:(kc + 1) * 128], (f"Xs{xsl}",),
                                  mark=(q == 7))
                    src = pt[:, :].rearrange("p (c q) -> p c q", q=128)
                    if half == 0:
                        tcopy("dve", XsT[:, 0:8, :], src, (pk,), (("XsT", 0),))
                    else:
                        act(XsT[:, 8:16, :], src, AF.Copy, (pk,), (("XsT", 1),))
                for hf in range(2):
                    u = 2 * s_ + hf
                    sl = u % NSL
                    if u + 1 < 2 * NSLOT:
                        load_unit(u + 1)
                    for c in range(2):
                        tt("pool", wd[sl][:, c, :], wd[sl][:, c, :], g2_b[:], ALU.mult, ((f"wd{sl}", c), ("g2_b",)),
                           ((f"wd{sl}", c),))
                    mm_group(pq0[:, 0:256], ("pq0",), [(XsT[:, kc, :], wg[sl][:, kc, :],
                                                        (("XsT", 0), ("XsT", 1), (f"wg{sl}", kc // 8))) for kc in range(KC)])
                    mm_group(pq1[:, 0:256], ("pq1",), [(XsT[:, kc, :], wu[sl][:, kc, :],
                                                        (("XsT", 0), ("XsT", 1), (f"wu{sl}", kc // 8))) for kc in range(KC)])
                    act(sgt[:], pq0[:, 0:256], AF.Silu, (("pq0",),), (("sgt",),))
                    tt("dve", hidh[:], sgt[:], pq1[:, 0:256], ALU.mult, (("sgt",), ("pq1",)), (("hidh",),))
                    for kf in range(2):
                        transpose(ptA[:, kf * 128:(kf + 1) * 128], ("ptA",), hidh[:, kf * 128:(kf + 1) * 128], ("hidh",),
                                  mark=(kf == 1))
                    tcopy("dve", hidT[:], ptA[:, 0:256].rearrange("p (c q) -> p c q", q=128), (("ptA",),), (("hidT",),))
                    for cg in range(4):
                        for kf in range(2):
                            first = (hf == 0 and kf == 0)
                            last = (hf == 1 and kf == 1)
                            mm(pbig[:, cg * 512:(cg + 1) * 512], hidT[:, kf, :], wd[sl][:, kf, cg * 512:(cg + 1) * 512],
                               first, last, (("hidT",), (f"wd{sl}", kf)),
                               (("pbig", cg),) if (first or last) else (), mark=last)
                for cg in range(4):
                    if cg % 2 == 0:
                        tcopy("dve", Ys[:, cg * 512:(cg + 1) * 512], pbig[:, cg * 512:(cg + 1) * 512], (("pbig", cg),),
                              (("Ys", 0),))
                    else:
                        act(Ys[:, cg * 512:(cg + 1) * 512], pbig[:, cg * 512:(cg + 1) * 512], AF.Copy, (("pbig", cg),),
                            (("Ys", 1),))
                dma("sp", "ysst", ys_scr[s_ * 128:(s_ + 1) * 128, :], Ys[:], (("Ys", 0), ("Ys", 1)), (YSK,))
            for i in range(NSL):
                S.free(f"wg{i}"); S.free(f"wu{i}"); S.free(f"wd{i}")
            for nm in ("Xs0", "Xs1", "XsT", "sgt", "hidh", "hidT", "Ys"):
                S.free(nm)
            yg = [S.alloc(f"yg{i}", [128, D], F32) for i in range(4)]
            gi = 0
            for b in range(NB):
                for (pi_, pk_, wt_, wk_) in ((pAi, ("pAi",), wA, ("wA",)), (pBi, ("pBi",), wB, ("wB",))):
                    g_ = gi % 4
                    gi += 1
                    S.dma("pool", f"yg{g_}",
                          lambda e, b=b, pi_=pi_, g_=g_: e.indirect_dma_start(
                              out=yg[g_][:], out_offset=None, in_=ys_scr[:, :],
                              in_offset=bass.IndirectOffsetOnAxis(ap=pi_[:, b:b + 1], axis=0)),
                          (YSK, pk_), ((f"yg{g_}",),))
                    stt(x1[:, b, :], yg[g_][:], wt_[:, b:b + 1], x1[:, b, :], ALU.mult, ALU.add,
                        ((f"yg{g_}",), wk_, ("x1", b)), (("x1", b),))
            for nm in ("yg0", "yg1", "yg2", "yg3", "pAi", "pBi", "wA", "wB", "eidi", "idxw"):
                S.free(nm)
        else:
            NSL = 3
            wg = [S.alloc(f"wg{i}", [128, KC, 256], BF16) for i in range(NSL)]
            wu = [S.alloc(f"wu{i}", [128, KC, 256], BF16) for i in range(NSL)]
            wd = [S.alloc(f"wd{i}", [128, 2, D], BF16) for i in range(NSL)]
            hid = [S.alloc(f"hid{i}", [128, 2, TOK], BF16) for i in range(2)]
            sgt = [S.alloc(f"sgt{i}", [128, 512], BF16) for i in range(2)]
            sgc = 0
            pqs = ((pq0, ("pq0",)), (pq1, ("pq1",)))
            pqc = 0
            def load_unit(u):
                e_, hf = u // 2, u % 2
                sl = u % NSL
                dma("pool", f"wg{sl}", wg[sl][:], w_gate[e_].rearrange("(kc p) n -> p kc n", p=128)[:, :, hf * 256:(hf + 1) * 256],
                    (), ((f"wg{sl}",),))
                dma("pool", f"wu{sl}", wu[sl][:], w_up[e_].rearrange("(kc p) n -> p kc n", p=128)[:, :, hf * 256:(hf + 1) * 256],
                    (), ((f"wu{sl}",),))
                dma("pool", f"wd{sl}", wd[sl][:], w_down[e_][hf * 256:(hf + 1) * 256, :].rearrange("(c p) n -> p c n", p=128),
                    (), ((f"wd{sl}",),))

            load_unit(0)
            for u in range(2 * NEXP):
                e_, hf = u // 2, u % 2
                sl = u % NSL
                hi = u % 2
                if u + 1 < 2 * NEXP:
                    load_unit(u + 1)
                for c in range(2):
                    tt("pool", wd[sl][:, c, :], wd[sl][:, c, :], g2_b[:], ALU.mult, ((f"wd{sl}",), ("g2_b",)), ((f"wd{sl}",),))
                for n in range(2):
                    for ffc in range(2):
                        bg, bgk = nbank()
                        mm_group(bg, bgk, [(wg[sl][:, kc, ffc * 128:(ffc + 1) * 128], h2T[:, kc, n * 512:(n + 1) * 512],
                                            ((f"wg{sl}",),) + k2(("h2T",))) for kc in range(KC)])
                        bu, buk = nbank()
                        mm_group(bu, buk, [(wu[sl][:, kc, ffc * 128:(ffc + 1) * 128], h2T[:, kc, n * 512:(n + 1) * 512],
                                            ((f"wu{sl}",),) + k2(("h2T",))) for kc in range(KC)])
                        si = sgc % 2
                        sgc += 1
                        act(sgt[si][:], bg, AF.Silu, (bgk,), ((f"sgt{si}",),))
                        tt("dve", hid[hi][:, ffc, n * 512:(n + 1) * 512], sgt[si][:], bu, ALU.mult,
                           ((f"sgt{si}",), buk), ((f"hid{hi}", n),))
                    for bb in range(4):
                        blk = n * 4 + bb
                        for cg in range(4):
                            pq, pqk = pqs[pqc % 2]
                            pqc += 1
                            mm_group(pq[:, :], pqk, [(hid[hi][:, c, blk * 128:(blk + 1) * 128], wd[sl][:, c, cg * 512:(cg + 1) * 512],
                                                      ((f"hid{hi}", n), (f"wd{sl}",))) for c in range(2)])
                            stt(x1[:, blk, cg * 512:(cg + 1) * 512], pq[:, :], comb[:, blk, e_:e_ + 1],
                                x1[:, blk, cg * 512:(cg + 1) * 512], ALU.mult, ALU.add,
                                (pqk, ("comb",), ("x1", blk)), (("x1", blk),))
            for i in range(NSL):
                S.free(f"wg{i}"); S.free(f"wu{i}"); S.free(f"wd{i}")
            S.free("hid0"); S.free("hid1"); S.free("sgt0"); S.free("sgt1"); S.free("h2T")

        lng2 = S.alloc("lng2", [128, D], F32)
        lnb2 = S.alloc("lnb2", [128, D], F32)
        dma("sp", "c_ln2g", lng2[:], ln2g_d[0:1, :].to_broadcast([128, D]), (), (("lng2",),))
        dma("sp", "c_ln2b", lnb2[:], ln2b_d[0:1, :].to_broadcast([128, D]), (), (("lnb2",),))
        for blk in range(NB):
            rstd, nmr, KR, KN = ln_stats(x1[:, blk, :], ("x1", blk), D)
            act(x1[:, blk, :], x1[:, blk, :], AF.Identity, (("x1", blk), KR, KN), (("x1", blk),),
                bias=nmr[:], scale=rstd[:])
            tt("pool", x1[:, blk, :], x1[:, blk, :], lng2[:], ALU.mult, (("x1", blk), ("lng2",)), (("x1", blk),))
            tt("dve", x1[:, blk, :], x1[:, blk, :], lnb2[:], ALU.add, (("x1", blk), ("lnb2",)), (("x1", blk),))
            dma("sp", "out", out_d[blk * 128:(blk + 1) * 128, :], x1[:, blk, :], (("x1", blk),), (("outd",),))

    try:
        body()
    except _Stop:
        pass
    S.wait_all("sp", list(S.dch.values()))

    import contextlib
    with contextlib.ExitStack() as es:
        for ch in list(S.chan.values()) + list(S.dch.values()):
            ch.sem = es.enter_context(nc.semaphore("s_" + ch.name))
        block = es.enter_context(nc.Block())

        def emit(name, e):
            if name == "pool" and SPARSE:
                BREG["r"] = e.alloc_register("bnd_reg")
                e.reg_mov(BREG["r"], NEXP * 512 - 1)
            for waits, fn, ch in S.ops[name]:
                for (c, v) in waits:
                    e.wait_ge(c.sem, v)
                if fn is None:
                    continue
                ins = fn(e)
                if ch is not None:
                    ins.then_inc(ch.sem, ch.inc)

        block.tensor(lambda e: emit("pe", e))
        block.scalar(lambda e: emit("act", e))
        block.vector(lambda e: emit("dve", e))
        block.gpsimd(lambda e: emit("pool", e))
        block.sync(lambda e: emit("sp", e))
    return nc, dbg_out, S


OWN = {0: [0, 3, 4, 7, 8, 11, 12, 15], 1: [1, 2, 5, 6, 9, 10, 13, 14]}


def make_in_maps(x, c, positions, w_ada, b_ada, w_in, q_norm_g, w_uq, kv_norm_g, w_ukv,
                 sgu_norm_g, sgu_norm_b, w_spatial, b_spatial, w_o, ln1_g, ln1_b,
                 w_router_group, b_router_group, w_router_expert, b_router_expert,
                 w_gate, w_up, w_down, ln2_g, ln2_b):
    f = lambda a: np.ascontiguousarray(np.asarray(a), dtype=np.float32)

    def relayout_gu(w):
        w = np.asarray(w, dtype=np.float32)
        if not SPARSE:
            return np.ascontiguousarray(w)
        E = w.shape[0]
        return np.ascontiguousarray(w.reshape(E, KC, 128, 2, 256).transpose(0, 3, 2, 1, 4)).reshape(E * 512, 2048)

    def relayout_d(w):
        w = np.asarray(w, dtype=np.float32)
        if not SPARSE:
            return np.ascontiguousarray(w)
        E = w.shape[0]
        return np.ascontiguousarray(w.reshape(E, 2, 2, 128, D).transpose(0, 1, 3, 2, 4)).reshape(E * 512, D)

    x = f(x); c = f(c)
    positions = np.asarray(positions).astype(np.int32)
    inv_freq = (1.0 / (10000.0 ** (np.arange(0, 64, 2, dtype=np.float32) / 64.0))).astype(np.float32)
    ropec = np.zeros((64, 2), np.float32)
    ropec[:, 0] = np.concatenate([inv_freq, inv_freq])
    ropec[:, 1] = np.concatenate([-np.ones(32, np.float32), np.ones(32, np.float32)])
    shared = {
        "ropec": ropec,
        "w_ada": f(w_ada[0]), "b_ada": f(b_ada[0]).reshape(1, -1), "w_in": f(w_in[0]),
        "qg": f(np.asarray(q_norm_g[0]).reshape(6, 128).T), "w_uq": f(w_uq[0]),
        "kvg": f(np.asarray(kv_norm_g[0]).reshape(4, 128).T), "w_ukv": f(w_ukv[0]),
        "sgug": f(sgu_norm_g[0]).reshape(1, -1), "sgub": f(sgu_norm_b[0]).reshape(1, -1),
        "wspT": f(np.asarray(w_spatial[0]).transpose(2, 0, 1)),
        "bsp": f(b_spatial[0]).reshape(1, -1),
        "w_o": f(w_o[0]), "ln1g": f(ln1_g[0]).reshape(1, -1), "ln1b": f(ln1_b[0]).reshape(1, -1),
        "wr": f(np.concatenate([np.asarray(w_router_group[0]), np.asarray(w_router_expert[0])], axis=1)),
        "br": f(np.concatenate([np.asarray(b_router_group[0]), np.asarray(b_router_expert[0])])).reshape(1, -1),
        "w_gate": relayout_gu(w_gate[0]), "w_up": relayout_gu(w_up[0]), "w_down": relayout_d(w_down[0]),
        "ln2g": f(ln2_g[0]).reshape(1, -1), "ln2b": f(ln2_b[0]).reshape(1, -1),
    }
    in_maps = []
    for core in range(8):
        b, p = core // 2, core % 2
        own, oth = OWN[p], OWN[1 - p]
        order = own + oth
        rows = np.concatenate([np.arange(k * 128, (k + 1) * 128) for k in order])
        m = dict(shared)
        m["x_loc"] = np.ascontiguousarray(x[b][rows])
        m["cT"] = np.ascontiguousarray(c[b].reshape(16, 128).T)
        m["pos"] = np.ascontiguousarray(positions[b][rows].reshape(1, -1))
        m["qidx"] = np.ascontiguousarray(
            (np.array(own, np.float32)[None, :] * 128 + np.arange(128, dtype=np.float32)[:, None]))
        m["oblk"] = np.ascontiguousarray(np.broadcast_to(np.array(oth, np.float32)[None, :] * 128, (128, 8)))
        in_maps.append(m)
    return in_maps


def assemble(results):
    out = np.zeros((4, 2048, 2048), np.float32)
    for core in range(8):
        b, p = core // 2, core % 2
        y = np.asarray(results[core]["out"])
        for j, k in enumerate(OWN[p]):
            out[b, k * 128:(k + 1) * 128] = y[j * 128:(j + 1) * 128]
    return out


def kernel(**inputs):
    nc, _, _ = build_nc()
    in_maps = make_in_maps(**inputs)
    res = run_bass_kernel_spmd(nc, in_maps, core_ids=list(range(8)))
    return assemble(res.results)
```
